# Optimizing a Trainium2 kernel written in Bass

```python
import math
import jax, jax.numpy as jnp
from jax import lax
import numpy as np

D_MODEL = 2048
BATCH = 4
SEQ = 2048
DEPTH = 4
DEC_BATCH = 128
DEC_SEQ = 1
PAST_LEN = 16384
PAGE_SIZE = 128

N_MIXERS = 4
NORM_EPS = 1e-6
S5_GROUP = 16
S5_GROUPS = D_MODEL // S5_GROUP
S5_STATE = 64
RW_HEAD = 64
RW_HEADS = D_MODEL // RW_HEAD
RW_DECAY_LORA = 96
RW_A_LORA = 96
RW_GATE_LORA = 256
RW_LN_EPS = 64e-5
GLA_HEADS = 4
GLA_KEY = D_MODEL // 2
GLA_VAL = D_MODEL
GLA_DK = GLA_KEY // GLA_HEADS
GLA_DV = GLA_VAL // GLA_HEADS
GLA_GATE_RANK = 16
GLA_GATE_TEMP = 16.0
HG_EXPAND = 128
HG_HEADS = D_MODEL // HG_EXPAND
HG_DK = HG_EXPAND
HG_DV = D_MODEL // HG_HEADS
CHUNK = 64
D_FF = 5632
CONV_W = 3

kernel_name = 'hybrid_s5_rwkv7_gla_hgrn2_convffn_step'


def _rmsnorm(x, g):
    xf = x.astype(jnp.float32)
    y = xf * lax.rsqrt(jnp.mean(xf * xf, axis=-1, keepdims=True) + NORM_EPS)
    return (y * g.astype(jnp.float32)).astype(x.dtype)


def _headnorm(o, g):
    o = o * lax.rsqrt(jnp.mean(o * o, axis=-1, keepdims=True) + NORM_EPS)
    return o * g.astype(jnp.float32)


def _chunked_gated_la(q, k, v, log_g, s0):
    b, t, h, dk = q.shape
    dv = v.shape[-1]
    c = min(CHUNK, t)
    pad = (-t) % c
    if pad:
        widths = ((0, 0), (0, pad), (0, 0), (0, 0))
        q, k, v, log_g = [jnp.pad(a, widths) for a in (q, k, v, log_g)]
    n = (t + pad) // c

    def blocks(a):
        return a.reshape(b, n, c, h, a.shape[-1]).transpose(1, 0, 3, 2, 4)

    causal = jnp.tril(jnp.ones((c, c), dtype=bool))[:, :, None]

    def step(s, blk):
        qb, kb, vb, gb = blk
        cum = jnp.cumsum(gb, axis=2)
        o_inter = jnp.einsum('bhtd,bhdv->bhtv', qb * jnp.exp(cum), s)
        diff = cum[:, :, :, None, :] - cum[:, :, None, :, :]
        decay = jnp.exp(jnp.where(causal, diff, -jnp.inf))
        att = jnp.einsum('bhtd,bhsd,bhtsd->bhts', qb, kb, decay)
        o = o_inter + jnp.einsum('bhts,bhsv->bhtv', att, vb)
        last = cum[:, :, -1:, :]
        s = jnp.exp(last[:, :, 0, :])[..., None] * s + jnp.einsum('bhsd,bhsv->bhdv', kb * jnp.exp(last - cum), vb)
        return s, o

    s, o = lax.scan(step, s0.astype(jnp.float32), (blocks(q), blocks(k), blocks(v), blocks(log_g)))
    o = o.transpose(1, 0, 3, 2, 4).reshape(b, n * c, h, dv)[:, :t]
    return o, s


def _s5_mixer(u, h_re, h_im, lam_re, lam_im, log_dt, b_re, b_im, c_re, c_im, d, w_glu):
    f32 = jnp.float32
    bsz, t, _ = u.shape
    uf = u.astype(f32)
    lr = jnp.minimum(lam_re.astype(f32), -1e-4)
    li = lam_im.astype(f32)
    dt = jnp.exp(log_dt.astype(f32))[:, None]
    mag = jnp.exp(lr * dt)
    ab_re = mag * jnp.cos(li * dt)
    ab_im = mag * jnp.sin(li * dt)
    den = lr * lr + li * li
    f_re = ((ab_re - 1.0) * lr + ab_im * li) / den
    f_im = (ab_im * lr - (ab_re - 1.0) * li) / den
    br = b_re.astype(f32)
    bi = b_im.astype(f32)
    bb_re = f_re[..., None] * br - f_im[..., None] * bi
    bb_im = f_re[..., None] * bi + f_im[..., None] * br
    ug = uf.reshape(bsz, t, S5_GROUPS, S5_GROUP)
    bu_re = jnp.einsum('gnp,btgp->btgn', bb_re, ug)
    bu_im = jnp.einsum('gnp,btgp->btgn', bb_im, ug)
    h_re = h_re.astype(f32)
    h_im = h_im.astype(f32)
    bu_re = bu_re.at[:, 0].add(ab_re * h_re - ab_im * h_im)
    bu_im = bu_im.at[:, 0].add(ab_re * h_im + ab_im * h_re)
    a_re = jnp.broadcast_to(ab_re, bu_re.shape)
    a_im = jnp.broadcast_to(ab_im, bu_im.shape)

    def combine(e1, e2):
        a1r, a1i, b1r, b1i = e1
        a2r, a2i, b2r, b2i = e2
        return (a2r * a1r - a2i * a1i, a2r * a1i + a2i * a1r,
                a2r * b1r - a2i * b1i + b2r, a2r * b1i + a2i * b1r + b2i)

    _, _, hs_re, hs_im = lax.associative_scan(combine, (a_re, a_im, bu_re, bu_im), axis=1)
    y = jnp.einsum('gpn,btgn->btgp', c_re.astype(f32), hs_re) - jnp.einsum('gpn,btgn->btgp', c_im.astype(f32), hs_im)
    y = y.reshape(bsz, t, D_MODEL) + d.astype(f32) * uf
    z = jax.nn.gelu(y).astype(u.dtype)
    out = z * jax.nn.sigmoid(z @ w_glu)
    return out, hs_re[:, -1], hs_im[:, -1]


def _rwkv7_mixer(xn, shift_prev, wkv0, mix, w_r, w_k, w_v, w_o, w0, w1, w2, a0, a1, a2, g1, g2, k_k, k_a, r_k, ln_w, ln_b):
    f32 = jnp.float32
    bsz, t, _ = xn.shape
    x_prev = jnp.concatenate([shift_prev[:, None].astype(xn.dtype), xn[:, :-1]], axis=1)
    xx = x_prev - xn
    xr = xn + xx * mix[0]
    xw = xn + xx * mix[1]
    xk = xn + xx * mix[2]
    xv = xn + xx * mix[3]
    xa = xn + xx * mix[4]
    xg = xn + xx * mix[5]
    r = (xr @ w_r).astype(f32)
    k = (xk @ w_k).astype(f32)
    v = (xv @ w_v).astype(f32)
    w = -jax.nn.softplus(-(w0.astype(f32) + (jnp.tanh(xw @ w1) @ w2).astype(f32))) - 0.5
    decay = jnp.exp(-jnp.exp(w))
    a = jax.nn.sigmoid(a0.astype(f32) + ((xa @ a1) @ a2).astype(f32))
    g = jax.nn.sigmoid(xg @ g1) @ g2
    heads = lambda z: z.reshape(bsz, t, RW_HEADS, RW_HEAD)
    kk = heads(k * k_k.astype(f32))
    kk = kk / jnp.maximum(jnp.linalg.norm(kk, axis=-1, keepdims=True), 1e-12)
    k = k * (1.0 + (a - 1.0) * k_a.astype(f32))
    rh = heads(r)
    kh = heads(k)
    vh = heads(v)
    ah = heads(a)
    wh = heads(decay)
    seq_first = lambda z: jnp.moveaxis(z, 1, 0)

    def step(s, inp):
        r_t, w_t, k_t, v_t, a_t, b_t = inp
        sa = jnp.einsum('bhij,bhj->bhi', s, a_t)
        s = s * w_t[:, :, None, :] + sa[..., None] * b_t[:, :, None, :] + v_t[..., None] * k_t[:, :, None, :]
        return s, jnp.einsum('bhij,bhj->bhi', s, r_t)

    xs = (seq_first(rh), seq_first(wh), seq_first(kh), seq_first(vh), seq_first(-kk), seq_first(kk * ah))
    s, o = lax.scan(step, wkv0.astype(f32), xs)
    o = jnp.moveaxis(o, 0, 1)
    mu = jnp.mean(o, axis=-1, keepdims=True)
    var = jnp.mean(jnp.square(o - mu), axis=-1, keepdims=True)
    o = ((o - mu) * lax.rsqrt(var + RW_LN_EPS)).reshape(bsz, t, D_MODEL) * ln_w.astype(f32) + ln_b.astype(f32)
    bonus = jnp.sum(rh * kh * r_k.astype(f32), axis=-1, keepdims=True) * vh
    o = o + bonus.reshape(bsz, t, D_MODEL)
    out = (o.astype(xn.dtype) * g) @ w_o
    return out, xn[:, -1], s


def _gla_mixer(xn, s0, w_q, w_k, w_v, w_gk1, w_gk2, b_gk, w_g, norm_w, w_o):
    f32 = jnp.float32
    bsz, t, _ = xn.shape
    q = (xn @ w_q).astype(f32).reshape(bsz, t, GLA_HEADS, GLA_DK) * (GLA_DK ** -0.5)
    k = (xn @ w_k).astype(f32).reshape(bsz, t, GLA_HEADS, GLA_DK)
    v = (xn @ w_v).astype(f32).reshape(bsz, t, GLA_HEADS, GLA_DV)
    log_g = jax.nn.log_sigmoid(((xn @ w_gk1) @ w_gk2).astype(f32) + b_gk.astype(f32)) / GLA_GATE_TEMP
    o, s = _chunked_gated_la(q, k, v, log_g.reshape(bsz, t, GLA_HEADS, GLA_DK), s0)
    o = _headnorm(o, norm_w).reshape(bsz, t, GLA_VAL)
    out = (o.astype(xn.dtype) * jax.nn.silu(xn @ w_g)) @ w_o
    return out, s


def _hgrn2_mixer(xn, s0, layer_idx, w_q, w_f, w_i, w_g, lb_param, norm_w, w_o):
    f32 = jnp.float32
    bsz, t, _ = xn.shape
    p = jax.nn.softmax(lb_param.astype(f32), axis=0)
    lb = (jnp.cumsum(p, axis=0) - p[0])[layer_idx]
    q = (xn @ w_q).astype(f32)
    z = (xn @ w_f).astype(f32)
    i = (xn @ w_i).astype(f32)
    log_f = jnp.logaddexp(jnp.log(lb), jnp.log1p(-lb) + jax.nn.log_sigmoid(z))
    k = jnp.exp(jnp.log1p(-lb) + jax.nn.log_sigmoid(-z))
    hk = lambda a: a.reshape(bsz, t, HG_HEADS, HG_DK)
    o, s = _chunked_gated_la(hk(q) * (HG_DK ** -0.5), hk(k), i.reshape(bsz, t, HG_HEADS, HG_DV), hk(log_f), s0)
    o = _headnorm(o, norm_w).reshape(bsz, t, D_MODEL)
    out = (o.astype(xn.dtype) * jax.nn.silu(xn @ w_g)) @ w_o
    return out, s


def _conv_ffn(xn, prev, w_up, w_gate, w_conv, b_conv, w_down):
    u = xn @ w_up
    gate = xn @ w_gate
    full = jnp.concatenate([prev.astype(u.dtype), u], axis=1)
    t = u.shape[1]
    c = sum(full[:, j:j + t] * w_conv[j] for j in range(CONV_W)) + b_conv
    out = (jax.nn.gelu(c) * gate) @ w_down
    return out, full[:, -(CONV_W - 1):]


def _trunk(x, s5_re, s5_im, rw_shift, rw_wkv, gla_s, hg_s, ffn_conv, p):
    h = x
    conv_new = []
    for i in range(DEPTH):
        xn = _rmsnorm(h, p['norm_mix'][i])
        kind = i % N_MIXERS
        if kind == 0:
            mix, s5_re, s5_im = _s5_mixer(xn, s5_re, s5_im, p['s5_lambda_re'], p['s5_lambda_im'], p['s5_log_dt'],
                                          p['s5_b_re'], p['s5_b_im'], p['s5_c_re'], p['s5_c_im'], p['s5_d'], p['s5_w_glu'])
        elif kind == 1:
            mix, rw_shift, rw_wkv = _rwkv7_mixer(xn, rw_shift, rw_wkv, p['rw_mix'], p['rw_w_r'], p['rw_w_k'], p['rw_w_v'],
                                                 p['rw_w_o'], p['rw_w0'], p['rw_w1'], p['rw_w2'], p['rw_a0'], p['rw_a1'],
                                                 p['rw_a2'], p['rw_g1'], p['rw_g2'], p['rw_k_k'], p['rw_k_a'], p['rw_r_k'],
                                                 p['rw_ln_w'], p['rw_ln_b'])
        elif kind == 2:
            mix, gla_s = _gla_mixer(xn, gla_s, p['gla_w_q'], p['gla_w_k'], p['gla_w_v'], p['gla_w_gk1'], p['gla_w_gk2'],
                                    p['gla_b_gk'], p['gla_w_g'], p['gla_norm'], p['gla_w_o'])
        else:
            mix, hg_s = _hgrn2_mixer(xn, hg_s, i, p['hg_w_q'], p['hg_w_f'], p['hg_w_i'], p['hg_w_g'], p['hg_lb'],
                                     p['hg_norm'], p['hg_w_o'])
        h = h + mix.astype(h.dtype)
        f, c = _conv_ffn(_rmsnorm(h, p['norm_ffn'][i]), ffn_conv[i], p['ffn_w_up'][i], p['ffn_w_gate'][i],
                         p['ffn_w_conv'][i], p['ffn_b_conv'][i], p['ffn_w_down'][i])
        h = h + f.astype(h.dtype)
        conv_new.append(c)
    y = _rmsnorm(h, p['norm_final'])
    return y, s5_re, s5_im, rw_shift, rw_wkv, gla_s, hg_s, jnp.stack(conv_new)


def setup_inputs(seed: int = 0) -> dict:
    key = jax.random.key(seed)
    ks = iter(jax.random.split(key, 96))
    f32 = jnp.float32
    nrm = lambda shape, scale: jax.random.normal(next(ks), shape, f32) * scale
    unif = lambda shape, lo, hi: jax.random.uniform(next(ks), shape, f32, lo, hi)
    D = D_MODEL
    G, N, P = S5_GROUPS, S5_STATE, S5_GROUP
    ratio = jnp.arange(D, dtype=f32) / (D - 1)
    conv_base = jnp.zeros((CONV_W,), f32).at[-1].set(1.0)[None, :, None]
    return {
        'x_prompt': nrm((BATCH, SEQ, D), 1.0),
        'x_sample': nrm((DEC_BATCH, DEC_SEQ, D), 1.0),
        'state_s5_re': nrm((DEC_BATCH, G, N), 0.5),
        'state_s5_im': nrm((DEC_BATCH, G, N), 0.5),
        'state_rwkv_shift': nrm((DEC_BATCH, D), 1.0),
        'state_rwkv_wkv': nrm((DEC_BATCH, RW_HEADS, RW_HEAD, RW_HEAD), 0.3),
        'state_gla': nrm((DEC_BATCH, GLA_HEADS, GLA_DK, GLA_DV), 0.1),
        'state_hgrn': nrm((DEC_BATCH, HG_HEADS, HG_DK, HG_DV), 0.1),
        'state_ffn_conv': nrm((DEPTH, DEC_BATCH, CONV_W - 1, D_FF), 1.0),
        'norm_mix': 1.0 + nrm((DEPTH, D), 0.02),
        'norm_ffn': 1.0 + nrm((DEPTH, D), 0.02),
        'norm_final': 1.0 + nrm((D,), 0.02),
        's5_lambda_re': -0.5 + nrm((G, N), 0.01),
        's5_lambda_im': math.pi * jnp.arange(N, dtype=f32)[None, :] + nrm((G, N), 0.01),
        's5_log_dt': unif((G,), math.log(1e-3), math.log(1e-1)),
        's5_b_re': nrm((G, N, P), (2 * P) ** -0.5),
        's5_b_im': nrm((G, N, P), (2 * P) ** -0.5),
        's5_c_re': nrm((G, P, N), (2 * N) ** -0.5),
        's5_c_im': nrm((G, P, N), (2 * N) ** -0.5),
        's5_d': nrm((D,), 1.0),
        's5_w_glu': nrm((D, D), D ** -0.5),
        'rw_mix': unif((6, D), 0.0, 1.0),
        'rw_w_r': nrm((D, D), D ** -0.5),
        'rw_w_k': nrm((D, D), D ** -0.5),
        'rw_w_v': nrm((D, D), D ** -0.5),
        'rw_w_o': nrm((D, D), D ** -0.5),
        'rw_w0': -6.0 + 5.0 * ratio ** 0.9 + nrm((D,), 0.1),
        'rw_w1': nrm((D, RW_DECAY_LORA), D ** -0.5),
        'rw_w2': nrm((RW_DECAY_LORA, D), 0.1 * RW_DECAY_LORA ** -0.5),
        'rw_a0': nrm((D,), 0.1),
        'rw_a1': nrm((D, RW_A_LORA), D ** -0.5),
        'rw_a2': nrm((RW_A_LORA, D), 0.1 * RW_A_LORA ** -0.5),
        'rw_g1': nrm((D, RW_GATE_LORA), D ** -0.5),
        'rw_g2': nrm((RW_GATE_LORA, D), RW_GATE_LORA ** -0.5),
        'rw_k_k': 0.85 + nrm((D,), 0.02),
        'rw_k_a': 1.0 + nrm((D,), 0.02),
        'rw_r_k': nrm((RW_HEADS, RW_HEAD), 0.1),
        'rw_ln_w': 1.0 + nrm((D,), 0.02),
        'rw_ln_b': nrm((D,), 0.02),
        'gla_w_q': nrm((D, GLA_KEY), D ** -0.5),
        'gla_w_k': nrm((D, GLA_KEY), D ** -0.5),
        'gla_w_v': nrm((D, GLA_VAL), D ** -0.5),
        'gla_w_gk1': nrm((D, GLA_GATE_RANK), D ** -0.5),
        'gla_w_gk2': nrm((GLA_GATE_RANK, GLA_KEY), GLA_GATE_RANK ** -0.5),
        'gla_b_gk': nrm((GLA_KEY,), 0.1),
        'gla_w_g': nrm((D, GLA_VAL), D ** -0.5),
        'gla_norm': 1.0 + nrm((GLA_DV,), 0.02),
        'gla_w_o': nrm((GLA_VAL, D), GLA_VAL ** -0.5),
        'hg_w_q': nrm((D, D), D ** -0.5),
        'hg_w_f': nrm((D, D), D ** -0.5),
        'hg_w_i': nrm((D, D), D ** -0.5),
        'hg_w_g': nrm((D, D), D ** -0.5),
        'hg_lb': nrm((DEPTH, D), 0.1),
        'hg_norm': 1.0 + nrm((HG_DV,), 0.02),
        'hg_w_o': nrm((D, D), D ** -0.5),
        'ffn_w_up': nrm((DEPTH, D, D_FF), D ** -0.5),
        'ffn_w_gate': nrm((DEPTH, D, D_FF), D ** -0.5),
        'ffn_w_conv': conv_base + nrm((DEPTH, CONV_W, D_FF), 0.2),
        'ffn_b_conv': nrm((DEPTH, D_FF), 0.02),
        'ffn_w_down': nrm((DEPTH, D_FF, D), D_FF ** -0.5),
    }


def reference(x_prompt, x_sample, state_s5_re, state_s5_im, state_rwkv_shift, state_rwkv_wkv, state_gla, state_hgrn,
              state_ffn_conv, norm_mix, norm_ffn, norm_final, s5_lambda_re, s5_lambda_im, s5_log_dt, s5_b_re, s5_b_im,
              s5_c_re, s5_c_im, s5_d, s5_w_glu, rw_mix, rw_w_r, rw_w_k, rw_w_v, rw_w_o, rw_w0, rw_w1, rw_w2, rw_a0,
              rw_a1, rw_a2, rw_g1, rw_g2, rw_k_k, rw_k_a, rw_r_k, rw_ln_w, rw_ln_b, gla_w_q, gla_w_k, gla_w_v,
              gla_w_gk1, gla_w_gk2, gla_b_gk, gla_w_g, gla_norm, gla_w_o, hg_w_q, hg_w_f, hg_w_i, hg_w_g, hg_lb,
              hg_norm, hg_w_o, ffn_w_up, ffn_w_gate, ffn_w_conv, ffn_b_conv, ffn_w_down):
    p = {
        'norm_mix': norm_mix, 'norm_ffn': norm_ffn, 'norm_final': norm_final,
        's5_lambda_re': s5_lambda_re, 's5_lambda_im': s5_lambda_im, 's5_log_dt': s5_log_dt,
        's5_b_re': s5_b_re, 's5_b_im': s5_b_im, 's5_c_re': s5_c_re, 's5_c_im': s5_c_im, 's5_d': s5_d,
        's5_w_glu': s5_w_glu,
        'rw_mix': rw_mix, 'rw_w_r': rw_w_r, 'rw_w_k': rw_w_k, 'rw_w_v': rw_w_v, 'rw_w_o': rw_w_o,
        'rw_w0': rw_w0, 'rw_w1': rw_w1, 'rw_w2': rw_w2, 'rw_a0': rw_a0, 'rw_a1': rw_a1, 'rw_a2': rw_a2,
        'rw_g1': rw_g1, 'rw_g2': rw_g2, 'rw_k_k': rw_k_k, 'rw_k_a': rw_k_a, 'rw_r_k': rw_r_k,
        'rw_ln_w': rw_ln_w, 'rw_ln_b': rw_ln_b,
        'gla_w_q': gla_w_q, 'gla_w_k': gla_w_k, 'gla_w_v': gla_w_v, 'gla_w_gk1': gla_w_gk1,
        'gla_w_gk2': gla_w_gk2, 'gla_b_gk': gla_b_gk, 'gla_w_g': gla_w_g, 'gla_norm': gla_norm,
        'gla_w_o': gla_w_o,
        'hg_w_q': hg_w_q, 'hg_w_f': hg_w_f, 'hg_w_i': hg_w_i, 'hg_w_g': hg_w_g, 'hg_lb': hg_lb,
        'hg_norm': hg_norm, 'hg_w_o': hg_w_o,
        'ffn_w_up': ffn_w_up, 'ffn_w_gate': ffn_w_gate, 'ffn_w_conv': ffn_w_conv, 'ffn_b_conv': ffn_b_conv,
        'ffn_w_down': ffn_w_down,
    }
    f32 = jnp.float32
    nb = x_prompt.shape[0]
    z_s5 = jnp.zeros((nb, S5_GROUPS, S5_STATE), f32)
    (y_prompt, s5_re_p, s5_im_p, rw_shift_p, rw_wkv_p, gla_p, hgrn_p, ffn_conv_p) = _trunk(
        x_prompt, z_s5, z_s5, jnp.zeros((nb, D_MODEL), x_prompt.dtype),
        jnp.zeros((nb, RW_HEADS, RW_HEAD, RW_HEAD), f32), jnp.zeros((nb, GLA_HEADS, GLA_DK, GLA_DV), f32),
        jnp.zeros((nb, HG_HEADS, HG_DK, HG_DV), f32), jnp.zeros((DEPTH, nb, CONV_W - 1, D_FF), x_prompt.dtype), p)
    (y_sample, s5_re_s, s5_im_s, rw_shift_s, rw_wkv_s, gla_s, hgrn_s, ffn_conv_s) = _trunk(
        x_sample, state_s5_re, state_s5_im, state_rwkv_shift, state_rwkv_wkv, state_gla, state_hgrn,
        state_ffn_conv, p)
    return (y_prompt, y_sample, s5_re_p, s5_im_p, rw_shift_p, rw_wkv_p, gla_p, hgrn_p, ffn_conv_p,
            s5_re_s, s5_im_s, rw_shift_s, rw_wkv_s, gla_s, hgrn_s, ffn_conv_s)
```

```python
import numpy as np
from contextlib import ExitStack
import concourse.bass as bass
import concourse.mybir as mybir
from concourse.bass_utils import run_bass_kernel_spmd

F32 = mybir.dt.float32
BF16 = mybir.dt.bfloat16
AF = mybir.ActivationFunctionType
ALU = mybir.AluOpType
AX = mybir.AxisListType

D = 2048
DC = 16
DFF = 5632
FC = 44
EPS = 1e-6


class R:
    __slots__ = ("w", "rd")

    def __init__(self):
        self.w = None
        self.rd = []


class Sched:
    COMPUTE = ("tensor", "vector", "scalar", "gpsimd")
    NDMA = 24

    def __init__(self, nc, es, dma_queues=("sync", "gpsimd")):
        self.nc = nc
        self.ops = {e: [] for e in ("tensor", "vector", "scalar", "gpsimd", "sync")}
        self.sems = {}
        self.cnt = {}
        for e in self.COMPUTE:
            self.sems[e] = es.enter_context(nc.semaphore("s_" + e))
            self.cnt[e] = 0
        self.dq = {}
        for q in dma_queues:
            lst = []
            for j in range(self.NDMA):
                k = "d_%s_%d" % (q, j)
                self.sems[k] = es.enter_context(nc.semaphore(k))
                self.cnt[k] = 0
                lst.append(k)
            self.dq[q] = [lst, 0]
        self.waited = {e: {} for e in self.ops}

    def _need(self, eng, ev, waits):
        if ev is None:
            return
        k, v = ev
        if k == eng and eng == "tensor":
            return
        if self.waited[eng].get(k, 0) >= v:
            return
        self.waited[eng][k] = v
        waits.append((k, v))

    def _deps(self, eng, reads, writes):
        waits = []
        for r in reads:
            self._need(eng, r.w, waits)
        for r in writes:
            self._need(eng, r.w, waits)
            for ev in r.rd:
                self._need(eng, ev, waits)
        return waits

    def _commit(self, ev, reads, writes):
        for r in reads:
            r.rd.append(ev)
            if len(r.rd) > 64:
                best = {}
                for k, v in r.rd:
                    if best.get(k, 0) < v:
                        best[k] = v
                r.rd = list(best.items())
        for r in writes:
            r.w = ev
            r.rd = []

    def op(self, eng, fn, reads=(), writes=()):
        waits = self._deps(eng, reads, writes)
        self.cnt[eng] += 1
        ev = (eng, self.cnt[eng])
        self.ops[eng].append((fn, waits, (eng, 1)))
        self._commit(ev, reads, writes)

    def dma(self, q, out, in_, reads=(), writes=()):
        lst, i = self.dq[q]
        k = lst[i % self.NDMA]
        self.dq[q][1] = i + 1
        waits = self._deps(q, reads, writes)
        if self.cnt[k] > 0:
            self._need(q, (k, self.cnt[k]), waits)
        self.cnt[k] += 16
        ev = (k, self.cnt[k])
        self.ops[q].append((lambda e: e.dma_start(out=out, in_=in_), waits, (k, 16)))
        self._commit(ev, reads, writes)

    def finish(self):
        for q in self.dq:
            for k in self.dq[q][0]:
                if self.cnt[k]:
                    self.ops["sync"].append((None, [(k, self.cnt[k])], None))

    def emit(self):
        nc = self.nc
        with nc.Block() as block:
            def mk(name):
                def body(eng):
                    for fn, waits, inc in self.ops[name]:
                        for k, v in waits:
                            eng.wait_ge(self.sems[k], v)
                        if fn is not None:
                            ins = fn(eng)
                            ins.then_inc(self.sems[inc[0]], inc[1])
                return body
            block.sync(mk("sync"))
            block.tensor(mk("tensor"))
            block.vector(mk("vector"))
            block.scalar(mk("scalar"))
            block.gpsimd(mk("gpsimd"))


class Buf:
    def __init__(self, t):
        self.t = t
        self.r = R()


class View:
    def __init__(self, ap):
        self.t = ap
        self.r = R()


class Ctx:
    def __init__(self, nc, es):
        self.nc = nc
        self.es = es
        self.S = Sched(nc, es)
        self.n = 0
        self.din = {}
        self.dout = {}

    def sb(self, shape, dt=F32):
        self.n += 1
        return Buf(self.es.enter_context(self.nc.sbuf_tensor("sb%d" % self.n, list(shape), dt)))

    def ps(self, shape=(128, 512), dt=F32):
        self.n += 1
        return Buf(self.es.enter_context(self.nc.psum_tensor("ps%d" % self.n, list(shape), dt)))

    def inp(self, name, shape):
        t = self.nc.dram_tensor(name, list(shape), F32, kind="ExternalInput").ap()
        self.din[name] = t
        return t

    def outp(self, name, shape):
        t = self.nc.dram_tensor(name, list(shape), F32, kind="ExternalOutput").ap()
        self.dout[name] = t
        return t

    def scratch(self, name, shape, dt=F32):
        return self.nc.dram_tensor(name, list(shape), dt, kind="Internal").ap()


def rs(bufs):
    return [b.r for b in bufs]


class Tile:
    def __init__(self, c0, n, kind, idx):
        self.c0, self.n, self.kind, self.idx = c0, n, kind, idx


class Net:
    def __init__(self, cx, cfg):
        self.cx = cx
        self.S = cx.S
        self.cfg = cfg
        TP, NS = cfg["TP"], cfg["NS"]
        self.TP, self.NS = TP, NS
        self.NT = TP + NS
        self.tiles = [Tile(i * 512, 512, "p", i) for i in range(TP // 512)] + [Tile(TP, NS, "s", TP // 512)]
        self.NPT = TP // 512
        c = cx
        self.hs = c.sb([128, DC, 512])
        self.xn = c.sb([128, DC, 512], BF16)
        self.NW = 3
        self.wring = [c.sb([128, 4096], BF16) for _ in range(self.NW)]
        self.wi = 0
        self.wcache = {}
        self.wcn = 0
        self.psr = [c.ps() for _ in range(4)]
        self.ring = self.psr
        self.pi = 0
        self.zeros = c.sb([128, 16])
        self.S.op("vector", lambda e: e.memset(self.zeros.t[:], 0.0), writes=[self.zeros.r])
        self.sq = [c.sb([128, 512]) for _ in range(2)]
        self.rstd = c.sb([128, 512])
        self.ones = c.sb([128, 128])
        self.ident = c.sb([128, 128])
        self.pv_cols = cfg["pv_cols"]
        self.pv = c.sb([128, cfg["pv_n"]])
        pv_d = c.inp("pvec", [128, cfg["pv_n"]])
        ident_d = c.inp("ident", [128, 128])
        self.S.op("vector", lambda e: e.memset(self.ones.t[:], 1.0), writes=[self.ones.r])
        self.S.dma("sync", self.pv.t[:], pv_d, writes=[self.pv.r])
        self.S.dma("sync", self.ident.t[:], ident_d, writes=[self.ident.r])
        self.act = c.sb([128, FC, 512], BF16)
        self.la_f = [c.sb([128, 2, 512]) for _ in range(5)]
        self.hT = c.scratch("hT", [D, self.NT])
        self.xT = c.inp("xT", [D, self.NT])
        self.hr = [R() for _ in self.tiles]

    def pcol(self, name, j=0, n=1):
        o = self.pv_cols[name] + j
        return self.pv.t[:, o:o + n]

    def psum(self):
        b = self.ring[self.pi % len(self.ring)]
        self.pi += 1
        return b

    def pbf(self, pb):
        return pb.t[:].bitcast(BF16)

    def zero_ap(self, N):
        return self.zeros.t[:, 0:N]

    def wslot(self):
        b = self.wring[self.wi % self.NW]
        self.wi += 1
        return b

    def proj(self, w, K, M, xin, N, consume, mw=256, wname=None):
        S = self.S
        cache = None
        if wname is not None:
            if wname not in self.wcache:
                self.wcache[wname] = {"first": True, "blocks": []}
            cache = self.wcache[wname]
        bi = 0
        kp = min(K, 128)
        kc = K // kp
        mw = min(mw, M)
        kg = max(1, min(kc, 4096 // mw))
        wv = w.rearrange("(c p) m -> p c m", p=kp)
        for m0 in range(0, M, mw):
            mcs = [(m0 + j, min(128, M - m0 - j)) for j in range(0, min(mw, M - m0), 128)]
            pbs = [self.psum() for _ in mcs]
            cw = min(mw, M - m0)
            for k0 in range(0, kc, kg):
                g = min(kg, kc - k0)
                slot = self.wslot()
                sv = slot.t[0:kp, 0:g * cw].rearrange("p (c m) -> p c m", c=g)
                if cache is None:
                    S.dma("gpsimd", sv, wv[:, k0:k0 + g, m0:m0 + cw], writes=[slot.r])
                elif cache["first"]:
                    self.wcn += 1
                    scr = self.cx.scratch("wc%d" % self.wcn, [128, 4096], BF16)
                    br = R()
                    cache["blocks"].append((scr, br))
                    S.dma("gpsimd", sv, wv[:, k0:k0 + g, m0:m0 + cw], writes=[slot.r])
                    S.dma("sync", scr[0:kp, 0:g * cw], slot.t[0:kp, 0:g * cw], reads=[slot.r], writes=[br])
                else:
                    scr, br = cache["blocks"][bi]
                    S.dma("sync", slot.t[0:kp, 0:g * cw], scr[0:kp, 0:g * cw], reads=[br], writes=[slot.r])
                bi += 1
                for (ms, msz), pb in zip(mcs, pbs):
                    for kk in range(g):
                        k = k0 + kk
                        xa, xr = xin(k)
                        S.op("tensor", lambda e, pb=pb, sv=sv, kk=kk, ms=ms, msz=msz, xa=xa, k=k, m0=m0:
                             e.matmul(pb.t[0:msz, 0:N], sv[:, kk, ms - m0:ms - m0 + msz], xa, start=(k == 0), stop=(k == kc - 1)),
                             reads=[slot.r] + xr, writes=[pb.r])
            for (ms, msz), pb in zip(mcs, pbs):
                consume(ms // 128, msz, pb)
        if cache is not None:
            cache["first"] = False

    def load_h(self, tl, first):
        src = self.xT if first else self.hT
        self.S.dma("sync", self.hs.t[:, :, 0:tl.n], src[:, tl.c0:tl.c0 + tl.n].rearrange("(c p) n -> p c n", p=128),
                   reads=[self.hr[tl.idx]], writes=[self.hs.r])

    def store_h(self, tl):
        self.S.dma("sync", self.hT[:, tl.c0:tl.c0 + tl.n].rearrange("(c p) n -> p c n", p=128), self.hs.t[:, :, 0:tl.n],
                   reads=[self.hs.r], writes=[self.hr[tl.idx]])

    def rms(self, src, N, gname, gj, out_bf=None, out_f32=None):
        S = self.S
        pb = self.psum()
        for c in range(DC):
            q = self.sq[c % 2]
            S.op("scalar", lambda e, q=q, c=c: e.activation(out=q.t[:, 0:N], in_=src.t[:, c, 0:N], func=AF.Square),
                 reads=[src.r], writes=[q.r])
            S.op("tensor", lambda e, q=q, c=c: e.matmul(pb.t[:, 0:N], self.ones.t[:], q.t[:, 0:N], start=(c == 0), stop=(c == DC - 1)),
                 reads=[q.r, self.ones.r], writes=[pb.r])
        S.op("scalar", lambda e: e.activation(out=self.rstd.t[:, 0:N], in_=pb.t[:, 0:N], func=AF.Sqrt, bias=self.eps_ap(), scale=1.0 / D),
             reads=[pb.r, self.pv.r], writes=[self.rstd.r])
        S.op("vector", lambda e: e.reciprocal(out=self.rstd.t[:, 0:N], in_=self.rstd.t[:, 0:N]), writes=[self.rstd.r])
        for c in range(DC):
            for o in (out_bf, out_f32):
                if o is None:
                    continue
                S.op("vector", lambda e, c=c, o=o: e.scalar_tensor_tensor(
                    out=o.t[:, c, 0:N], in0=src.t[:, c, 0:N], scalar=self.pcol(gname, gj * DC + c), in1=self.rstd.t[:, 0:N],
                    op0=ALU.mult, op1=ALU.mult), reads=[src.r, self.rstd.r, self.pv.r], writes=[o.r])

    def eps_ap(self):
        return self.pcol("eps")

    def ffn_setup(self):
        c = self.cx
        flat = lambda b: View(b.t[:].rearrange("p k n -> p (k n)"))
        self.ubuf = [flat(self.la_f[0]), flat(self.la_f[1])]
        self.cbuf = [flat(self.la_f[2]), flat(self.la_f[3])]
        self.gbuf = [flat(self.la_f[4]), c.sb([128, 512])]
        self.halo = c.sb([128, FC, 2])
        self.cprev = c.sb([128, FC, self.NS, 2])
        L = self.cfg["DEPTH"]
        self.w_up = c.inp("ffn_w_up", [L, D, DFF])
        self.w_gate = c.inp("ffn_w_gate", [L, D, DFF])
        self.w_down = c.inp("ffn_w_down", [L, DFF, D])
        self.conv_in = c.inp("conv_s", [L, 128, FC * self.NS * 2])
        self.conv_p = c.outp("conv_p_o", [L, 128, FC * 2])
        self.conv_so = c.outp("conv_s_o", [L, 128, FC * self.NS * 2])
        self.ui = 0

    def ffn(self, li, first=False):
        S = self.S
        S.op("vector", lambda e: e.memset(self.halo.t[:], 0.0), writes=[self.halo.r])
        S.dma("sync", self.cprev.t[:].rearrange("p f s t -> p (f s t)"), self.conv_in[li], writes=[self.cprev.r])
        self.ring = self.psr + self.obank
        for tl in self.tiles:
            N = tl.n
            self.load_h(tl, first)
            self.rms(self.hs, N, "norm_ffn", li, out_bf=self.xn)
            xin = lambda k: (self.xn.t[:, k, 0:N], [self.xn.r])
            for f0 in range(0, FC, 2):
                ups = {}

                def cons_up(mc, msz, pb, ups=ups):
                    ups[mc] = pb
                self.proj(self.w_up[li][:, f0 * 128:(f0 + 2) * 128], D, 256, xin, N, cons_up, wname="up%d_%d" % (li, f0))

                def cons_gate(mc, msz, pb, ups=ups, f0=f0, tl=tl, N=N):
                    f = f0 + mc
                    pu = ups[mc]
                    ub = self.ubuf[self.ui % 2]
                    cb = self.cbuf[self.ui % 2]
                    gb = self.gbuf[self.ui % 2]
                    self.ui += 1
                    wc = lambda j: self.pcol("ffn_w_conv", (li * 3 + j) * FC + f)
                    bc = self.pcol("ffn_b_conv", li * FC + f)
                    if tl.kind == "p":
                        S.op("scalar", lambda e: e.copy(out=ub.t[:, 0:2], in_=self.halo.t[:, f, :]), reads=[self.halo.r], writes=[ub.r])
                        S.op("scalar", lambda e: e.copy(out=ub.t[:, 2:2 + N], in_=pu.t[:, 0:N]), reads=[pu.r], writes=[ub.r])
                        S.op("vector", lambda e: e.tensor_scalar(out=cb.t[:, 0:N], in0=ub.t[:, 2:2 + N], scalar1=wc(2), scalar2=bc, op0=ALU.mult, op1=ALU.add),
                             reads=[ub.r, self.pv.r], writes=[cb.r])
                        for j in (1, 0):
                            S.op("vector", lambda e, j=j: e.scalar_tensor_tensor(out=cb.t[:, 0:N], in0=ub.t[:, j:j + N], scalar=wc(j), in1=cb.t[:, 0:N], op0=ALU.mult, op1=ALU.add),
                                 reads=[ub.r, self.pv.r], writes=[cb.r])
                        S.op("scalar", lambda e: e.copy(out=self.halo.t[:, f, :], in_=ub.t[:, N:N + 2]), reads=[ub.r], writes=[self.halo.r])
                    else:
                        S.op("scalar", lambda e: e.copy(out=ub.t[:, 0:N], in_=pu.t[:, 0:N]), reads=[pu.r], writes=[ub.r])
                        S.op("vector", lambda e: e.tensor_scalar(out=cb.t[:, 0:N], in0=ub.t[:, 0:N], scalar1=wc(2), scalar2=bc, op0=ALU.mult, op1=ALU.add),
                             reads=[ub.r, self.pv.r], writes=[cb.r])
                        for j in (1, 0):
                            S.op("vector", lambda e, j=j: e.scalar_tensor_tensor(out=cb.t[:, 0:N], in0=self.cprev.t[:, f, :, j], scalar=wc(j), in1=cb.t[:, 0:N], op0=ALU.mult, op1=ALU.add),
                                 reads=[self.cprev.r, self.pv.r], writes=[cb.r])
                        S.op("scalar", lambda e: e.copy(out=self.cprev.t[:, f, :, 0], in_=self.cprev.t[:, f, :, 1]), reads=[cb.r], writes=[self.cprev.r])
                        S.op("scalar", lambda e: e.copy(out=self.cprev.t[:, f, :, 1], in_=ub.t[:, 0:N]), reads=[ub.r], writes=[self.cprev.r])
                    S.op("scalar", lambda e: e.activation(out=gb.t[:, 0:N], in_=cb.t[:, 0:N], func=AF.Gelu_apprx_tanh), reads=[cb.r], writes=[gb.r])
                    S.op("vector", lambda e: e.tensor_tensor(out=self.act.t[:, f, 0:N], in0=gb.t[:, 0:N], in1=pb.t[:, 0:N], op=ALU.mult),
                         reads=[gb.r, pb.r], writes=[self.act.r])
                self.proj(self.w_gate[li][:, f0 * 128:(f0 + 2) * 128], D, 256, xin, N, cons_gate, wname="gate%d_%d" % (li, f0))
            if tl.kind == "p" and tl.idx == self.NPT - 1:
                S.dma("sync", self.conv_p[li], self.halo.t[:].rearrange("p f t -> p (f t)"), reads=[self.halo.r])
            if tl.kind == "s":
                S.dma("sync", self.conv_so[li], self.cprev.t[:].rearrange("p f s t -> p (f s t)"), reads=[self.cprev.r])
            ain = lambda k: (self.act.t[:, k, 0:N], [self.act.r])

            def cons_down(mc, msz, pb, N=N):
                S.op("vector", lambda e: e.tensor_tensor(out=self.hs.t[:, mc, 0:N], in0=self.hs.t[:, mc, 0:N], in1=pb.t[:, 0:N], op=ALU.add),
                     reads=[pb.r], writes=[self.hs.r])
            self.proj(self.w_down[li], DFF, D, ain, N, cons_down, wname="down%d" % li)
            self.store_h(tl)
        self.ring = self.psr

    def final(self):
        yT = self.cx.outp("yT", [D, self.NT])
        for tl in self.tiles:
            N = tl.n
            self.load_h(tl, False)
            self.rms(self.hs, N, "norm_final", 0, out_f32=self.hs)
            self.S.dma("sync", yT[:, tl.c0:tl.c0 + N].rearrange("(c p) n -> p c n", p=128), self.hs.t[:, :, 0:N], reads=[self.hs.r])


def fm(a):
    a = np.asarray(a, np.float32)
    if a.ndim == 1:
        a = a[None]
    r, f = a.shape
    return np.ascontiguousarray(a.reshape(r, f // 128, 128).transpose(2, 0, 1).reshape(128, -1))


PV_SPEC = [("eps", None), ("one", None), ("neghalf", None), ("lneps", None), ("halfpi", None), ("s5_d", "s5_d"), ("rw_mix", "rw_mix"), ("rw_w0", "rw_w0"), ("rw_a0", "rw_a0"),
           ("rw_k_k", "rw_k_k"), ("rw_k_a", "rw_k_a"), ("rw_r_k", "rw_r_k"), ("rw_ln_w", "rw_ln_w"), ("rw_ln_b", "rw_ln_b"), ("gla_b_gk", "gla_b_gk"), ("gla_norm", "gla_norm"), ("hg_norm", "hg_norm"), ("hg_lb", "hg_lb"),
           ("norm_mix", "norm_mix"), ("norm_ffn", "norm_ffn"), ("norm_final", "norm_final"),
           ("ffn_w_conv", "ffn_w_conv"), ("ffn_b_conv", "ffn_b_conv")]


def s5_lay(a):
    return np.ascontiguousarray(np.asarray(a, np.float32).reshape(2, 64, 64).transpose(0, 2, 1).reshape(128, 64))


def s5_inputs(inp):
    par = np.zeros((128, 320), np.float32)
    par[:, 0:64] = s5_lay(inp["s5_lambda_re"])
    par[:, 64:128] = s5_lay(inp["s5_lambda_im"])
    par[:, 128:192] = s5_lay(np.broadcast_to(np.asarray(inp["s5_log_dt"], np.float32)[:, None], (128, 64)))
    lb = lambda b: np.asarray(b, np.float32).reshape(2, 64, 64, 16).transpose(0, 2, 1, 3).reshape(128, 1024)
    lc = lambda c_: np.asarray(c_, np.float32).reshape(2, 64, 16, 64).transpose(0, 3, 1, 2).reshape(128, 1024)
    msk = np.zeros((128, 136), np.float32)
    for g8 in range(8):
        msk[g8 * 16:(g8 + 1) * 16, g8 * 16:(g8 + 1) * 16] = 1.0
        msk[g8 * 16:(g8 + 1) * 16, 128 + g8] = 1.0
    return {"s5_par": par, "s5_B": np.ascontiguousarray(np.stack([lb(inp["s5_b_re"]), lb(inp["s5_b_im"])])),
            "s5_C": np.ascontiguousarray(np.stack([lc(inp["s5_c_re"]), lc(inp["s5_c_im"])])), "s5_msk": msk,
            "s5_w_glu": np.ascontiguousarray(inp["s5_w_glu"], dtype=np.float32)}


def s5_state_in(re, im, ns):
    f = lambda a: np.asarray(a, np.float32).reshape(ns, 2, 64, 64).transpose(1, 3, 2, 0).reshape(128, 64 * ns)
    return np.ascontiguousarray(np.stack([f(re), f(im)]))


def s5_state_out_p(o):
    f = lambda a: a.reshape(2, 64, 64).transpose(0, 2, 1).reshape(128, 64)
    return f(o[0]), f(o[1])


def s5_state_out_s(o, ns):
    f = lambda a: a.reshape(2, 64, 64, ns).transpose(3, 0, 2, 1).reshape(ns, 128, 64)
    return f(o[0]), f(o[1])


def rw_consts():
    su = np.triu(np.ones((64, 64), np.float32), 1)
    ui = np.triu(np.ones((64, 64), np.float32), 0)
    sl_ = np.tril(np.ones((64, 64), np.float32), -1)
    c = np.zeros((128, 1216), np.float32)
    c[0:64, 0:64] = 1.0
    c[64:128, 64:128] = 1.0
    c[:, 128:192] = np.concatenate([np.eye(64), np.eye(64)], 0)
    c[0:64, 192:704] = np.tile(np.concatenate([su, ui], 1), (1, 4))
    c[0:64, 704:1216] = np.tile(np.concatenate([sl_, sl_], 1), (1, 4))
    return c


PV_NCOLS = {"s5_d": 16, "rw_mix": 96, "rw_w0": 16, "rw_a0": 16, "rw_k_k": 16, "rw_k_a": 16, "rw_r_k": 16, "rw_ln_w": 16, "rw_ln_b": 16,
            "gla_b_gk": 8, "gla_norm": 4, "hg_norm": 1, "hg_lb": 64, "norm_mix": 64, "norm_ffn": 64, "norm_final": 16,
            "ffn_w_conv": 528, "ffn_b_conv": 176}


def pack_pvec(inp):
    cols = {}
    parts = []
    o = 0
    for name, key in PV_SPEC:
        if name == "eps":
            a = np.full((128, 1), EPS, np.float32)
        elif name == "one":
            a = np.full((128, 1), 1.0, np.float32)
        elif name == "neghalf":
            a = np.full((128, 1), -0.5, np.float32)
        elif name == "lneps":
            a = np.full((128, 1), 64e-5, np.float32)
        elif name == "halfpi":
            a = np.full((128, 1), np.pi / 2, np.float32)
        elif name == "rw_r_k" and key in inp:
            a = fm(np.asarray(inp[key], np.float32).reshape(1, -1))
        elif key not in inp:
            a = np.zeros((128, PV_NCOLS[name]), np.float32)
        else:
            v = np.asarray(inp[key], np.float32)
            a = fm(v.reshape(-1, v.shape[-1]))
        cols[name] = o
        o += a.shape[1]
        parts.append(a)
    return cols, np.ascontiguousarray(np.concatenate(parts, axis=1))


def la_setup(self):
    c = self.cx
    self.la_qe = View(self.act.t[:, 36:38, :])
    self.la_ke = View(self.act.t[:, 38:40, :])
    self.la_vh = View(self.act.t[:, 32:36, :])
    self.la_VT = View(self.act.t[0:64, 16:32, :])
    self.la_KET = [c.sb([64, 2, 128], BF16) for _ in range(2)]
    self.la_att = [c.sb([64, 64], BF16) for _ in range(2)]
    self.la_S = c.sb([128, 4096])
    self.la_Sbf = c.sb([128, 4096], BF16)
    self.la_Ss = c.sb([128, 1024])
    self.la_Ssbf = c.sb([128, 1024], BF16)
    self.la_t = [c.sb([128, 512]) for _ in range(3)]
    self.la_og = View(self.act.t[:, 0:DC, :])
    self.la_t1 = c.sb([16, 512], BF16)
    self.la_lb = c.sb([128, 2 * DC])
    self.la_e = c.sb([128, 5 * DC])
    self.identb = c.sb([128, 128], BF16)
    self.masks = c.sb([64, 64])
    self.rmask = c.sb([128, 512])
    md = c.inp("maskT", [64, 64])
    rd = c.inp("rmask", [128, 512])
    S = self.S
    S.dma("sync", self.masks.t[:, 0:64], md, writes=[self.masks.r])
    S.dma("sync", self.rmask.t[:], rd, writes=[self.rmask.r])
    S.op("vector", lambda e: e.tensor_copy(out=self.identb.t[:], in_=self.ident.t[:]), reads=[self.ident.r], writes=[self.identb.r])
    self.obank = [c.ps() for _ in range(4)]


def la_layer(self, kind, first=False):
    S = self.S
    c = self.cx
    if kind == "gla":
        H, kcn, vcn, li = 4, 2, 4, 2
        wq, wk, wv = c.inp("gla_w_q", [D, 1024]), c.inp("gla_w_k", [D, 1024]), c.inp("gla_w_v", [D, D])
        wg1, wg2 = c.inp("gla_w_gk1", [D, 16]), c.inp("gla_w_gk2", [16, 1024])
        wg, wo = c.inp("gla_w_g", [D, D]), c.inp("gla_w_o", [D, D])
        st_in = c.inp("gla_s", [self.NS, H, 128, kcn * 512])
        st_p = c.outp("gla_p_o", [H, 128, kcn * 512])
        st_s = c.outp("gla_s_o", [self.NS, H, 128, kcn * 512])
        nname = "gla_norm"
    else:
        H, kcn, vcn, li = 16, 1, 1, 3
        wq, wk, wv = c.inp("hg_w_q", [D, D]), c.inp("hg_w_f", [D, D]), c.inp("hg_w_i", [D, D])
        wg, wo = c.inp("hg_w_g", [D, D]), c.inp("hg_w_o", [D, D])
        st_in = c.inp("hg_s", [self.NS, H, 128, 128])
        st_p = c.outp("hg_p_o", [H, 128, 128])
        st_s = c.outp("hg_s_o", [self.NS, H, 128, 128])
        nname = "hg_norm"
        E = self.la_e
        for j in range(4):
            S.op("scalar", lambda e, j=j: e.activation(out=E.t[:, j * DC:(j + 1) * DC], in_=self.pcol("hg_lb", j * DC, DC), func=AF.Exp),
                 reads=[self.pv.r], writes=[E.r])
        S.op("vector", lambda e: e.tensor_tensor(out=E.t[:, 4 * DC:5 * DC], in0=E.t[:, 0:DC], in1=E.t[:, DC:2 * DC], op=ALU.add), writes=[E.r])
        for j in (2, 3):
            S.op("vector", lambda e, j=j: e.tensor_tensor(out=E.t[:, 4 * DC:5 * DC], in0=E.t[:, 4 * DC:5 * DC], in1=E.t[:, j * DC:(j + 1) * DC], op=ALU.add), writes=[E.r])
        S.op("vector", lambda e: e.reciprocal(out=E.t[:, 4 * DC:5 * DC], in_=E.t[:, 4 * DC:5 * DC]), writes=[E.r])
        S.op("vector", lambda e: e.tensor_tensor(out=self.la_lb.t[:, DC:2 * DC], in0=E.t[:, 0:DC], in1=E.t[:, 4 * DC:5 * DC], op=ALU.mult), reads=[E.r], writes=[self.la_lb.r])
        S.op("vector", lambda e: e.tensor_scalar(out=self.la_lb.t[:, 0:DC], in0=self.la_lb.t[:, DC:2 * DC], scalar1=-1.0, scalar2=1.0, op0=ALU.mult, op1=ALU.add), writes=[self.la_lb.r])
    dk, dv = kcn * 128, vcn * 128
    qf, kf, lg, cum, eg = self.la_f
    qe, ke, vh, VT = self.la_qe, self.la_ke, self.la_vh, self.la_VT
    t0, t1b, t2 = self.la_t
    og = self.la_og
    Sst, Sbf = self.la_S, self.la_Sbf
    S.op("vector", lambda e: e.memset(Sst.t[:], 0.0), writes=[Sst.r])
    S.op("vector", lambda e: e.memset(Sbf.t[:], 0.0), writes=[Sbf.r])
    self.ring = self.psr[0:4]
    def tile_body(tl):
        N = tl.n
        C = 64 if tl.kind == "p" else 1
        nch = N // C
        self.load_h(tl, first)
        self.rms(self.hs, N, "norm_mix", li, out_bf=self.xn)
        xin = lambda k: (self.xn.t[:, k, 0:N], [self.xn.r])
        if kind == "gla":
            def cons_t1(mc, msz, pb):
                S.op("scalar", lambda e: e.copy(out=self.la_t1.t[:, 0:N], in_=pb.t[0:16, 0:N]), reads=[pb.r], writes=[self.la_t1.r])
            self.proj(wg1, D, 16, xin, N, cons_t1)
        def head_body(h):
            def cons_q(mc, msz, pb):
                S.op("scalar", lambda e: e.mul(out=qf.t[:, mc, 0:N], in_=pb.t[:, 0:N], mul=float(dk) ** -0.5), reads=[pb.r], writes=[qf.r])
            self.proj(wq[:, h * dk:(h + 1) * dk], D, dk, xin, N, cons_q, mw=min(256, dk), wname="%s_q%d" % (kind, h))
            if kind == "gla":
                def cons_k(mc, msz, pb):
                    S.op("scalar", lambda e: e.copy(out=kf.t[:, mc, 0:N], in_=pb.t[:, 0:N]), reads=[pb.r], writes=[kf.r])
                self.proj(wk[:, h * dk:(h + 1) * dk], D, dk, xin, N, cons_k, wname="%s_k%d" % (kind, h))
                t1in = lambda k: (self.la_t1.t[:, 0:N], [self.la_t1.r])

                def cons_gk(mc, msz, pb):
                    bcol = self.pcol("gla_b_gk", h * kcn + mc)
                    S.op("vector", lambda e: e.tensor_scalar(out=t0.t[:, 0:N], in0=pb.t[:, 0:N], scalar1=bcol, scalar2=-1.0, op0=ALU.add, op1=ALU.mult),
                         reads=[pb.r, self.pv.r], writes=[t0.r])
                    S.op("scalar", lambda e: e.activation(out=t0.t[:, 0:N], in_=t0.t[:, 0:N], func=AF.Exp), writes=[t0.r])
                    S.op("scalar", lambda e: e.activation(out=t0.t[:, 0:N], in_=t0.t[:, 0:N], func=AF.Ln, bias=self.pcol("one"), scale=1.0), reads=[self.pv.r], writes=[t0.r])
                    S.op("vector", lambda e: e.tensor_scalar(out=lg.t[:, mc, 0:N], in0=t0.t[:, 0:N], scalar1=-1.0 / 16.0, scalar2=None, op0=ALU.mult), reads=[t0.r], writes=[lg.r])
                self.proj(wg2[:, h * dk:(h + 1) * dk], 16, dk, t1in, N, cons_gk)
            else:
                def cons_f(mc, msz, pb):
                    lbc = self.la_lb.t[:, h:h + 1]
                    omc = self.la_lb.t[:, DC + h:DC + h + 1]
                    S.op("scalar", lambda e: e.activation(out=t0.t[:, 0:N], in_=pb.t[:, 0:N], func=AF.Sigmoid), reads=[pb.r], writes=[t0.r])
                    S.op("vector", lambda e: e.tensor_scalar(out=t1b.t[:, 0:N], in0=t0.t[:, 0:N], scalar1=omc, scalar2=lbc, op0=ALU.mult, op1=ALU.add),
                         reads=[t0.r, self.la_lb.r], writes=[t1b.r])
                    S.op("scalar", lambda e: e.activation(out=lg.t[:, 0, 0:N], in_=t1b.t[:, 0:N], func=AF.Ln), reads=[t1b.r], writes=[lg.r])
                    S.op("vector", lambda e: e.tensor_scalar(out=t1b.t[:, 0:N], in0=t0.t[:, 0:N], scalar1=-1.0, scalar2=1.0, op0=ALU.mult, op1=ALU.add),
                         reads=[t0.r], writes=[t1b.r])
                    S.op("vector", lambda e: e.tensor_scalar(out=kf.t[:, 0, 0:N], in0=t1b.t[:, 0:N], scalar1=omc, scalar2=None, op0=ALU.mult),
                         reads=[self.la_lb.r, t1b.r], writes=[kf.r])
                self.proj(wk[:, h * dk:(h + 1) * dk], D, dk, xin, N, cons_f, mw=128, wname="%s_k%d" % (kind, h))

            def cons_v(mc, msz, pb):
                S.op("scalar", lambda e: e.copy(out=vh.t[:, mc, 0:N], in_=pb.t[:, 0:N]), reads=[pb.r], writes=[vh.r])
            self.proj(wv[:, h * dv:(h + 1) * dv], D, dv, xin, N, cons_v, mw=min(256, dv), wname="%s_v%d" % (kind, h))
            for kc in range(kcn):
                S.op("vector", lambda e, kc=kc: e.tensor_tensor_scan(out=cum.t[:, kc, 0:N], data0=self.rmask.t[:, 0:N] if C > 1 else self.zero_ap(N), data1=lg.t[:, kc, 0:N],
                                                                     initial=0.0, op0=ALU.mult, op1=ALU.add), reads=[lg.r, self.rmask.r, self.zeros.r], writes=[cum.r])
                S.op("scalar", lambda e, kc=kc: e.activation(out=eg.t[:, kc, 0:N], in_=cum.t[:, kc, 0:N], func=AF.Exp), reads=[cum.r], writes=[eg.r])
                S.op("vector", lambda e, kc=kc: e.tensor_tensor(out=qe.t[:, kc, 0:N], in0=qf.t[:, kc, 0:N], in1=eg.t[:, kc, 0:N], op=ALU.mult), reads=[qf.r, eg.r], writes=[qe.r])
                S.op("scalar", lambda e, kc=kc: e.activation(out=cum.t[:, kc, 0:N], in_=cum.t[:, kc, 0:N], func=AF.Exp, scale=-1.0), writes=[cum.r])
                S.op("vector", lambda e, kc=kc: e.tensor_tensor(out=ke.t[:, kc, 0:N], in0=kf.t[:, kc, 0:N], in1=cum.t[:, kc, 0:N], op=ALU.mult), reads=[kf.r, cum.r], writes=[ke.r])
            for ch in range(nch):
                for vc in range(vcn):
                    pb = self.psum()
                    S.op("tensor", lambda e, pb=pb, ch=ch, vc=vc: e.transpose(out=self.pbf(pb)[0:C, 0:128], in_=vh.t[:, vc, ch * C:(ch + 1) * C], identity=self.identb.t[:]),
                         reads=[vh.r, self.identb.r], writes=[pb.r])
                    S.op("scalar", lambda e, pb=pb, ch=ch, vc=vc: e.copy(out=VT.t[0:C, ch, vc * 128:(vc + 1) * 128], in_=self.pbf(pb)[0:C, 0:128]), reads=[pb.r], writes=[VT.r])
            obs = self.obank[0:vcn]
            for ch in range(nch):
                cs = slice(ch * C, (ch + 1) * C)
                if tl.kind == "s":
                    Sc, Scb, so = self.la_Ss, self.la_Ssbf, 0
                    S.dma("sync", Sc.t[:, 0:kcn * dv], st_in[ch, h], writes=[Sc.r])
                    S.op("scalar", lambda e, Sc=Sc, Scb=Scb: e.copy(out=Scb.t[:, 0:kcn * dv], in_=Sc.t[:, 0:kcn * dv]), reads=[Sc.r], writes=[Scb.r])
                else:
                    Sc, Scb, so = Sst, Sbf, h * kcn * dv
                pa = self.psum()
                for kc in range(kcn):
                    S.op("tensor", lambda e, pa=pa, kc=kc, cs=cs: e.matmul(pa.t[0:C, 0:C], ke.t[:, kc, cs], qe.t[:, kc, cs], start=(kc == 0), stop=(kc == kcn - 1)),
                         reads=[ke.r, qe.r], writes=[pa.r])
                att = self.la_att[ch % 2]
                S.op("vector", lambda e, pa=pa, att=att: e.tensor_tensor(out=att.t[0:C, 0:C], in0=pa.t[0:C, 0:C], in1=self.masks.t[0:C, 0:C], op=ALU.mult),
                     reads=[pa.r, self.masks.r], writes=[att.r])
                KET = self.la_KET[ch % 2]
                for kc in range(kcn):
                    pb = self.psum()
                    S.op("tensor", lambda e, pb=pb, kc=kc, cs=cs: e.transpose(out=self.pbf(pb)[0:C, 0:128], in_=ke.t[:, kc, cs], identity=self.identb.t[:]),
                         reads=[ke.r, self.identb.r], writes=[pb.r])
                    S.op("scalar", lambda e, pb=pb, kc=kc, KET=KET: e.copy(out=KET.t[0:C, kc, :], in_=self.pbf(pb)[0:C, 0:128]), reads=[pb.r], writes=[KET.r])
                for vc in range(vcn):
                    ob = obs[vc]
                    S.op("tensor", lambda e, ob=ob, vc=vc, ch=ch, cs=cs, att=att: e.matmul(ob.t[:, cs], VT.t[0:C, ch, vc * 128:(vc + 1) * 128], att.t[0:C, 0:C], start=True, stop=False),
                         reads=[VT.r, att.r], writes=[ob.r])
                    for kc in range(kcn):
                        S.op("tensor", lambda e, ob=ob, vc=vc, kc=kc, cs=cs, Scb=Scb, so=so: e.matmul(ob.t[:, cs], Scb.t[:, so + kc * dv + vc * 128: so + kc * dv + (vc + 1) * 128], qe.t[:, kc, cs],
                                                                                                  start=False, stop=(kc == kcn - 1)),
                             reads=[Scb.r, qe.r], writes=[ob.r])
                for kc in range(kcn):
                    pb = self.psum()
                    S.op("tensor", lambda e, pb=pb, kc=kc, ch=ch, KET=KET: e.matmul(pb.t[:, 0:dv], KET.t[0:C, kc, :], VT.t[0:C, ch, 0:dv], start=True, stop=True),
                         reads=[KET.r, VT.r], writes=[pb.r])
                    sl = slice(so + kc * dv, so + (kc + 1) * dv)
                    el = eg.t[:, kc, (ch + 1) * C - 1:(ch + 1) * C]
                    S.op("vector", lambda e, sl=sl, el=el, Sc=Sc: e.tensor_scalar(out=Sc.t[:, sl], in0=Sc.t[:, sl], scalar1=el, scalar2=None, op0=ALU.mult), reads=[eg.r, Scb.r], writes=[Sc.r])
                    S.op("vector", lambda e, sl=sl, el=el, Sc=Sc, pb=pb: e.scalar_tensor_tensor(out=Sc.t[:, sl], in0=pb.t[:, 0:dv], scalar=el, in1=Sc.t[:, sl], op0=ALU.mult, op1=ALU.add),
                         reads=[pb.r, eg.r], writes=[Sc.r])
                    S.op("scalar", lambda e, sl=sl, Sc=Sc, Scb=Scb: e.copy(out=Scb.t[:, sl], in_=Sc.t[:, sl]), reads=[Sc.r], writes=[Scb.r])
                if tl.kind == "s":
                    S.dma("sync", st_s[ch, h], Sc.t[:, 0:kcn * dv], reads=[Sc.r])
            if tl.kind == "p" and tl.idx == self.NPT - 1:
                S.dma("sync", st_p[h], Sst.t[:, h * kcn * dv:(h + 1) * kcn * dv], reads=[Sst.r])
            pn = self.psum()
            for vc in range(vcn):
                q = self.sq[vc % 2]
                S.op("scalar", lambda e, q=q, vc=vc: e.activation(out=q.t[:, 0:N], in_=obs[vc].t[:, 0:N], func=AF.Square), reads=[obs[vc].r], writes=[q.r])
                S.op("tensor", lambda e, q=q, vc=vc, pn=pn: e.matmul(pn.t[:, 0:N], self.ones.t[:], q.t[:, 0:N], start=(vc == 0), stop=(vc == vcn - 1)),
                     reads=[q.r, self.ones.r], writes=[pn.r])
            S.op("scalar", lambda e, pn=pn: e.activation(out=t2.t[:, 0:N], in_=pn.t[:, 0:N], func=AF.Sqrt, bias=self.eps_ap(), scale=1.0 / dv), reads=[pn.r, self.pv.r], writes=[t2.r])
            S.op("vector", lambda e: e.reciprocal(out=t2.t[:, 0:N], in_=t2.t[:, 0:N]), writes=[t2.r])

            def cons_g(mc, msz, pb):
                S.op("scalar", lambda e: e.activation(out=t0.t[:, 0:N], in_=pb.t[:, 0:N], func=AF.Silu), reads=[pb.r], writes=[t0.r])
                S.op("vector", lambda e: e.scalar_tensor_tensor(out=t1b.t[:, 0:N], in0=obs[mc].t[:, 0:N], scalar=self.pcol(nname, mc), in1=t2.t[:, 0:N], op0=ALU.mult, op1=ALU.mult),
                     reads=[obs[mc].r, t2.r, self.pv.r], writes=[t1b.r])
                S.op("vector", lambda e: e.tensor_tensor(out=og.t[:, h * vcn + mc, 0:N], in0=t1b.t[:, 0:N], in1=t0.t[:, 0:N], op=ALU.mult), reads=[t0.r, t1b.r], writes=[og.r])
            self.proj(wg[:, h * dv:(h + 1) * dv], D, dv, xin, N, cons_g, mw=min(256, dv), wname="%s_g%d" % (kind, h))
        for h in range(H):
            head_body(h)
        oin = lambda k: (og.t[:, k, 0:N], [og.r])

        def cons_o(mc, msz, pb):
            S.op("vector", lambda e: e.tensor_tensor(out=self.hs.t[:, mc, 0:N], in0=self.hs.t[:, mc, 0:N], in1=pb.t[:, 0:N], op=ALU.add), reads=[pb.r], writes=[self.hs.r])
        self.proj(wo, D, D, oin, N, cons_o, wname="%s_o" % kind)
        self.store_h(tl)
    for tl in self.tiles:
        tile_body(tl)
    self.ring = self.psr


Net.la_setup = la_setup
Net.la_layer = la_layer


NCORES = 8
TPF, NSF = 2048, 16


def build_full(cols, pvn):
    nc = bass.Bass("TRN2", target_bir_lowering=False)
    es = ExitStack()
    cx = Ctx(nc, es)
    cfg = dict(TP=TPF, NS=NSF, DEPTH=4, pv_cols=cols, pv_n=pvn)
    net = Net(cx, cfg)
    net.la_setup()
    net.ffn_setup()
    net.rw_setup()
    net.s5_setup()
    S = cx.S
    net.s5_generate()
    net.s5_layer()
    barrier(S)
    net.ffn(0)
    barrier(S)
    net.rw_layer()
    barrier(S)
    net.ffn(1)
    barrier(S)
    net.la_layer("gla")
    barrier(S)
    net.ffn(2)
    barrier(S)
    net.la_layer("hg")
    barrier(S)
    net.ffn(3)
    barrier(S)
    net.final()
    cx.S.finish()
    cx.S.emit()
    es.close()
    return nc


def kernel(**inp):
    inp = {k: np.asarray(v) for k, v in inp.items()}
    cols, pv = pack_pvec(inp)
    nc = build_full(cols, pv.shape[1])
    B, T = inp["x_prompt"].shape[0], inp["x_prompt"].shape[1]
    rm = np.ones((128, 512), np.float32)
    rm[:, ::64] = 0
    shared = {"pvec": pv, "ident": np.eye(128, dtype=np.float32), "maskT": np.triu(np.ones((64, 64), np.float32)), "rmask": rm, "rwc": rw_consts()}
    shared.update(s5_inputs(inp))
    for k in ["rw_w_r", "rw_w_k", "rw_w_v", "rw_w_o", "rw_w1", "rw_w2", "rw_a1", "rw_a2", "rw_g1", "rw_g2", "ffn_w_up", "ffn_w_gate", "ffn_w_down", "gla_w_q", "gla_w_k", "gla_w_v", "gla_w_gk1", "gla_w_gk2", "gla_w_g", "gla_w_o",
              "hg_w_q", "hg_w_f", "hg_w_i", "hg_w_g", "hg_w_o"]:
        shared[k] = np.ascontiguousarray(inp[k], dtype=np.float32)
    in_maps = []
    for c in range(NCORES):
        b = c % B
        sl = slice(c * NSF, (c + 1) * NSF)
        m = dict(shared)
        m["xT"] = np.ascontiguousarray(np.concatenate([inp["x_prompt"][b], inp["x_sample"][sl, 0]], 0).T)
        cs = inp["state_ffn_conv"][:, sl]
        m["conv_s"] = np.ascontiguousarray(cs.reshape(4, NSF, 2, FC, 128).transpose(0, 4, 3, 1, 2).reshape(4, 128, -1))
        m["gla_s"] = np.ascontiguousarray(inp["state_gla"][sl].reshape(NSF, 4, 2, 128, 512).transpose(0, 1, 3, 2, 4).reshape(NSF, 4, 128, 1024))
        m["hg_s"] = np.ascontiguousarray(inp["state_hgrn"][sl])
        m["s5_h_s"] = s5_state_in(inp["state_s5_re"][sl], inp["state_s5_im"][sl], NSF)
        m["rw_shift_s"] = np.ascontiguousarray(inp["state_rwkv_shift"][sl].reshape(NSF, DC, 128).transpose(2, 1, 0).reshape(128, -1))
        m["rw_wkv_s"] = np.ascontiguousarray(inp["state_rwkv_wkv"][sl].transpose(3, 0, 1, 2).reshape(64, NSF, D))
        in_maps.append(m)
    res = run_bass_kernel_spmd(nc, in_maps, core_ids=list(range(NCORES)))
    R_ = res.results
    f32 = np.float32
    NSA = NCORES * NSF
    y_p = np.stack([R_[b]["yT"][:, :T].T for b in range(B)]).astype(f32)
    y_s = np.concatenate([R_[c]["yT"][:, T:].T for c in range(NCORES)])[:, None, :].astype(f32)
    conv_p = np.stack([R_[b]["conv_p_o"].reshape(4, 128, FC, 2).transpose(0, 3, 2, 1).reshape(4, 2, DFF) for b in range(B)], axis=1).astype(f32)
    conv_s = np.concatenate([R_[c]["conv_s_o"].reshape(4, 128, FC, NSF, 2).transpose(0, 3, 4, 2, 1).reshape(4, NSF, 2, DFF) for c in range(NCORES)], axis=1).astype(f32)
    gla_p = np.stack([R_[b]["gla_p_o"].reshape(4, 128, 2, 512).transpose(0, 2, 1, 3).reshape(4, 256, 512) for b in range(B)]).astype(f32)
    gla_s = np.concatenate([R_[c]["gla_s_o"].reshape(NSF, 4, 128, 2, 512).transpose(0, 1, 3, 2, 4).reshape(NSF, 4, 256, 512) for c in range(NCORES)]).astype(f32)
    hg_p = np.stack([R_[b]["hg_p_o"] for b in range(B)]).astype(f32)
    hg_s = np.concatenate([R_[c]["hg_s_o"] for c in range(NCORES)]).astype(f32)
    sh_p = np.stack([R_[b]["rw_shift_p_o"].T.reshape(-1) for b in range(B)]).astype(f32)
    sh_s = np.concatenate([R_[c]["rw_shift_s_o"].reshape(128, DC, NSF).transpose(2, 1, 0).reshape(NSF, D) for c in range(NCORES)]).astype(f32)
    wkv_p = np.stack([R_[b]["rw_wkv_p_o"].reshape(64, 32, 64).transpose(1, 2, 0) for b in range(B)]).astype(f32)
    wkv_s = np.concatenate([R_[c]["rw_wkv_s_o"].reshape(64, NSF, 32, 64).transpose(1, 2, 3, 0) for c in range(NCORES)]).astype(f32)
    s5p = [s5_state_out_p(R_[b]["s5_hp_o"]) for b in range(B)]
    s5s = [s5_state_out_s(R_[c]["s5_hs_o"], NSF) for c in range(NCORES)]
    s5_re_p = np.stack([a[0] for a in s5p]).astype(f32)
    s5_im_p = np.stack([a[1] for a in s5p]).astype(f32)
    s5_re_s = np.concatenate([a[0] for a in s5s]).astype(f32)
    s5_im_s = np.concatenate([a[1] for a in s5s]).astype(f32)
    return (y_p, y_s, s5_re_p, s5_im_p, sh_p, wkv_p, gla_p, hg_p, conv_p,
            s5_re_s, s5_im_s, sh_s, wkv_s, gla_s, hg_s, conv_s)


def barrier(S):
    evs = [(k, v) for k, v in S.cnt.items() if v > 0]
    for eng in S.ops:
        waits = []
        for ev in evs:
            if ev[0] == eng and eng == "tensor":
                continue
            S._need(eng, ev, waits)
        if waits:
            S.ops[eng].append((None, waits, None))


def rw_setup(self):
    c, S = self.cx, self.S
    NS = self.NS
    self.rwc = c.sb([128, 128 + 64 + 512 + 512])
    S.dma("sync", self.rwc.t[:], c.inp("rwc", [128, 1216]), writes=[self.rwc.r])
    self.rw_halo = c.sb([128, DC])
    self.rw_shs = c.sb([128, DC, NS])
    self.rw_tw = c.sb([128, 2, 512], BF16)
    self.rw_der = c.sb([128, 7 * DC])
    S.op("vector", lambda e: e.tensor_scalar(out=self.rw_der.t[:, 0:6 * DC], in0=self.pcol("rw_mix", 0, 6 * DC), scalar1=-1.0, scalar2=1.0, op0=ALU.mult, op1=ALU.add),
         reads=[self.pv.r], writes=[self.rw_der.r])
    S.op("vector", lambda e: e.tensor_scalar(out=self.rw_der.t[:, 6 * DC:7 * DC], in0=self.pcol("rw_w0", 0, DC), scalar1=-1.0, scalar2=None, op0=ALU.mult),
         reads=[self.pv.r], writes=[self.rw_der.r])
    self.rw_scr = c.scratch("rw_scr", [6, D, 512])
    self.rw_spR = [[R() for _ in range(DC)] for _ in range(6)]
    self.sti = 0


def rw_layer(self):
    S, c = self.S, self.cx
    NS = self.NS
    li = 1
    w_r, w_k, w_v, w_o = c.inp("rw_w_r", [D, D]), c.inp("rw_w_k", [D, D]), c.inp("rw_w_v", [D, D]), c.inp("rw_w_o", [D, D])
    w1, w2 = c.inp("rw_w1", [D, 96]), c.inp("rw_w2", [96, D])
    a1, a2 = c.inp("rw_a1", [D, 96]), c.inp("rw_a2", [96, D])
    g1, g2 = c.inp("rw_g1", [D, 256]), c.inp("rw_g2", [256, D])
    shift_in = c.inp("rw_shift_s", [128, DC * NS])
    wkv_in = c.inp("rw_wkv_s", [64, NS, D])
    shift_p_o = c.outp("rw_shift_p_o", [128, DC])
    shift_s_o = c.outp("rw_shift_s_o", [128, DC * NS])
    wkv_p_o = c.outp("rw_wkv_p_o", [64, D])
    wkv_s_o = c.outp("rw_wkv_s_o", [64, NS, D])
    hs, xn, og = self.hs, self.xn, self.la_og
    rwc = self.rwc
    bones = rwc.t[:, 0:128]
    ident2 = rwc.t[:, 128:192]
    mG12 = rwc.t[0:64, 192:704].rearrange("p (u w) -> p u w", u=4)
    mG3 = rwc.t[0:64, 704:1216].rearrange("p (u w) -> p u w", u=4)
    identF = self.ident
    E = []
    for b in self.la_f:
        E.append(View(b.t[:, 0, :]))
        E.append(View(b.t[:, 1, :]))
    E += [View(b.t[:]) for b in self.la_t] + [View(self.gbuf[1].t[:])] + [View(b.t[:]) for b in self.sq]
    stage = [E[10], E[11]]
    st_p = View(self.la_S.t[0:64, 0:2048].rearrange("p (h i) -> p h i", h=32))
    st_s = View(self.la_S.t[0:64, 2048:4096].rearrange("p (s i) -> p s i", s=NS))
    S.op("vector", lambda e: e.memset(self.la_S.t[:], 0.0), writes=[self.la_S.r, st_p.r])
    S.op("vector", lambda e: e.memset(self.rw_halo.t[:], 0.0), writes=[self.rw_halo.r])
    S.dma("sync", self.rw_shs.t[:].rearrange("p c s -> p (c s)"), shift_in, writes=[self.rw_shs.r])
    hsF = hs.t[:].rearrange("p c n -> p (c n)")
    xnF = xn.t[:].rearrange("p c n -> p (c n)").bitcast(F32)
    actF = self.act.t[:, DC:FC, :].rearrange("p f n -> p (f n)").bitcast(F32)
    self.ring = self.psr[0:4]
    ob = self.obank[0]

    def tile_body(tl):
        N = tl.n
        C = 64 if tl.kind == "p" else 1
        nch = N // C
        U = nch
        W2 = 2 * C
        barrier(S)
        self.load_h(tl, False)
        self.rms(hs, N, "norm_mix", li, out_f32=hs)
        if tl.kind == "s":
            S.dma("sync", shift_s_o.rearrange("p (c s) -> p c s", c=DC), hs.t[:, :, 0:N], reads=[hs.r])

        def variant(mi):
            tmp = E[12]
            for cc in range(DC):
                m = self.pcol("rw_mix", mi * DC + cc)
                om = self.rw_der.t[:, mi * DC + cc: mi * DC + cc + 1]
                if tl.kind == "p":
                    S.op("vector", lambda e, cc=cc, m=m: e.tensor_scalar(out=tmp.t[:, 1:N], in0=hs.t[:, cc, 0:N - 1], scalar1=m, scalar2=None, op0=ALU.mult),
                         reads=[hs.r, self.pv.r], writes=[tmp.r])
                    S.op("vector", lambda e, cc=cc, m=m: e.tensor_scalar(out=tmp.t[:, 0:1], in0=self.rw_halo.t[:, cc:cc + 1], scalar1=m, scalar2=None, op0=ALU.mult),
                         reads=[self.rw_halo.r, self.pv.r], writes=[tmp.r])
                else:
                    S.op("vector", lambda e, cc=cc, m=m: e.tensor_scalar(out=tmp.t[:, 0:N], in0=self.rw_shs.t[:, cc, :], scalar1=m, scalar2=None, op0=ALU.mult),
                         reads=[self.rw_shs.r, self.pv.r], writes=[tmp.r])
                S.op("vector", lambda e, cc=cc, om=om: e.scalar_tensor_tensor(out=xn.t[:, cc, 0:N], in0=hs.t[:, cc, 0:N], scalar=om, in1=tmp.t[:, 0:N], op0=ALU.mult, op1=ALU.add),
                     reads=[hs.r, tmp.r, self.rw_der.r], writes=[xn.r])
        xin = lambda k: (xn.t[:, k, 0:N], [xn.r])

        def spill(ti):
            def cons(mc, msz, pb):
                st = stage[self.sti % 2]
                self.sti += 1
                S.op("scalar", lambda e: e.copy(out=st.t[:, 0:N], in_=pb.t[:, 0:N]), reads=[pb.r], writes=[st.r])
                S.dma("sync", self.rw_scr[ti][mc * 128:(mc + 1) * 128, 0:N], st.t[:, 0:N], reads=[st.r], writes=[self.rw_spR[ti][mc]])
            return cons
        tw = self.rw_tw
        variant(0)
        self.proj(w_r, D, D, xin, N, spill(0), wname="rw_r")
        variant(2)
        self.proj(w_k, D, D, xin, N, spill(1), wname="rw_k")
        variant(3)
        self.proj(w_v, D, D, xin, N, spill(2), wname="rw_v")
        variant(1)

        def cons_tw(mc, msz, pb):
            S.op("scalar", lambda e: e.activation(out=tw.t[0:96, 0, 0:N], in_=pb.t[0:96, 0:N], func=AF.Tanh), reads=[pb.r], writes=[tw.r])
        self.proj(w1, D, 96, xin, N, cons_tw)
        self.proj(w2, 96, D, lambda k: (tw.t[0:96, 0, 0:N], [tw.r]), N, spill(3))
        variant(4)

        def cons_ta(mc, msz, pb):
            S.op("scalar", lambda e: e.copy(out=tw.t[0:96, 0, 0:N], in_=pb.t[0:96, 0:N]), reads=[pb.r], writes=[tw.r])
        self.proj(a1, D, 96, xin, N, cons_ta)
        self.proj(a2, 96, D, lambda k: (tw.t[0:96, 0, 0:N], [tw.r]), N, spill(4))
        variant(5)

        def cons_tg(mc, msz, pb):
            S.op("scalar", lambda e: e.activation(out=tw.t[:, mc, 0:N], in_=pb.t[:, 0:N], func=AF.Sigmoid), reads=[pb.r], writes=[tw.r])
        self.proj(g1, D, 256, xin, N, cons_tg)
        self.proj(g2, 256, D, lambda k: (tw.t[:, k, 0:N], [tw.r]), N, spill(5))
        if tl.kind == "p":
            S.op("vector", lambda e: e.tensor_copy(out=self.rw_halo.t[:], in_=hs.t[:, :, N - 1]), reads=[hs.r], writes=[self.rw_halo.r])
            if tl.idx == self.NPT - 1:
                S.dma("sync", shift_p_o, self.rw_halo.t[:], reads=[self.rw_halo.r])
        barrier(S)
        o = [0]

        def carve(arena, shape):
            n = shape[1] * shape[2]
            v = View(arena[0:shape[0], o[0]:o[0] + n].rearrange("p (a b) -> p a b", a=shape[1]))
            o[0] += n
            return v
        G1m, G2m, G3m = [carve(hsF, [64, U, W2]) for _ in range(3)]
        Z1a, Qh, Eh, Fh = [carve(hsF, [64, U, 64]) for _ in range(4)]
        Z1b, Dh = [carve(hsF, [64, U, C]) for _ in range(2)]
        Xs = [carve(hsF, [64, U, C]) for _ in range(2)]
        Ys = [carve(hsF, [64, U, C]) for _ in range(2)]
        assert o[0] <= 8192
        o[0] = 0
        TMs = [carve(actF, [64, 8, 512])]
        Ts = [carve(actF, [64, U, C]) for _ in range(2)]
        Dg = carve(actF, [128, nch, 64])
        assert o[0] <= 7168
        if nch > 8:
            o[0] = 0
            TMs.append(carve(xnF, [64, 8, 512]))
        TM = TMs[0]

        def tmr(u, c0, c1):
            return TMs[u // 8].t[0:C, u % 8, c0:c1]

        def pair_body(hp):
            bufs = {}
            for ti, nm in enumerate(["r", "k", "v", "wl", "a", "g"]):
                b = E[ti]
                S.dma("sync", b.t[:, 0:N], self.rw_scr[ti][hp * 128:(hp + 1) * 128, 0:N], reads=[self.rw_spR[ti][hp]], writes=[b.r])
                bufs[nm] = b
            r, k, v, wl, a, g = [bufs[n] for n in ["r", "k", "v", "wl", "a", "g"]]
            kk, t1, t2, cum, eg, en, bonus, btp, ktp = E[6], E[7], E[8], E[9], E[10], E[11], E[12], E[13], E[14]
            Xb_, Yb_ = self.la_Ss, None
            X = View(self.la_Ss.t[:, 0:1024].rearrange("p (a n) -> p a n", a=2))
            Y = View(self.la_Sbf.t[:, 0:2048].bitcast(F32).rearrange("p (a n) -> p a n", a=2))
            sl = slice(0, N)

            def act_(out, in_, func, reads, writes, **kw):
                S.op("scalar", lambda e: e.activation(out=out, in_=in_, func=func, **kw), reads=reads, writes=writes)

            def tt(out, in0, in1, op, reads, writes):
                S.op("vector", lambda e: e.tensor_tensor(out=out, in0=in0, in1=in1, op=op), reads=reads, writes=writes)

            def ts(out, in0, s1, s2, op0, op1, reads, writes):
                if s2 is None:
                    S.op("vector", lambda e: e.tensor_scalar(out=out, in0=in0, scalar1=s1, scalar2=None, op0=op0), reads=reads, writes=writes)
                else:
                    S.op("vector", lambda e: e.tensor_scalar(out=out, in0=in0, scalar1=s1, scalar2=s2, op0=op0, op1=op1), reads=reads, writes=writes)
            pvr = self.pv.r
            act_(wl.t[:, sl], wl.t[:, sl], AF.Exp, [self.rw_der.r], [wl.r], scale=-1.0, bias=self.rw_der.t[:, 6 * DC + hp:6 * DC + hp + 1])
            act_(wl.t[:, sl], wl.t[:, sl], AF.Ln, [pvr], [wl.r], bias=self.pcol("one"))
            act_(wl.t[:, sl], wl.t[:, sl], AF.Exp, [pvr], [wl.r], scale=-1.0, bias=self.pcol("neghalf"))
            ts(wl.t[:, sl], wl.t[:, sl], -1.0, None, ALU.mult, None, [], [wl.r])
            act_(a.t[:, sl], a.t[:, sl], AF.Sigmoid, [pvr], [a.r], bias=self.pcol("rw_a0", hp))
            ts(kk.t[:, sl], k.t[:, sl], self.pcol("rw_k_k", hp), None, ALU.mult, None, [k.r, pvr], [kk.r])
            tt(t1.t[:, sl], kk.t[:, sl], kk.t[:, sl], ALU.mult, [kk.r], [t1.r])
            pb = self.psum()
            S.op("tensor", lambda e, pb=pb: e.matmul(pb.t[:, sl], bones, t1.t[:, sl], start=True, stop=True), reads=[t1.r, rwc.r], writes=[pb.r])
            act_(t1.t[:, sl], pb.t[:, sl], AF.Sqrt, [pb.r], [t1.r])
            ts(t1.t[:, sl], t1.t[:, sl], 1e-12, None, ALU.max, None, [], [t1.r])
            S.op("vector", lambda e: e.reciprocal(out=t1.t[:, sl], in_=t1.t[:, sl]), writes=[t1.r])
            tt(kk.t[:, sl], kk.t[:, sl], t1.t[:, sl], ALU.mult, [t1.r], [kk.r])
            ts(t2.t[:, sl], a.t[:, sl], -1.0, self.pcol("rw_k_a", hp), ALU.add, ALU.mult, [a.r, pvr], [t2.r])
            S.op("vector", lambda e: e.scalar_tensor_tensor(out=k.t[:, sl], in0=t2.t[:, sl], scalar=1.0, in1=k.t[:, sl], op0=ALU.add, op1=ALU.mult), reads=[t2.r], writes=[k.r])
            tt(t1.t[:, sl], r.t[:, sl], k.t[:, sl], ALU.mult, [r.r, k.r], [t1.r])
            ts(t1.t[:, sl], t1.t[:, sl], self.pcol("rw_r_k", hp), None, ALU.mult, None, [pvr], [t1.r])
            pb2 = self.psum()
            S.op("tensor", lambda e, pb2=pb2: e.matmul(pb2.t[:, sl], bones, t1.t[:, sl], start=True, stop=True), reads=[t1.r, rwc.r], writes=[pb2.r])
            tt(bonus.t[:, sl], pb2.t[:, sl], v.t[:, sl], ALU.mult, [pb2.r, v.r], [bonus.r])
            S.op("vector", lambda e: e.tensor_tensor_scan(out=cum.t[:, sl], data0=self.rmask.t[:, sl] if C > 1 else self.zero_ap(N), data1=wl.t[:, sl], initial=0.0, op0=ALU.mult, op1=ALU.add),
                 reads=[wl.r, self.rmask.r, self.zeros.r], writes=[cum.r])
            act_(eg.t[:, sl], cum.t[:, sl], AF.Exp, [cum.r], [eg.r])
            act_(en.t[:, sl], cum.t[:, sl], AF.Exp, [cum.r], [en.r], scale=-1.0)
            tt(t2.t[:, sl], cum.t[:, sl], wl.t[:, sl], ALU.subtract, [cum.r, wl.r], [t2.r])
            act_(t2.t[:, sl], t2.t[:, sl], AF.Exp, [], [t2.r])
            S.op("vector", lambda e: e.scalar_tensor_tensor(out=X.t[:, 0, sl], in0=kk.t[:, sl], scalar=-1.0, in1=t2.t[:, sl], op0=ALU.mult, op1=ALU.mult), reads=[kk.r, t2.r], writes=[X.r])
            tt(X.t[:, 1, sl], r.t[:, sl], eg.t[:, sl], ALU.mult, [r.r, eg.r], [X.r])
            tt(t1.t[:, sl], kk.t[:, sl], a.t[:, sl], ALU.mult, [kk.r, a.r], [t1.r])
            tt(Y.t[:, 0, sl], t1.t[:, sl], en.t[:, sl], ALU.mult, [t1.r, en.r], [Y.r])
            tt(Y.t[:, 1, sl], k.t[:, sl], en.t[:, sl], ALU.mult, [k.r, en.r], [Y.r])
            for ch in range(nch):
                cs = slice(ch * C, (ch + 1) * C)
                el = eg.t[:, (ch + 1) * C - 1:(ch + 1) * C]
                ts(btp.t[:, cs], Y.t[:, 0, cs], el, None, ALU.mult, None, [Y.r, eg.r], [btp.r])
                ts(ktp.t[:, cs], Y.t[:, 1, cs], el, None, ALU.mult, None, [Y.r, eg.r], [ktp.r])
                ts(Dg.t[:, ch, :], ident2, el, None, ALU.mult, None, [rwc.r, eg.r], [Dg.r])
            for ch in range(nch):
                cs = slice(ch * C, (ch + 1) * C)
                pbt = self.psum()
                for wi_, src in enumerate([X.t[:, 0, cs], btp.t[:, cs], ktp.t[:, cs], v.t[:, cs]]):
                    S.op("tensor", lambda e, pbt=pbt, wi_=wi_, src=src: e.transpose(out=pbt.t[0:C, wi_ * 128:(wi_ + 1) * 128], in_=src, identity=identF.t[:]),
                         reads=[X.r, btp.r, ktp.r, v.r, identF.r], writes=[pbt.r])
                S.op("scalar", lambda e, pbt=pbt, ch=ch: e.copy(out=tmr(ch, 0, 512), in_=pbt.t[0:C, 0:512]), reads=[pbt.r], writes=[TM.r])

            def head_body(hd):
                P = slice(hd * 64, hd * 64 + 64)
                hcol = slice(hd * 64, hd * 64 + 64)
                UB = 512 // W2
                for (Gm, lh, rh, mk) in ((G1m, Y.t[P, 0, :], X, mG12), (G2m, Y.t[P, 1, :], X, mG12), (G3m, X.t[P, 0, :], Y, mG3)):
                    for u0 in range(0, U, 4):
                        pbg = self.psum()
                        for u in range(u0, min(U, u0 + 4)):
                            cs = slice(u * C, (u + 1) * C)
                            S.op("tensor", lambda e, pbg=pbg, u=u, u0=u0, cs=cs, lh=lh, rh=rh: e.matmul(pbg.t[0:C, (u - u0) * W2:(u - u0 + 1) * W2], lh[:, cs], rh.t[P, :, cs], start=True, stop=True),
                                 reads=[X.r, Y.r], writes=[pbg.r])
                        nu = min(U, u0 + 4) - u0
                        mview = mk[0:C, 0:nu, :] if C == 64 else mk[0:1, 0:nu, 0:128:64]
                        S.op("vector", lambda e, pbg=pbg, u0=u0, nu=nu, Gm=Gm, mview=mview: e.tensor_tensor(out=Gm.t[0:C, u0:u0 + nu, :], in0=pbg.t[0:C, 0:nu * W2].rearrange("p (u w) -> p u w", u=nu), in1=mview, op=ALU.mult),
                             reads=[pbg.r, rwc.r], writes=[Gm.r])
                Tc, Tn = Ts[0], Ts[1]
                for u in range(U):
                    S.op("vector", lambda e, u=u, Tc=Tc: e.tensor_tensor(out=Tc.t[0:C, u, :], in0=G1m.t[0:C, u, 0:C], in1=identF.t[0:C, 0:C], op=ALU.add), reads=[G1m.r, identF.r], writes=[Tc.r])
                Xc, Yc = View(G1m.t[:, :, 0:C]), View(G3m.t[:, :, 0:C])
                Xc.r, Yc.r = G1m.r, G3m.r
                nlev = 5 if C == 64 else 0
                for lv in range(1, nlev + 1):
                    UBc = max(1, 512 // C)

                    def mm_level(lhb, rhb, dst, addto=None):
                        for u0 in range(0, U, UBc):
                            pbm = self.psum()
                            nu = min(U, u0 + UBc) - u0
                            for u in range(u0, u0 + nu):
                                S.op("tensor", lambda e, pbm=pbm, u=u, u0=u0: e.matmul(pbm.t[0:C, (u - u0) * C:(u - u0 + 1) * C], lhb.t[0:C, u, :], rhb.t[0:C, u, :], start=True, stop=True),
                                     reads=[lhb.r, rhb.r], writes=[pbm.r])
                            pv_ = pbm.t[0:C, 0:nu * C].rearrange("p (u w) -> p u w", u=nu)
                            if addto is None:
                                S.op("scalar", lambda e, pv_=pv_, u0=u0, nu=nu: e.copy(out=dst.t[0:C, u0:u0 + nu, :], in_=pv_), reads=[pbm.r], writes=[dst.r])
                            else:
                                S.op("vector", lambda e, pv_=pv_, u0=u0, nu=nu: e.tensor_tensor(out=dst.t[0:C, u0:u0 + nu, :], in0=pv_, in1=addto.t[0:C, u0:u0 + nu, :], op=ALU.add),
                                     reads=[pbm.r, addto.r], writes=[dst.r])
                    Xn, Yn = Xs[lv % 2], Ys[lv % 2]
                    if lv < nlev:
                        mm_level(Yc, Xc, Xn)
                    mm_level(Xc, Yc, Yn)
                    mm_level(Yn, Tc, Tn, addto=Tc)
                    Xc, Yc = Xn, Yn
                    Tc, Tn = Tn, Tc
                T = Tc
                for u0 in range(0, U, 8):
                    nu = min(U, u0 + 8) - u0
                    pa_, pb_ = self.psum(), self.psum()
                    for u in range(u0, u0 + nu):
                        S.op("tensor", lambda e, pa_=pa_, u=u, u0=u0: e.matmul(pa_.t[0:C, (u - u0) * 64:(u - u0 + 1) * 64], T.t[0:C, u, :], tmr(u, hd * 64, hd * 64 + 64), start=True, stop=True),
                             reads=[T.r, TM.r], writes=[pa_.r])
                        S.op("tensor", lambda e, pb_=pb_, u=u, u0=u0: e.matmul(pb_.t[0:C, (u - u0) * C:(u - u0 + 1) * C], T.t[0:C, u, :], G3m.t[0:C, u, C:W2], start=True, stop=True),
                             reads=[T.r, G3m.r], writes=[pb_.r])
                    S.op("scalar", lambda e, pa_=pa_, u0=u0, nu=nu: e.copy(out=Z1a.t[0:C, u0:u0 + nu, :], in_=pa_.t[0:C, 0:nu * 64].rearrange("p (u w) -> p u w", u=nu)), reads=[pa_.r], writes=[Z1a.r])
                    S.op("vector", lambda e, pb_=pb_, u0=u0, nu=nu: e.tensor_copy(out=Z1b.t[0:C, u0:u0 + nu, :], in_=pb_.t[0:C, 0:nu * C].rearrange("p (u w) -> p u w", u=nu)), reads=[pb_.r], writes=[Z1b.r])
                for u0 in range(0, U, 8):
                    nu = min(U, u0 + 8) - u0
                    pq, pe, pd, pf = self.psum(), self.psum(), self.psum(), self.psum()
                    for u in range(u0, u0 + nu):
                        j = u - u0
                        cs = slice(u * C, (u + 1) * C)
                        S.op("tensor", lambda e, pq=pq, j=j, cs=cs: e.matmul(pq.t[0:64, j * C:(j + 1) * C], identF.t[:, hcol], X.t[:, 1, cs], start=True, stop=False), reads=[identF.r, X.r], writes=[pq.r])
                        S.op("tensor", lambda e, pq=pq, j=j, u=u: e.matmul(pq.t[0:64, j * C:(j + 1) * C], Z1a.t[0:C, u, :], G1m.t[0:C, u, C:W2], start=False, stop=True), reads=[Z1a.r, G1m.r], writes=[pq.r])
                        S.op("tensor", lambda e, pe=pe, j=j, u=u: e.matmul(pe.t[0:64, j * 64:(j + 1) * 64], identF.t[:, hcol], Dg.t[:, u, :], start=True, stop=False), reads=[identF.r, Dg.r], writes=[pe.r])
                        S.op("tensor", lambda e, pe=pe, j=j, u=u: e.matmul(pe.t[0:64, j * 64:(j + 1) * 64], Z1a.t[0:C, u, :], tmr(u, 128 + hd * 64, 128 + hd * 64 + 64), start=False, stop=True), reads=[Z1a.r, TM.r], writes=[pe.r])
                        S.op("tensor", lambda e, pd=pd, j=j, u=u: e.matmul(pd.t[0:C, j * C:(j + 1) * C], Z1b.t[0:C, u, :], G1m.t[0:C, u, C:W2], start=True, stop=True), reads=[Z1b.r, G1m.r], writes=[pd.r])
                        S.op("tensor", lambda e, pf=pf, j=j, u=u: e.matmul(pf.t[0:C, j * 64:(j + 1) * 64], Z1b.t[0:C, u, :], tmr(u, 128 + hd * 64, 128 + hd * 64 + 64), start=True, stop=True), reads=[Z1b.r, TM.r], writes=[pf.r])
                    S.op("scalar", lambda e, pq=pq, u0=u0, nu=nu: e.copy(out=Qh.t[:, u0:u0 + nu, 0:C], in_=pq.t[0:64, 0:nu * C].rearrange("p (u w) -> p u w", u=nu)), reads=[pq.r], writes=[Qh.r])
                    S.op("scalar", lambda e, pe=pe, u0=u0, nu=nu: e.copy(out=Eh.t[:, u0:u0 + nu, :], in_=pe.t[0:64, 0:nu * 64].rearrange("p (u w) -> p u w", u=nu)), reads=[pe.r], writes=[Eh.r])
                    S.op("vector", lambda e, pd=pd, u0=u0, nu=nu: e.tensor_tensor(out=Dh.t[0:C, u0:u0 + nu, :], in0=pd.t[0:C, 0:nu * C].rearrange("p (u w) -> p u w", u=nu), in1=G2m.t[0:C, u0:u0 + nu, C:W2], op=ALU.add),
                         reads=[pd.r, G2m.r], writes=[Dh.r])
                    S.op("vector", lambda e, pf=pf, u0=u0, nu=nu: e.tensor_tensor(out=Fh.t[0:C, u0:u0 + nu, :], in0=pf.t[0:C, 0:nu * 64].rearrange("p (u w) -> p u w", u=nu), in1=TMs[u0 // 8].t[0:C, 0:nu, 256 + hd * 64:256 + hd * 64 + 64], op=ALU.add),
                         reads=[pf.r, TM.r], writes=[Fh.r])
                for u in range(U):
                    cs = slice(u * C, (u + 1) * C)
                    if tl.kind == "p":
                        stb, stv = st_p, st_p.t[:, 2 * hp + hd, :]
                    else:
                        stb, stv = st_s, st_s.t[:, u, hd * 64:hd * 64 + 64]
                    vt = tmr(u, 384 + hd * 64, 384 + hd * 64 + 64)
                    S.op("tensor", lambda e, u=u, cs=cs, stv=stv: e.matmul(ob.t[P, cs], stv, Qh.t[:, u, 0:C], start=True, stop=False), reads=[stb.r, Qh.r], writes=[ob.r])
                    S.op("tensor", lambda e, u=u, cs=cs, vt=vt: e.matmul(ob.t[P, cs], vt, Dh.t[0:C, u, :], start=False, stop=True), reads=[TM.r, Dh.r], writes=[ob.r])
                    pst = self.psum()
                    S.op("tensor", lambda e, u=u, pst=pst, stv=stv: e.matmul(pst.t[0:64, 0:64], Eh.t[:, u, :], stv, start=True, stop=False), reads=[Eh.r, stb.r], writes=[pst.r])
                    S.op("tensor", lambda e, u=u, pst=pst, vt=vt: e.matmul(pst.t[0:64, 0:64], Fh.t[0:C, u, :], vt, start=False, stop=True), reads=[Fh.r, TM.r], writes=[pst.r])
                    S.op("scalar", lambda e, pst=pst, stv=stv: e.copy(out=stv, in_=pst.t[0:64, 0:64]), reads=[pst.r], writes=[stb.r])
            if tl.kind == "s":
                S.dma("sync", st_s.t[:], wkv_in[:, :, hp * 128:(hp + 1) * 128], writes=[st_s.r])
            head_body(0)
            head_body(1)
            if tl.kind == "s":
                S.dma("sync", wkv_s_o[:, :, hp * 128:(hp + 1) * 128], st_s.t[:], reads=[st_s.r])
            osb, d = t1, t2
            S.op("scalar", lambda e: e.copy(out=osb.t[:, sl], in_=ob.t[:, sl]), reads=[ob.r], writes=[osb.r])
            pm = self.psum()
            S.op("tensor", lambda e, pm=pm: e.matmul(pm.t[:, sl], bones, osb.t[:, sl], start=True, stop=True), reads=[osb.r, rwc.r], writes=[pm.r])
            S.op("vector", lambda e, pm=pm: e.scalar_tensor_tensor(out=d.t[:, sl], in0=pm.t[:, sl], scalar=-1.0 / 64.0, in1=osb.t[:, sl], op0=ALU.mult, op1=ALU.add), reads=[pm.r, osb.r], writes=[d.r])
            tt(osb.t[:, sl], d.t[:, sl], d.t[:, sl], ALU.mult, [d.r], [osb.r])
            pv2 = self.psum()
            S.op("tensor", lambda e, pv2=pv2: e.matmul(pv2.t[:, sl], bones, osb.t[:, sl], start=True, stop=True), reads=[osb.r, rwc.r], writes=[pv2.r])
            act_(osb.t[:, sl], pv2.t[:, sl], AF.Sqrt, [pv2.r, pvr], [osb.r], scale=1.0 / 64.0, bias=self.pcol("lneps"))
            S.op("vector", lambda e: e.reciprocal(out=osb.t[:, sl], in_=osb.t[:, sl]), writes=[osb.r])
            tt(d.t[:, sl], d.t[:, sl], osb.t[:, sl], ALU.mult, [osb.r], [d.r])
            ts(d.t[:, sl], d.t[:, sl], self.pcol("rw_ln_w", hp), self.pcol("rw_ln_b", hp), ALU.mult, ALU.add, [pvr], [d.r])
            tt(d.t[:, sl], d.t[:, sl], bonus.t[:, sl], ALU.add, [bonus.r], [d.r])
            tt(og.t[:, hp, sl], d.t[:, sl], g.t[:, sl], ALU.mult, [d.r, g.r], [og.r])
        for hp in range(DC):
            pair_body(hp)
        if tl.kind == "p" and tl.idx == self.NPT - 1:
            S.dma("sync", wkv_p_o, st_p.t[:].rearrange("p h i -> p (h i)"), reads=[st_p.r])
        barrier(S)
        self.load_h(tl, False)
        oin = lambda k: (og.t[:, k, 0:N], [og.r])

        def cons_o(mc, msz, pb):
            S.op("vector", lambda e: e.tensor_tensor(out=hs.t[:, mc, 0:N], in0=hs.t[:, mc, 0:N], in1=pb.t[:, 0:N], op=ALU.add), reads=[pb.r], writes=[hs.r])
        self.proj(w_o, D, D, oin, N, cons_o, wname="rw_o")
        self.store_h(tl)
    for tl in self.tiles:
        tile_body(tl)
    barrier(S)
    self.ring = self.psr


Net.rw_setup = rw_setup
Net.rw_layer = rw_layer


def s5_setup(self):
    c, S = self.cx, self.S
    NS = self.NS
    self.s5_par = c.inp("s5_par", [128, 5 * 64])
    self.s5_B = c.inp("s5_B", [2, 128, 1024])
    self.s5_C = c.inp("s5_C", [2, 128, 1024])
    self.s5_msk = c.inp("s5_msk", [128, 128 + 8])
    self.s5_wglu = c.inp("s5_w_glu", [D, D])
    self.s5_hin = c.inp("s5_h_s", [2, 128, 64 * NS])
    self.s5_hp_o = c.outp("s5_hp_o", [2, 128, 64])
    self.s5_hs_o = c.outp("s5_hs_o", [2, 128, 64 * NS])
    self.KBDd = c.scratch("s5_kbd", [16, 128, 8 * 128], BF16)
    self.WBDd = c.scratch("s5_wbd", [16, 128, 8 * 8 * 2 * 64], BF16)
    self.s5_small = c.sb([128, 6, 64])
    self.s5_mk = c.sb([128, 136])
    S.dma("sync", self.s5_mk.t[:], self.s5_msk, writes=[self.s5_mk.r])


def s5_generate(self):
    S, c = self.S, self.cx
    sm = self.s5_small
    A1r, A1i, A8r, A8i, Hcr, Hci = [sm.t[:, j, :] for j in range(6)]
    tsm = [self.la_t[1].t[:, j * 64:(j + 1) * 64] for j in range(8)] + [self.la_t[2].t[:, j * 64:(j + 1) * 64] for j in range(2)]
    par = self.la_t[0]
    S.dma("sync", par.t[:, 0:320], self.s5_par, writes=[par.r])
    lam_re, lam_im, logdt, halfpi = [par.t[:, j * 64:(j + 1) * 64] for j in range(4)]
    smr = sm.r

    def tt(out, a, b, op, extra=()):
        S.op("vector", lambda e: e.tensor_tensor(out=out, in0=a, in1=b, op=op), reads=[par.r] + list(extra), writes=[smr])

    def tsc(out, a, s1, s2, op0, op1=None):
        if s2 is None:
            S.op("vector", lambda e: e.tensor_scalar(out=out, in0=a, scalar1=s1, scalar2=None, op0=op0), reads=[par.r], writes=[smr])
        else:
            S.op("vector", lambda e: e.tensor_scalar(out=out, in0=a, scalar1=s1, scalar2=s2, op0=op0, op1=op1), reads=[par.r], writes=[smr])

    def act_(out, a, func, **kw):
        S.op("scalar", lambda e: e.activation(out=out, in_=a, func=func, **kw), reads=[par.r, self.pv.r], writes=[smr])
    lr, dt, xr, th, cc, ss, t0, t1, t2, t3 = tsm
    tsc(lr, lam_re, -1e-4, None, ALU.min)
    act_(dt, logdt, AF.Exp)
    tt(xr, lr, dt, ALU.mult)
    tt(th, lam_im, dt, ALU.mult)
    act_(ss, th, AF.Sin, scale=1.0 / 16.0)
    act_(cc, th, AF.Sin, scale=1.0 / 16.0, bias=self.pcol("halfpi"))
    for _ in range(4):
        tt(t0, cc, cc, ALU.mult)
        tt(t1, ss, ss, ALU.mult)
        tt(t2, cc, ss, ALU.mult)
        tt(cc, t0, t1, ALU.subtract)
        tsc(ss, t2, 2.0, None, ALU.mult)
    act_(t3, xr, AF.Exp)
    tt(A1r, t3, cc, ALU.mult)
    tt(A1i, t3, ss, ALU.mult)
    tt(t0, lr, lr, ALU.mult)
    tt(t1, lam_im, lam_im, ALU.mult)
    tt(t0, t0, t1, ALU.add)
    S.op("vector", lambda e: e.reciprocal(out=t0, in_=t0), writes=[smr])
    tsc(t1, A1r, -1.0, None, ALU.add)
    tt(t2, t1, lr, ALU.mult)
    tt(t3, A1i, lam_im, ALU.mult)
    tt(t2, t2, t3, ALU.add)
    tt(cc, t2, t0, ALU.mult)
    tt(t2, A1i, lr, ALU.mult)
    tt(t3, t1, lam_im, ALU.mult)
    tt(t2, t2, t3, ALU.subtract)
    tt(ss, t2, t0, ALU.mult)
    S.op("vector", lambda e: e.tensor_copy(out=A8r, in_=A1r), writes=[smr])
    S.op("vector", lambda e: e.tensor_copy(out=A8i, in_=A1i), writes=[smr])
    for _ in range(3):
        tt(t0, A8r, A8r, ALU.mult)
        tt(t1, A8i, A8i, ALU.mult)
        tt(t2, A8r, A8i, ALU.mult)
        tt(A8r, t0, t1, ALU.subtract)
        tsc(A8i, t2, 2.0, None, ALU.mult)
    S.op("vector", lambda e: e.memset(sm.t[:, 4:6, :], 0.0), writes=[smr])
    big = [View(b.t[:].rearrange("p k n -> p (k n)").rearrange("p (g q) -> p g q", q=16)) for b in self.la_f]
    Wr, Wi, T1, T2, Bb = big
    Cre = View(self.la_Ss.t[:, 0:1024].rearrange("p (g q) -> p g q", q=16))
    Cni = View(self.la_Sbf.t[:, 0:2048].bitcast(F32).rearrange("p (g q) -> p g q", q=16))
    self.s5_Cre, self.s5_Cni = Cre, Cni
    S.dma("sync", Cre.t[:].rearrange("p g q -> p (g q)"), self.s5_C[0], writes=[Cre.r])
    S.dma("sync", Cni.t[:].rearrange("p g q -> p (g q)"), self.s5_C[1], writes=[Cni.r])
    S.op("vector", lambda e: e.tensor_scalar(out=Cni.t[:], in0=Cni.t[:], scalar1=-1.0, scalar2=None, op0=ALU.mult), writes=[Cni.r])
    S.dma("sync", T1.t[:].rearrange("p g q -> p (g q)"), self.s5_B[0], writes=[T1.r])
    S.dma("sync", T2.t[:].rearrange("p g q -> p (g q)"), self.s5_B[1], writes=[T2.r])
    bc = lambda a: a.rearrange("p (g o) -> p g o", o=1).broadcast_to([128, 64, 16])

    def btt(out, a, b, op, rd):
        S.op("vector", lambda e: e.tensor_tensor(out=out.t[:], in0=a, in1=b, op=op), reads=rd + [smr], writes=[out.r])
    btt(Wr, T1.t[:], bc(cc), ALU.mult, [T1.r])
    btt(Bb, T2.t[:], bc(ss), ALU.mult, [T2.r])
    btt(Wr, Wr.t[:], Bb.t[:], ALU.subtract, [Bb.r])
    btt(Wi, T2.t[:], bc(cc), ALU.mult, [T2.r])
    btt(Bb, T1.t[:], bc(ss), ALU.mult, [T1.r])
    btt(Wi, Wi.t[:], Bb.t[:], ALU.add, [Bb.r])
    self.ring = self.psr[0:4] + self.obank
    identF = self.ident
    bdm = self.s5_mk.t[:, 0:128]
    stgK = [View(self.act.t[:, j, 0:128]) for j in range(2)]
    stgW = [View(self.act.t[:, 2 + 2 * j:4 + 2 * j, :].rearrange("p f n -> p (f n)")) for j in range(2)]
    ki = [0]
    for tau in range(8):
        sp = 7 - tau
        for gb in range(16):
            gh, g0 = gb // 8, (gb % 8) * 8
            P = slice(gh * 64, gh * 64 + 64)
            wre = Wr.t[P, g0:g0 + 8, :].rearrange("p g q -> p (g q)")
            wim = Wi.t[P, g0:g0 + 8, :].rearrange("p g q -> p (g q)")
            cre = Cre.t[P, g0:g0 + 8, :].rearrange("p g q -> p (g q)")
            cni = Cni.t[P, g0:g0 + 8, :].rearrange("p g q -> p (g q)")
            pk = self.psum()
            S.op("tensor", lambda e, pk=pk, wre=wre, cre=cre: e.matmul(pk.t[:, 0:128], wre, cre, start=True, stop=False), reads=[Wr.r, Cre.r], writes=[pk.r])
            S.op("tensor", lambda e, pk=pk, wim=wim, cni=cni: e.matmul(pk.t[:, 0:128], wim, cni, start=False, stop=True), reads=[Wi.r, Cni.r], writes=[pk.r])
            sk = stgK[ki[0] % 2]
            S.op("vector", lambda e, pk=pk, sk=sk: e.tensor_tensor(out=sk.t, in0=pk.t[:, 0:128], in1=bdm, op=ALU.mult), reads=[pk.r, self.s5_mk.r], writes=[sk.r])
            S.dma("sync", self.KBDd[gb][:, tau * 128:(tau + 1) * 128], sk.t, reads=[sk.r])
            sw = stgW[ki[0] % 2]
            ki[0] += 1
            for ri, wsrc in enumerate((wre, wim)):
                pt = self.psum()
                S.op("tensor", lambda e, pt=pt, wsrc=wsrc, P=P: e.transpose(out=pt.t[:, 0:64], in_=wsrc, identity=identF.t[P, P]), reads=[Wr.r, Wi.r, identF.r], writes=[pt.r])
                for g8 in range(8):
                    S.op("vector", lambda e, pt=pt, g8=g8, ri=ri, sw=sw: e.tensor_scalar(out=self.s5_swslice(sw, g8, ri), in0=pt.t[:, 0:64], scalar1=self.s5_mk.t[:, 128 + g8:129 + g8], scalar2=None, op0=ALU.mult),
                         reads=[pt.r, self.s5_mk.r], writes=[sw.r])
            dview = self.WBDd[gb].rearrange("p (g s r n) -> p g s r n", g=8, s=8, r=2)[:, :, sp, :, :]
            S.dma("sync", dview, sw.t[:, 0:1024].rearrange("p (g r n) -> p g r n", g=8, r=2), reads=[sw.r])
        if tau < 7:
            btt(T1, Wr.t[:], bc(A1r), ALU.mult, [Wr.r])
            btt(T2, Wi.t[:], bc(A1i), ALU.mult, [Wi.r])
            btt(T1, T1.t[:], T2.t[:], ALU.subtract, [T2.r])
            btt(T2, Wr.t[:], bc(A1i), ALU.mult, [Wr.r])
            btt(Bb, Wi.t[:], bc(A1r), ALU.mult, [Wi.r])
            btt(Wi, T2.t[:], Bb.t[:], ALU.add, [T2.r, Bb.r])
            S.op("vector", lambda e: e.tensor_copy(out=Wr.t[:], in_=T1.t[:]), reads=[T1.r], writes=[Wr.r])
    barrier(S)
    self.ring = self.psr


def s5_swslice(self, sw, g8, ri):
    o = (g8 * 2 + ri) * 64
    return sw.t[:, o:o + 64]


Net.s5_setup = s5_setup
Net.s5_generate = s5_generate
Net.s5_swslice = s5_swslice


def s5_layer(self):
    S, c = self.S, self.cx
    NS = self.NS
    hs, xn = self.hs, self.xn
    sm = self.s5_small
    A1r, A1i, A8r, A8i, Hcr, Hci = [sm.t[:, j, :] for j in range(6)]
    smr = sm.r
    Cre, Cni = self.s5_Cre, self.s5_Cni
    HBr = View(self.la_S.t[:, 0:4096].rearrange("p (g c) -> p g c", c=64))
    actF = self.act.t[:].rearrange("p f n -> p (f n)")
    HBi = View(actF[:, 0:8192].bitcast(F32).rearrange("p (g c) -> p g c", c=64))
    YH = View(actF[0:64, 8192:16384].rearrange("p (t g q) -> p t g q", t=4, q=16))
    Zl = View(actF[:, 16384:16640].bitcast(F32).rearrange("p (r g) -> p r g", r=2))
    RT = [View(actF[:, 16640 + j * 2048:16640 + (j + 1) * 2048].bitcast(F32).rearrange("p (g c) -> p g c", c=64)) for j in range(2)]
    tq = [View(b.t[:, 0:64]) for b in self.la_t] + [View(b.t[:, 64:128]) for b in self.la_t]
    ybuf = hs
    identb = self.identb
    bcs = lambda a, n: a.rearrange("p (g o) -> p g o", o=1).broadcast_to([128, a.shape[1], n])
    self.ring = self.psr[0:4] + self.obank

    def tile_body(tl):
        N = tl.n
        prompt = tl.kind == "p"
        NCH = 64 if prompt else NS
        self.load_h(tl, True)
        self.rms(hs, N, "norm_mix", 0, out_bf=xn)
        barrier(S)
        if not prompt:
            for ri, HB in enumerate((HBr, HBi)):
                S.dma("sync", HB.t[:, :, 0:NS], self.s5_hin[ri].rearrange("p (g s) -> p g s", s=NS), writes=[HB.r])
        for gb in range(16):
            gh, g0 = gb // 8, (gb % 8) * 8
            P = slice(gh * 64, gh * 64 + 64)
            pz = [self.psum(), self.psum()]
            for half in range(2):
                slot = self.wslot()
                wv = slot.t[:, 0:4096].rearrange("p (g s r n) -> p g s r n", g=4, s=8, r=2)
                S.dma("sync", slot.t[:, 0:4096], self.WBDd[gb][:, half * 4096:(half + 1) * 4096], writes=[slot.r])
                for gl in range(4):
                    j = half * 4 + gl
                    for ri in range(2):
                        sps = range(8) if prompt else [7]
                        for si, sp in enumerate(sps):
                            rhs = xn.t[:, gb, sp:N:8] if prompt else xn.t[:, gb, 0:N]
                            S.op("tensor", lambda e, ri=ri, j=j, gl=gl, sp=sp, si=si, rhs=rhs, wv=wv, P=P, pz=pz, nl=len(sps): e.matmul(pz[ri].t[P, j * NCH:(j + 1) * NCH], wv[:, gl, sp, ri, :], rhs, start=(si == 0), stop=(si == nl - 1)),
                                 reads=[slot.r, xn.r], writes=[pz[ri].r])
            for ri, HB in enumerate((HBr, HBi)):
                src = pz[ri].t[P, 0:8 * NCH].rearrange("p (g c) -> p g c", c=NCH)
                if prompt:
                    S.op("scalar", lambda e, HB=HB, src=src, P=P, g0=g0: e.copy(out=HB.t[P, g0:g0 + 8, 1:64], in_=src[:, :, 0:63]), reads=[pz[ri].r], writes=[HB.r])
                    S.op("scalar", lambda e, src=src, P=P, g0=g0, ri=ri: e.copy(out=Zl.t[P, ri, g0:g0 + 8], in_=src[:, :, 63]), reads=[pz[ri].r], writes=[Zl.r])
                else:
                    S.op("scalar", lambda e, HB=HB, src=src, P=P, g0=g0: e.copy(out=HB.t[P, g0:g0 + 8, NS:2 * NS], in_=src), reads=[pz[ri].r], writes=[HB.r])

        def vtt(out, a, b, op, rd, wr):
            S.op("vector", lambda e: e.tensor_tensor(out=out, in0=a, in1=b, op=op), reads=rd + [smr], writes=wr)
        t1, t2, t3, t4 = tq[0], tq[1], tq[2], tq[3]
        if prompt:
            S.op("vector", lambda e: e.tensor_copy(out=HBr.t[:, :, 0], in_=Hcr), reads=[smr], writes=[HBr.r])
            S.op("vector", lambda e: e.tensor_copy(out=HBi.t[:, :, 0], in_=Hci), reads=[smr], writes=[HBi.r])
            for cch in range(64):
                hr, hi = HBr.t[:, :, cch], HBi.t[:, :, cch]
                if cch < 63:
                    nr, ni, wr_, wi_ = HBr.t[:, :, cch + 1], HBi.t[:, :, cch + 1], [HBr.r], [HBi.r]
                else:
                    nr, ni, wr_, wi_ = Zl.t[:, 0, :], Zl.t[:, 1, :], [Zl.r], [Zl.r]
                vtt(t1.t, A8r, hr, ALU.mult, [HBr.r], [t1.r])
                vtt(t2.t, A8i, hi, ALU.mult, [HBi.r], [t2.r])
                vtt(t1.t, t1.t, t2.t, ALU.subtract, [t2.r], [t1.r])
                vtt(nr, nr, t1.t, ALU.add, [t1.r], wr_)
                vtt(t3.t, A8r, hi, ALU.mult, [HBi.r], [t3.r])
                vtt(t4.t, A8i, hr, ALU.mult, [HBr.r], [t4.r])
                vtt(t3.t, t3.t, t4.t, ALU.add, [t4.r], [t3.r])
                vtt(ni, ni, t3.t, ALU.add, [t3.r], wi_)
            S.op("vector", lambda e: e.tensor_copy(out=sm.t[:, 4:6, :], in_=Zl.t[:]), reads=[Zl.r], writes=[smr])
            if tl.idx == self.NPT - 1:
                S.dma("sync", self.s5_hp_o.rearrange("r p g -> p r g"), sm.t[:, 4:6, :], reads=[smr])
            hsl = slice(0, 64)
            nq, tcount = 8, 8
        else:
            a, b, o = slice(0, NS), slice(NS, 2 * NS), slice(2 * NS, 3 * NS)
            R0, R1 = RT[0], RT[1]
            r0 = View(self.la_f[0].t[:].rearrange("p k n -> p (k n)")[:, 0:64 * NS].rearrange("p (g s) -> p g s", s=NS))
            r1 = View(self.la_f[1].t[:].rearrange("p k n -> p (k n)")[:, 0:64 * NS].rearrange("p (g s) -> p g s", s=NS))
            vtt(r0.t, HBr.t[:, :, a], bcs(A1r, NS), ALU.mult, [HBr.r], [r0.r])
            vtt(r1.t, HBi.t[:, :, a], bcs(A1i, NS), ALU.mult, [HBi.r], [r1.r])
            vtt(r0.t, r0.t, r1.t, ALU.subtract, [r1.r], [r0.r])
            vtt(HBr.t[:, :, o], r0.t, HBr.t[:, :, b], ALU.add, [r0.r], [HBr.r])
            vtt(r0.t, HBi.t[:, :, a], bcs(A1r, NS), ALU.mult, [HBi.r], [r0.r])
            vtt(r1.t, HBr.t[:, :, a], bcs(A1i, NS), ALU.mult, [HBr.r], [r1.r])
            vtt(r0.t, r0.t, r1.t, ALU.add, [r1.r], [r0.r])
            vtt(HBi.t[:, :, o], r0.t, HBi.t[:, :, b], ALU.add, [r0.r], [HBi.r])
            for ri, HB in enumerate((HBr, HBi)):
                S.dma("sync", self.s5_hs_o[ri].rearrange("p (g s) -> p g s", s=NS), HB.t[:, :, o], reads=[HB.r])
            hsl = o
            nq, tcount = 0, 1
        for th in range((tcount + 3) // 4):
            tps = list(range(th * 4, min(tcount, th * 4 + 4)))
            for tp in tps:
                if prompt:
                    for gq in range(4):
                        gs = slice(gq * 16, gq * 16 + 16)
                        R0, R1 = RT[0], RT[1]
                        xr_, xi_ = HBr.t[:, gs, :], HBi.t[:, gs, :]
                        ar, ai = bcs(A1r[:, gs], 64), bcs(A1i[:, gs], 64)
                        vtt(R0.t, xr_, ar, ALU.mult, [HBr.r], [R0.r])
                        vtt(R1.t, xi_, ai, ALU.mult, [HBi.r], [R1.r])
                        vtt(R0.t, R0.t, R1.t, ALU.subtract, [R1.r], [R0.r])
                        vtt(R1.t, xr_, ai, ALU.mult, [HBr.r], [R1.r])
                        vtt(xi_, xi_, ar, ALU.mult, [], [HBi.r])
                        vtt(xi_, xi_, R1.t, ALU.add, [R1.r], [HBi.r])
                        S.op("vector", lambda e, xr_=xr_, R0=R0: e.tensor_copy(out=xr_, in_=R0.t), reads=[R0.r], writes=[HBr.r])
                for gh in range(2):
                    P = slice(gh * 64, gh * 64 + 64)
                    for blk in range(2):
                        ph = self.psum()
                        for gi in range(32):
                            gl = blk * 32 + gi
                            S.op("tensor", lambda e, ph=ph, gi=gi, gl=gl, P=P: e.matmul(ph.t[0:NCH, gi * 16:(gi + 1) * 16], HBr.t[P, gl, hsl], Cre.t[P, gl, :], start=True, stop=False), reads=[HBr.r, Cre.r], writes=[ph.r])
                            S.op("tensor", lambda e, ph=ph, gi=gi, gl=gl, P=P: e.matmul(ph.t[0:NCH, gi * 16:(gi + 1) * 16], HBi.t[P, gl, hsl], Cni.t[P, gl, :], start=False, stop=True), reads=[HBi.r, Cni.r], writes=[ph.r])
                        gbase = gh * 64 + blk * 32
                        S.op("scalar", lambda e, ph=ph, gbase=gbase, tp=tp: e.copy(out=YH.t[0:NCH, tp % 4, gbase:gbase + 32, :], in_=ph.t[0:NCH, 0:512].rearrange("p (g q) -> p g q", q=16)), reads=[ph.r], writes=[YH.r])
            for gb in range(16):
                if prompt:
                    kslot = self.wslot()
                    kv = kslot.t[:, 0:1024].rearrange("p (t m) -> p t m", t=8)
                    S.dma("sync", kslot.t[:, 0:1024], self.KBDd[gb], writes=[kslot.r])
                for tp in tps:
                    py = self.psum()
                    if prompt:
                        for sp in range(tp + 1):
                            S.op("tensor", lambda e, py=py, kv=kv, tp=tp, sp=sp, gb=gb: e.matmul(py.t[:, 0:64], kv[:, tp - sp, :], xn.t[:, gb, sp:N:8], start=(sp == 0), stop=False), reads=[kslot.r, xn.r], writes=[py.r])
                    S.op("tensor", lambda e, py=py, tp=tp, gb=gb: e.matmul(py.t[:, 0:NCH], YH.t[0:NCH, tp % 4, gb * 8:(gb + 1) * 8, :].rearrange("p g q -> p (g q)"), identb.t[0:NCH, 0:NCH], start=(not prompt), stop=True),
                         reads=[YH.r, identb.r], writes=[py.r])
                    ysl = ybuf.t[:, gb, tp:N:8] if prompt else ybuf.t[:, gb, 0:N]
                    xsl = xn.t[:, gb, tp:N:8] if prompt else xn.t[:, gb, 0:N]
                    S.op("vector", lambda e, py=py, ysl=ysl, xsl=xsl, gb=gb: e.scalar_tensor_tensor(out=ysl, in0=xsl, scalar=self.pcol("s5_d", gb), in1=py.t[:, 0:NCH], op0=ALU.mult, op1=ALU.add),
                         reads=[py.r, xn.r, self.pv.r], writes=[ybuf.r])
        if self.cfg.get("dbg") and not prompt:
            dbg = self.cx.outp("dbg_y", [128, DC * NS])
            S.dma("sync", dbg.rearrange("p (c s) -> p c s", c=DC), ybuf.t[:, :, 0:N], reads=[ybuf.r])
        for cc_ in range(DC):
            S.op("scalar", lambda e, cc_=cc_: e.activation(out=xn.t[:, cc_, 0:N], in_=ybuf.t[:, cc_, 0:N], func=AF.Gelu_apprx_tanh), reads=[ybuf.r], writes=[xn.r])
        barrier(S)
        self.load_h(tl, True)
        zin = lambda k: (xn.t[:, k, 0:N], [xn.r])
        g_t = tq[4]
        gt = View(self.la_f[2].t[:, 0, :])

        def cons_glu(mc, msz, pb):
            S.op("scalar", lambda e: e.activation(out=gt.t[:, 0:N], in_=pb.t[:, 0:N], func=AF.Sigmoid), reads=[pb.r], writes=[gt.r])
            S.op("vector", lambda e: e.tensor_tensor(out=gt.t[:, 0:N], in0=gt.t[:, 0:N], in1=xn.t[:, mc, 0:N], op=ALU.mult), reads=[xn.r], writes=[gt.r])
            S.op("vector", lambda e: e.tensor_tensor(out=hs.t[:, mc, 0:N], in0=hs.t[:, mc, 0:N], in1=gt.t[:, 0:N], op=ALU.add), reads=[gt.r], writes=[hs.r])
        self.proj(self.s5_wglu, D, D, zin, N, cons_glu, wname="s5_glu")
        self.store_h(tl)
        barrier(S)
    for tl in self.tiles:
        tile_body(tl)
    self.ring = self.psr


Net.s5_layer = s5_layer
```

```python
import numpy as np
from contextlib import ExitStack
import concourse.bass as bass
import concourse.mybir as mybir
from concourse.bass_utils import run_bass_kernel_spmd

F32 = mybir.dt.float32
BF16 = mybir.dt.bfloat16
AF = mybir.ActivationFunctionType
ALU = mybir.AluOpType
AX = mybir.AxisListType

D = 2048
DC = 16
DFF = 5632
FC = 44
EPS = 1e-6


class R:
    __slots__ = ("w", "rd")

    def __init__(self):
        self.w = None
        self.rd = []


class Sched:
    COMPUTE = ("tensor", "vector", "scalar", "gpsimd")
    NDMA = 24

    def __init__(self, nc, es, dma_queues=("sync", "gpsimd")):
        self.nc = nc
        self.ops = {e: [] for e in ("tensor", "vector", "scalar", "gpsimd", "sync")}
        self.sems = {}
        self.cnt = {}
        for e in self.COMPUTE:
            self.sems[e] = es.enter_context(nc.semaphore("s_" + e))
            self.cnt[e] = 0
        self.dq = {}
        for q in dma_queues:
            lst = []
            for j in range(self.NDMA):
                k = "d_%s_%d" % (q, j)
                self.sems[k] = es.enter_context(nc.semaphore(k))
                self.cnt[k] = 0
                lst.append(k)
            self.dq[q] = [lst, 0]
        self.waited = {e: {} for e in self.ops}

    def _need(self, eng, ev, waits):
        if ev is None:
            return
        k, v = ev
        if k == eng and eng == "tensor":
            return
        if self.waited[eng].get(k, 0) >= v:
            return
        self.waited[eng][k] = v
        waits.append((k, v))

    def _deps(self, eng, reads, writes):
        waits = []
        for r in reads:
            self._need(eng, r.w, waits)
        for r in writes:
            self._need(eng, r.w, waits)
            for ev in r.rd:
                self._need(eng, ev, waits)
        return waits

    def _commit(self, ev, reads, writes):
        for r in reads:
            r.rd.append(ev)
            if len(r.rd) > 64:
                best = {}
                for k, v in r.rd:
                    if best.get(k, 0) < v:
                        best[k] = v
                r.rd = list(best.items())
        for r in writes:
            r.w = ev
            r.rd = []

    def op(self, eng, fn, reads=(), writes=()):
        waits = self._deps(eng, reads, writes)
        self.cnt[eng] += 1
        ev = (eng, self.cnt[eng])
        self.ops[eng].append((fn, waits, (eng, 1)))
        self._commit(ev, reads, writes)

    def dma(self, q, out, in_, reads=(), writes=()):
        lst, i = self.dq[q]
        k = lst[i % self.NDMA]
        self.dq[q][1] = i + 1
        waits = self._deps(q, reads, writes)
        if self.cnt[k] > 0:
            self._need(q, (k, self.cnt[k]), waits)
        self.cnt[k] += 16
        ev = (k, self.cnt[k])
        self.ops[q].append((lambda e: e.dma_start(out=out, in_=in_), waits, (k, 16)))
        self._commit(ev, reads, writes)

    def finish(self):
        for q in self.dq:
            for k in self.dq[q][0]:
                if self.cnt[k]:
                    self.ops["sync"].append((None, [(k, self.cnt[k])], None))

    def emit(self):
        nc = self.nc
        with nc.Block() as block:
            def mk(name):
                def body(eng):
                    for fn, waits, inc in self.ops[name]:
                        for k, v in waits:
                            eng.wait_ge(self.sems[k], v)
                        if fn is not None:
                            ins = fn(eng)
                            ins.then_inc(self.sems[inc[0]], inc[1])
                return body
            block.sync(mk("sync"))
            block.tensor(mk("tensor"))
            block.vector(mk("vector"))
            block.scalar(mk("scalar"))
            block.gpsimd(mk("gpsimd"))


class Buf:
    def __init__(self, t):
        self.t = t
        self.r = R()


class View:
    def __init__(self, ap):
        self.t = ap
        self.r = R()


class Ctx:
    def __init__(self, nc, es):
        self.nc = nc
        self.es = es
        self.S = Sched(nc, es)
        self.n = 0
        self.din = {}
        self.dout = {}

    def sb(self, shape, dt=F32):
        self.n += 1
        return Buf(self.es.enter_context(self.nc.sbuf_tensor("sb%d" % self.n, list(shape), dt)))

    def ps(self, shape=(128, 512), dt=F32):
        self.n += 1
        return Buf(self.es.enter_context(self.nc.psum_tensor("ps%d" % self.n, list(shape), dt)))

    def inp(self, name, shape):
        t = self.nc.dram_tensor(name, list(shape), F32, kind="ExternalInput").ap()
        self.din[name] = t
        return t

    def outp(self, name, shape):
        t = self.nc.dram_tensor(name, list(shape), F32, kind="ExternalOutput").ap()
        self.dout[name] = t
        return t

    def scratch(self, name, shape, dt=F32):
        return self.nc.dram_tensor(name, list(shape), dt, kind="Internal").ap()


def rs(bufs):
    return [b.r for b in bufs]


class Tile:
    def __init__(self, c0, n, kind, idx):
        self.c0, self.n, self.kind, self.idx = c0, n, kind, idx


class Net:
    def __init__(self, cx, cfg):
        self.cx = cx
        self.S = cx.S
        self.cfg = cfg
        TP, NS = cfg["TP"], cfg["NS"]
        self.TP, self.NS = TP, NS
        self.NT = TP + NS
        self.tiles = [Tile(i * 512, 512, "p", i) for i in range(TP // 512)] + [Tile(TP, NS, "s", TP // 512)]
        self.NPT = TP // 512
        c = cx
        self.hs = c.sb([128, DC, 512])
        self.xn = c.sb([128, DC, 512], BF16)
        self.NW = 3
        self.wring = [c.sb([128, 4096], BF16) for _ in range(self.NW)]
        self.wi = 0
        self.wcache = {}
        self.wcn = 0
        self.psr = [c.ps() for _ in range(4)]
        self.ring = self.psr
        self.pi = 0
        self.zeros = c.sb([128, 16])
        self.S.op("vector", lambda e: e.memset(self.zeros.t[:], 0.0), writes=[self.zeros.r])
        self.sq = [c.sb([128, 512]) for _ in range(2)]
        self.rstd = c.sb([128, 512])
        self.ones = c.sb([128, 128])
        self.ident = c.sb([128, 128])
        self.pv_cols = cfg["pv_cols"]
        self.pv = c.sb([128, cfg["pv_n"]])
        pv_d = c.inp("pvec", [128, cfg["pv_n"]])
        ident_d = c.inp("ident", [128, 128])
        self.S.op("vector", lambda e: e.memset(self.ones.t[:], 1.0), writes=[self.ones.r])
        self.S.dma("sync", self.pv.t[:], pv_d, writes=[self.pv.r])
        self.S.dma("sync", self.ident.t[:], ident_d, writes=[self.ident.r])
        self.act = c.sb([128, FC, 512], BF16)
        self.la_f = [c.sb([128, 2, 512]) for _ in range(5)]
        self.hT = c.scratch("hT", [D, self.NT])
        self.xT = c.inp("xT", [D, self.NT])
        self.hr = [R() for _ in self.tiles]

    def pcol(self, name, j=0, n=1):
        o = self.pv_cols[name] + j
        return self.pv.t[:, o:o + n]

    def psum(self):
        b = self.ring[self.pi % len(self.ring)]
        self.pi += 1
        return b

    def pbf(self, pb):
        return pb.t[:].bitcast(BF16)

    def zero_ap(self, N):
        return self.zeros.t[:, 0:N]

    def wslot(self):
        b = self.wring[self.wi % self.NW]
        self.wi += 1
        return b

    def proj(self, w, K, M, xin, N, consume, mw=256, wname=None):
        S = self.S
        cache = None
        if wname is not None:
            if wname not in self.wcache:
                self.wcache[wname] = {"first": True, "blocks": []}
            cache = self.wcache[wname]
        bi = 0
        kp = min(K, 128)
        kc = K // kp
        mw = min(mw, M)
        kg = max(1, min(kc, 4096 // mw))
        wv = w.rearrange("(c p) m -> p c m", p=kp)
        for m0 in range(0, M, mw):
            mcs = [(m0 + j, min(128, M - m0 - j)) for j in range(0, min(mw, M - m0), 128)]
            pbs = [self.psum() for _ in mcs]
            cw = min(mw, M - m0)
            for k0 in range(0, kc, kg):
                g = min(kg, kc - k0)
                slot = self.wslot()
                sv = slot.t[0:kp, 0:g * cw].rearrange("p (c m) -> p c m", c=g)
                if cache is None:
                    S.dma("gpsimd", sv, wv[:, k0:k0 + g, m0:m0 + cw], writes=[slot.r])
                elif cache["first"]:
                    self.wcn += 1
                    scr = self.cx.scratch("wc%d" % self.wcn, [128, 4096], BF16)
                    br = R()
                    cache["blocks"].append((scr, br))
                    S.dma("gpsimd", sv, wv[:, k0:k0 + g, m0:m0 + cw], writes=[slot.r])
                    S.dma("sync", scr[0:kp, 0:g * cw], slot.t[0:kp, 0:g * cw], reads=[slot.r], writes=[br])
                else:
                    scr, br = cache["blocks"][bi]
                    S.dma("sync", slot.t[0:kp, 0:g * cw], scr[0:kp, 0:g * cw], reads=[br], writes=[slot.r])
                bi += 1
                for (ms, msz), pb in zip(mcs, pbs):
                    for kk in range(g):
                        k = k0 + kk
                        xa, xr = xin(k)
                        S.op("tensor", lambda e, pb=pb, sv=sv, kk=kk, ms=ms, msz=msz, xa=xa, k=k, m0=m0:
                             e.matmul(pb.t[0:msz, 0:N], sv[:, kk, ms - m0:ms - m0 + msz], xa, start=(k == 0), stop=(k == kc - 1)),
                             reads=[slot.r] + xr, writes=[pb.r])
            for (ms, msz), pb in zip(mcs, pbs):
                consume(ms // 128, msz, pb)
        if cache is not None:
            cache["first"] = False

    def load_h(self, tl, first):
        src = self.xT if first else self.hT
        self.S.dma("sync", self.hs.t[:, :, 0:tl.n], src[:, tl.c0:tl.c0 + tl.n].rearrange("(c p) n -> p c n", p=128),
                   reads=[self.hr[tl.idx]], writes=[self.hs.r])

    def store_h(self, tl):
        self.S.dma("sync", self.hT[:, tl.c0:tl.c0 + tl.n].rearrange("(c p) n -> p c n", p=128), self.hs.t[:, :, 0:tl.n],
                   reads=[self.hs.r], writes=[self.hr[tl.idx]])

    def rms(self, src, N, gname, gj, out_bf=None, out_f32=None):
        S = self.S
        pb = self.psum()
        for c in range(DC):
            q = self.sq[c % 2]
            S.op("scalar", lambda e, q=q, c=c: e.activation(out=q.t[:, 0:N], in_=src.t[:, c, 0:N], func=AF.Square),
                 reads=[src.r], writes=[q.r])
            S.op("tensor", lambda e, q=q, c=c: e.matmul(pb.t[:, 0:N], self.ones.t[:], q.t[:, 0:N], start=(c == 0), stop=(c == DC - 1)),
                 reads=[q.r, self.ones.r], writes=[pb.r])
        S.op("scalar", lambda e: e.activation(out=self.rstd.t[:, 0:N], in_=pb.t[:, 0:N], func=AF.Sqrt, bias=self.eps_ap(), scale=1.0 / D),
             reads=[pb.r, self.pv.r], writes=[self.rstd.r])
        S.op("vector", lambda e: e.reciprocal(out=self.rstd.t[:, 0:N], in_=self.rstd.t[:, 0:N]), writes=[self.rstd.r])
        for c in range(DC):
            for o in (out_bf, out_f32):
                if o is None:
                    continue
                S.op("vector", lambda e, c=c, o=o: e.scalar_tensor_tensor(
                    out=o.t[:, c, 0:N], in0=src.t[:, c, 0:N], scalar=self.pcol(gname, gj * DC + c), in1=self.rstd.t[:, 0:N],
                    op0=ALU.mult, op1=ALU.mult), reads=[src.r, self.rstd.r, self.pv.r], writes=[o.r])

    def eps_ap(self):
        return self.pcol("eps")

    def ffn_setup(self):
        c = self.cx
        flat = lambda b: View(b.t[:].rearrange("p k n -> p (k n)"))
        self.ubuf = [flat(self.la_f[0]), flat(self.la_f[1])]
        self.cbuf = [flat(self.la_f[2]), flat(self.la_f[3])]
        self.gbuf = [flat(self.la_f[4]), c.sb([128, 512])]
        self.halo = c.sb([128, FC, 2])
        self.cprev = c.sb([128, FC, self.NS, 2])
        L = self.cfg["DEPTH"]
        self.w_up = c.inp("ffn_w_up", [L, D, DFF])
        self.w_gate = c.inp("ffn_w_gate", [L, D, DFF])
        self.w_down = c.inp("ffn_w_down", [L, DFF, D])
        self.conv_in = c.inp("conv_s", [L, 128, FC * self.NS * 2])
        self.conv_p = c.outp("conv_p_o", [L, 128, FC * 2])
        self.conv_so = c.outp("conv_s_o", [L, 128, FC * self.NS * 2])
        self.ui = 0

    def ffn(self, li, first=False):
        S = self.S
        S.op("vector", lambda e: e.memset(self.halo.t[:], 0.0), writes=[self.halo.r])
        S.dma("sync", self.cprev.t[:].rearrange("p f s t -> p (f s t)"), self.conv_in[li], writes=[self.cprev.r])
        self.ring = self.psr + self.obank
        for tl in self.tiles:
            N = tl.n
            self.load_h(tl, first)
            self.rms(self.hs, N, "norm_ffn", li, out_bf=self.xn)
            xin = lambda k: (self.xn.t[:, k, 0:N], [self.xn.r])
            for f0 in range(0, FC, 2):
                ups = {}

                def cons_up(mc, msz, pb, ups=ups):
                    ups[mc] = pb
                self.proj(self.w_up[li][:, f0 * 128:(f0 + 2) * 128], D, 256, xin, N, cons_up, wname="up%d_%d" % (li, f0))

                def cons_gate(mc, msz, pb, ups=ups, f0=f0, tl=tl, N=N):
                    f = f0 + mc
                    pu = ups[mc]
                    ub = self.ubuf[self.ui % 2]
                    cb = self.cbuf[self.ui % 2]
                    gb = self.gbuf[self.ui % 2]
                    self.ui += 1
                    wc = lambda j: self.pcol("ffn_w_conv", (li * 3 + j) * FC + f)
                    bc = self.pcol("ffn_b_conv", li * FC + f)
                    if tl.kind == "p":
                        S.op("scalar", lambda e: e.copy(out=ub.t[:, 0:2], in_=self.halo.t[:, f, :]), reads=[self.halo.r], writes=[ub.r])
                        S.op("scalar", lambda e: e.copy(out=ub.t[:, 2:2 + N], in_=pu.t[:, 0:N]), reads=[pu.r], writes=[ub.r])
                        S.op("vector", lambda e: e.tensor_scalar(out=cb.t[:, 0:N], in0=ub.t[:, 2:2 + N], scalar1=wc(2), scalar2=bc, op0=ALU.mult, op1=ALU.add),
                             reads=[ub.r, self.pv.r], writes=[cb.r])
                        for j in (1, 0):
                            S.op("vector", lambda e, j=j: e.scalar_tensor_tensor(out=cb.t[:, 0:N], in0=ub.t[:, j:j + N], scalar=wc(j), in1=cb.t[:, 0:N], op0=ALU.mult, op1=ALU.add),
                                 reads=[ub.r, self.pv.r], writes=[cb.r])
                        S.op("scalar", lambda e: e.copy(out=self.halo.t[:, f, :], in_=ub.t[:, N:N + 2]), reads=[ub.r], writes=[self.halo.r])
                    else:
                        S.op("scalar", lambda e: e.copy(out=ub.t[:, 0:N], in_=pu.t[:, 0:N]), reads=[pu.r], writes=[ub.r])
                        S.op("vector", lambda e: e.tensor_scalar(out=cb.t[:, 0:N], in0=ub.t[:, 0:N], scalar1=wc(2), scalar2=bc, op0=ALU.mult, op1=ALU.add),
                             reads=[ub.r, self.pv.r], writes=[cb.r])
                        for j in (1, 0):
                            S.op("vector", lambda e, j=j: e.scalar_tensor_tensor(out=cb.t[:, 0:N], in0=self.cprev.t[:, f, :, j], scalar=wc(j), in1=cb.t[:, 0:N], op0=ALU.mult, op1=ALU.add),
                                 reads=[self.cprev.r, self.pv.r], writes=[cb.r])
                        S.op("scalar", lambda e: e.copy(out=self.cprev.t[:, f, :, 0], in_=self.cprev.t[:, f, :, 1]), reads=[cb.r], writes=[self.cprev.r])
                        S.op("scalar", lambda e: e.copy(out=self.cprev.t[:, f, :, 1], in_=ub.t[:, 0:N]), reads=[ub.r], writes=[self.cprev.r])
                    S.op("scalar", lambda e: e.activation(out=gb.t[:, 0:N], in_=cb.t[:, 0:N], func=AF.Gelu_apprx_tanh), reads=[cb.r], writes=[gb.r])
                    S.op("vector", lambda e: e.tensor_tensor(out=self.act.t[:, f, 0:N], in0=gb.t[:, 0:N], in1=pb.t[:, 0:N], op=ALU.mult),
                         reads=[gb.r, pb.r], writes=[self.act.r])
                self.proj(self.w_gate[li][:, f0 * 128:(f0 + 2) * 128], D, 256, xin, N, cons_gate, wname="gate%d_%d" % (li, f0))
            if tl.kind == "p" and tl.idx == self.NPT - 1:
                S.dma("sync", self.conv_p[li], self.halo.t[:].rearrange("p f t -> p (f t)"), reads=[self.halo.r])
            if tl.kind == "s":
                S.dma("sync", self.conv_so[li], self.cprev.t[:].rearrange("p f s t -> p (f s t)"), reads=[self.cprev.r])
            ain = lambda k: (self.act.t[:, k, 0:N], [self.act.r])

            def cons_down(mc, msz, pb, N=N):
                S.op("vector", lambda e: e.tensor_tensor(out=self.hs.t[:, mc, 0:N], in0=self.hs.t[:, mc, 0:N], in1=pb.t[:, 0:N], op=ALU.add),
                     reads=[pb.r], writes=[self.hs.r])
            self.proj(self.w_down[li], DFF, D, ain, N, cons_down, wname="down%d" % li)
            self.store_h(tl)
        self.ring = self.psr

    def final(self):
        yT = self.cx.outp("yT", [D, self.NT])
        for tl in self.tiles:
            N = tl.n
            self.load_h(tl, False)
            self.rms(self.hs, N, "norm_final", 0, out_f32=self.hs)
            self.S.dma("sync", yT[:, tl.c0:tl.c0 + N].rearrange("(c p) n -> p c n", p=128), self.hs.t[:, :, 0:N], reads=[self.hs.r])


def fm(a):
    a = np.asarray(a, np.float32)
    if a.ndim == 1:
        a = a[None]
    r, f = a.shape
    return np.ascontiguousarray(a.reshape(r, f // 128, 128).transpose(2, 0, 1).reshape(128, -1))


PV_SPEC = [("eps", None), ("one", None), ("neghalf", None), ("lneps", None), ("halfpi", None), ("s5_d", "s5_d"), ("rw_mix", "rw_mix"), ("rw_w0", "rw_w0"), ("rw_a0", "rw_a0"),
           ("rw_k_k", "rw_k_k"), ("rw_k_a", "rw_k_a"), ("rw_r_k", "rw_r_k"), ("rw_ln_w", "rw_ln_w"), ("rw_ln_b", "rw_ln_b"), ("gla_b_gk", "gla_b_gk"), ("gla_norm", "gla_norm"), ("hg_norm", "hg_norm"), ("hg_lb", "hg_lb"),
           ("norm_mix", "norm_mix"), ("norm_ffn", "norm_ffn"), ("norm_final", "norm_final"),
           ("ffn_w_conv", "ffn_w_conv"), ("ffn_b_conv", "ffn_b_conv")]


def s5_lay(a):
    return np.ascontiguousarray(np.asarray(a, np.float32).reshape(2, 64, 64).transpose(0, 2, 1).reshape(128, 64))


def s5_inputs(inp):
    par = np.zeros((128, 320), np.float32)
    par[:, 0:64] = s5_lay(inp["s5_lambda_re"])
    par[:, 64:128] = s5_lay(inp["s5_lambda_im"])
    par[:, 128:192] = s5_lay(np.broadcast_to(np.asarray(inp["s5_log_dt"], np.float32)[:, None], (128, 64)))
    lb = lambda b: np.asarray(b, np.float32).reshape(2, 64, 64, 16).transpose(0, 2, 1, 3).reshape(128, 1024)
    lc = lambda c_: np.asarray(c_, np.float32).reshape(2, 64, 16, 64).transpose(0, 3, 1, 2).reshape(128, 1024)
    msk = np.zeros((128, 136), np.float32)
    for g8 in range(8):
        msk[g8 * 16:(g8 + 1) * 16, g8 * 16:(g8 + 1) * 16] = 1.0
        msk[g8 * 16:(g8 + 1) * 16, 128 + g8] = 1.0
    return {"s5_par": par, "s5_B": np.ascontiguousarray(np.stack([lb(inp["s5_b_re"]), lb(inp["s5_b_im"])])),
            "s5_C": np.ascontiguousarray(np.stack([lc(inp["s5_c_re"]), lc(inp["s5_c_im"])])), "s5_msk": msk,
            "s5_w_glu": np.ascontiguousarray(inp["s5_w_glu"], dtype=np.float32)}


def s5_state_in(re, im, ns):
    f = lambda a: np.asarray(a, np.float32).reshape(ns, 2, 64, 64).transpose(1, 3, 2, 0).reshape(128, 64 * ns)
    return np.ascontiguousarray(np.stack([f(re), f(im)]))


def s5_state_out_p(o):
    f = lambda a: a.reshape(2, 64, 64).transpose(0, 2, 1).reshape(128, 64)
    return f(o[0]), f(o[1])


def s5_state_out_s(o, ns):
    f = lambda a: a.reshape(2, 64, 64, ns).transpose(3, 0, 2, 1).reshape(ns, 128, 64)
    return f(o[0]), f(o[1])


def rw_consts():
    su = np.triu(np.ones((64, 64), np.float32), 1)
    ui = np.triu(np.ones((64, 64), np.float32), 0)
    sl_ = np.tril(np.ones((64, 64), np.float32), -1)
    c = np.zeros((128, 1216), np.float32)
    c[0:64, 0:64] = 1.0
    c[64:128, 64:128] = 1.0
    c[:, 128:192] = np.concatenate([np.eye(64), np.eye(64)], 0)
    c[0:64, 192:704] = np.tile(np.concatenate([su, ui], 1), (1, 4))
    c[0:64, 704:1216] = np.tile(np.concatenate([sl_, sl_], 1), (1, 4))
    return c


PV_NCOLS = {"s5_d": 16, "rw_mix": 96, "rw_w0": 16, "rw_a0": 16, "rw_k_k": 16, "rw_k_a": 16, "rw_r_k": 16, "rw_ln_w": 16, "rw_ln_b": 16,
            "gla_b_gk": 8, "gla_norm": 4, "hg_norm": 1, "hg_lb": 64, "norm_mix": 64, "norm_ffn": 64, "norm_final": 16,
            "ffn_w_conv": 528, "ffn_b_conv": 176}


def pack_pvec(inp):
    cols = {}
    parts = []
    o = 0
    for name, key in PV_SPEC:
        if name == "eps":
            a = np.full((128, 1), EPS, np.float32)
        elif name == "one":
            a = np.full((128, 1), 1.0, np.float32)
        elif name == "neghalf":
            a = np.full((128, 1), -0.5, np.float32)
        elif name == "lneps":
            a = np.full((128, 1), 64e-5, np.float32)
        elif name == "halfpi":
            a = np.full((128, 1), np.pi / 2, np.float32)
        elif name == "rw_r_k" and key in inp:
            a = fm(np.asarray(inp[key], np.float32).reshape(1, -1))
        elif key not in inp:
            a = np.zeros((128, PV_NCOLS[name]), np.float32)
        else:
            v = np.asarray(inp[key], np.float32)
            a = fm(v.reshape(-1, v.shape[-1]))
        cols[name] = o
        o += a.shape[1]
        parts.append(a)
    return cols, np.ascontiguousarray(np.concatenate(parts, axis=1))


def la_setup(self):
    c = self.cx
    self.la_qe = View(self.act.t[:, 36:38, :])
    self.la_ke = View(self.act.t[:, 38:40, :])
    self.la_vh = View(self.act.t[:, 32:36, :])
    self.la_VT = View(self.act.t[0:64, 16:32, :])
    self.la_KET = [c.sb([64, 2, 128], BF16) for _ in range(2)]
    self.la_att = [c.sb([64, 64], BF16) for _ in range(2)]
    self.la_S = c.sb([128, 4096])
    self.la_Sbf = c.sb([128, 4096], BF16)
    self.la_Ss = c.sb([128, 1024])
    self.la_Ssbf = c.sb([128, 1024], BF16)
    self.la_t = [c.sb([128, 512]) for _ in range(3)]
    self.la_og = View(self.act.t[:, 0:DC, :])
    self.la_t1 = c.sb([16, 512], BF16)
    self.la_lb = c.sb([128, 2 * DC])
    self.la_e = c.sb([128, 5 * DC])
    self.identb = c.sb([128, 128], BF16)
    self.masks = c.sb([64, 64])
    self.rmask = c.sb([128, 512])
    md = c.inp("maskT", [64, 64])
    rd = c.inp("rmask", [128, 512])
    S = self.S
    S.dma("sync", self.masks.t[:, 0:64], md, writes=[self.masks.r])
    S.dma("sync", self.rmask.t[:], rd, writes=[self.rmask.r])
    S.op("vector", lambda e: e.tensor_copy(out=self.identb.t[:], in_=self.ident.t[:]), reads=[self.ident.r], writes=[self.identb.r])
    self.obank = [c.ps() for _ in range(4)]


def la_layer(self, kind, first=False):
    S = self.S
    c = self.cx
    if kind == "gla":
        H, kcn, vcn, li = 4, 2, 4, 2
        wq, wk, wv = c.inp("gla_w_q", [D, 1024]), c.inp("gla_w_k", [D, 1024]), c.inp("gla_w_v", [D, D])
        wg1, wg2 = c.inp("gla_w_gk1", [D, 16]), c.inp("gla_w_gk2", [16, 1024])
        wg, wo = c.inp("gla_w_g", [D, D]), c.inp("gla_w_o", [D, D])
        st_in = c.inp("gla_s", [self.NS, H, 128, kcn * 512])
        st_p = c.outp("gla_p_o", [H, 128, kcn * 512])
        st_s = c.outp("gla_s_o", [self.NS, H, 128, kcn * 512])
        nname = "gla_norm"
    else:
        H, kcn, vcn, li = 16, 1, 1, 3
        wq, wk, wv = c.inp("hg_w_q", [D, D]), c.inp("hg_w_f", [D, D]), c.inp("hg_w_i", [D, D])
        wg, wo = c.inp("hg_w_g", [D, D]), c.inp("hg_w_o", [D, D])
        st_in = c.inp("hg_s", [self.NS, H, 128, 128])
        st_p = c.outp("hg_p_o", [H, 128, 128])
        st_s = c.outp("hg_s_o", [self.NS, H, 128, 128])
        nname = "hg_norm"
        E = self.la_e
        for j in range(4):
            S.op("scalar", lambda e, j=j: e.activation(out=E.t[:, j * DC:(j + 1) * DC], in_=self.pcol("hg_lb", j * DC, DC), func=AF.Exp),
                 reads=[self.pv.r], writes=[E.r])
        S.op("vector", lambda e: e.tensor_tensor(out=E.t[:, 4 * DC:5 * DC], in0=E.t[:, 0:DC], in1=E.t[:, DC:2 * DC], op=ALU.add), writes=[E.r])
        for j in (2, 3):
            S.op("vector", lambda e, j=j: e.tensor_tensor(out=E.t[:, 4 * DC:5 * DC], in0=E.t[:, 4 * DC:5 * DC], in1=E.t[:, j * DC:(j + 1) * DC], op=ALU.add), writes=[E.r])
        S.op("vector", lambda e: e.reciprocal(out=E.t[:, 4 * DC:5 * DC], in_=E.t[:, 4 * DC:5 * DC]), writes=[E.r])
        S.op("vector", lambda e: e.tensor_tensor(out=self.la_lb.t[:, DC:2 * DC], in0=E.t[:, 0:DC], in1=E.t[:, 4 * DC:5 * DC], op=ALU.mult), reads=[E.r], writes=[self.la_lb.r])
        S.op("vector", lambda e: e.tensor_scalar(out=self.la_lb.t[:, 0:DC], in0=self.la_lb.t[:, DC:2 * DC], scalar1=-1.0, scalar2=1.0, op0=ALU.mult, op1=ALU.add), writes=[self.la_lb.r])
    dk, dv = kcn * 128, vcn * 128
    qf, kf, lg, cum, eg = self.la_f
    qe, ke, vh, VT = self.la_qe, self.la_ke, self.la_vh, self.la_VT
    t0, t1b, t2 = self.la_t
    og = self.la_og
    Sst, Sbf = self.la_S, self.la_Sbf
    S.op("vector", lambda e: e.memset(Sst.t[:], 0.0), writes=[Sst.r])
    S.op("vector", lambda e: e.memset(Sbf.t[:], 0.0), writes=[Sbf.r])
    self.ring = self.psr[0:4]
    def tile_body(tl):
        N = tl.n
        C = 64 if tl.kind == "p" else 1
        nch = N // C
        self.load_h(tl, first)
        self.rms(self.hs, N, "norm_mix", li, out_bf=self.xn)
        xin = lambda k: (self.xn.t[:, k, 0:N], [self.xn.r])
        if kind == "gla":
            def cons_t1(mc, msz, pb):
                S.op("scalar", lambda e: e.copy(out=self.la_t1.t[:, 0:N], in_=pb.t[0:16, 0:N]), reads=[pb.r], writes=[self.la_t1.r])
            self.proj(wg1, D, 16, xin, N, cons_t1)
        def head_body(h):
            def cons_q(mc, msz, pb):
                S.op("scalar", lambda e: e.mul(out=qf.t[:, mc, 0:N], in_=pb.t[:, 0:N], mul=float(dk) ** -0.5), reads=[pb.r], writes=[qf.r])
            self.proj(wq[:, h * dk:(h + 1) * dk], D, dk, xin, N, cons_q, mw=min(256, dk), wname="%s_q%d" % (kind, h))
            if kind == "gla":
                def cons_k(mc, msz, pb):
                    S.op("scalar", lambda e: e.copy(out=kf.t[:, mc, 0:N], in_=pb.t[:, 0:N]), reads=[pb.r], writes=[kf.r])
                self.proj(wk[:, h * dk:(h + 1) * dk], D, dk, xin, N, cons_k, wname="%s_k%d" % (kind, h))
                t1in = lambda k: (self.la_t1.t[:, 0:N], [self.la_t1.r])

                def cons_gk(mc, msz, pb):
                    bcol = self.pcol("gla_b_gk", h * kcn + mc)
                    S.op("vector", lambda e: e.tensor_scalar(out=t0.t[:, 0:N], in0=pb.t[:, 0:N], scalar1=bcol, scalar2=-1.0, op0=ALU.add, op1=ALU.mult),
                         reads=[pb.r, self.pv.r], writes=[t0.r])
                    S.op("scalar", lambda e: e.activation(out=t0.t[:, 0:N], in_=t0.t[:, 0:N], func=AF.Exp), writes=[t0.r])
                    S.op("scalar", lambda e: e.activation(out=t0.t[:, 0:N], in_=t0.t[:, 0:N], func=AF.Ln, bias=self.pcol("one"), scale=1.0), reads=[self.pv.r], writes=[t0.r])
                    S.op("vector", lambda e: e.tensor_scalar(out=lg.t[:, mc, 0:N], in0=t0.t[:, 0:N], scalar1=-1.0 / 16.0, scalar2=None, op0=ALU.mult), reads=[t0.r], writes=[lg.r])
                self.proj(wg2[:, h * dk:(h + 1) * dk], 16, dk, t1in, N, cons_gk)
            else:
                def cons_f(mc, msz, pb):
                    lbc = self.la_lb.t[:, h:h + 1]
                    omc = self.la_lb.t[:, DC + h:DC + h + 1]
                    S.op("scalar", lambda e: e.activation(out=t0.t[:, 0:N], in_=pb.t[:, 0:N], func=AF.Sigmoid), reads=[pb.r], writes=[t0.r])
                    S.op("vector", lambda e: e.tensor_scalar(out=t1b.t[:, 0:N], in0=t0.t[:, 0:N], scalar1=omc, scalar2=lbc, op0=ALU.mult, op1=ALU.add),
                         reads=[t0.r, self.la_lb.r], writes=[t1b.r])
                    S.op("scalar", lambda e: e.activation(out=lg.t[:, 0, 0:N], in_=t1b.t[:, 0:N], func=AF.Ln), reads=[t1b.r], writes=[lg.r])
                    S.op("vector", lambda e: e.tensor_scalar(out=t1b.t[:, 0:N], in0=t0.t[:, 0:N], scalar1=-1.0, scalar2=1.0, op0=ALU.mult, op1=ALU.add),
                         reads=[t0.r], writes=[t1b.r])
                    S.op("vector", lambda e: e.tensor_scalar(out=kf.t[:, 0, 0:N], in0=t1b.t[:, 0:N], scalar1=omc, scalar2=None, op0=ALU.mult),
                         reads=[self.la_lb.r, t1b.r], writes=[kf.r])
                self.proj(wk[:, h * dk:(h + 1) * dk], D, dk, xin, N, cons_f, mw=128, wname="%s_k%d" % (kind, h))

            def cons_v(mc, msz, pb):
                S.op("scalar", lambda e: e.copy(out=vh.t[:, mc, 0:N], in_=pb.t[:, 0:N]), reads=[pb.r], writes=[vh.r])
            self.proj(wv[:, h * dv:(h + 1) * dv], D, dv, xin, N, cons_v, mw=min(256, dv), wname="%s_v%d" % (kind, h))
            for kc in range(kcn):
                S.op("vector", lambda e, kc=kc: e.tensor_tensor_scan(out=cum.t[:, kc, 0:N], data0=self.rmask.t[:, 0:N] if C > 1 else self.zero_ap(N), data1=lg.t[:, kc, 0:N],
                                                                     initial=0.0, op0=ALU.mult, op1=ALU.add), reads=[lg.r, self.rmask.r, self.zeros.r], writes=[cum.r])
                S.op("scalar", lambda e, kc=kc: e.activation(out=eg.t[:, kc, 0:N], in_=cum.t[:, kc, 0:N], func=AF.Exp), reads=[cum.r], writes=[eg.r])
                S.op("vector", lambda e, kc=kc: e.tensor_tensor(out=qe.t[:, kc, 0:N], in0=qf.t[:, kc, 0:N], in1=eg.t[:, kc, 0:N], op=ALU.mult), reads=[qf.r, eg.r], writes=[qe.r])
                S.op("scalar", lambda e, kc=kc: e.activation(out=cum.t[:, kc, 0:N], in_=cum.t[:, kc, 0:N], func=AF.Exp, scale=-1.0), writes=[cum.r])
                S.op("vector", lambda e, kc=kc: e.tensor_tensor(out=ke.t[:, kc, 0:N], in0=kf.t[:, kc, 0:N], in1=cum.t[:, kc, 0:N], op=ALU.mult), reads=[kf.r, cum.r], writes=[ke.r])
            for ch in range(nch):
                for vc in range(vcn):
                    pb = self.psum()
                    S.op("tensor", lambda e, pb=pb, ch=ch, vc=vc: e.transpose(out=self.pbf(pb)[0:C, 0:128], in_=vh.t[:, vc, ch * C:(ch + 1) * C], identity=self.identb.t[:]),
                         reads=[vh.r, self.identb.r], writes=[pb.r])
                    S.op("scalar", lambda e, pb=pb, ch=ch, vc=vc: e.copy(out=VT.t[0:C, ch, vc * 128:(vc + 1) * 128], in_=self.pbf(pb)[0:C, 0:128]), reads=[pb.r], writes=[VT.r])
            obs = self.obank[0:vcn]
            for ch in range(nch):
                cs = slice(ch * C, (ch + 1) * C)
                if tl.kind == "s":
                    Sc, Scb, so = self.la_Ss, self.la_Ssbf, 0
                    S.dma("sync", Sc.t[:, 0:kcn * dv], st_in[ch, h], writes=[Sc.r])
                    S.op("scalar", lambda e, Sc=Sc, Scb=Scb: e.copy(out=Scb.t[:, 0:kcn * dv], in_=Sc.t[:, 0:kcn * dv]), reads=[Sc.r], writes=[Scb.r])
                else:
                    Sc, Scb, so = Sst, Sbf, h * kcn * dv
                pa = self.psum()
                for kc in range(kcn):
                    S.op("tensor", lambda e, pa=pa, kc=kc, cs=cs: e.matmul(pa.t[0:C, 0:C], ke.t[:, kc, cs], qe.t[:, kc, cs], start=(kc == 0), stop=(kc == kcn - 1)),
                         reads=[ke.r, qe.r], writes=[pa.r])
                att = self.la_att[ch % 2]
                S.op("vector", lambda e, pa=pa, att=att: e.tensor_tensor(out=att.t[0:C, 0:C], in0=pa.t[0:C, 0:C], in1=self.masks.t[0:C, 0:C], op=ALU.mult),
                     reads=[pa.r, self.masks.r], writes=[att.r])
                KET = self.la_KET[ch % 2]
                for kc in range(kcn):
                    pb = self.psum()
                    S.op("tensor", lambda e, pb=pb, kc=kc, cs=cs: e.transpose(out=self.pbf(pb)[0:C, 0:128], in_=ke.t[:, kc, cs], identity=self.identb.t[:]),
                         reads=[ke.r, self.identb.r], writes=[pb.r])
                    S.op("scalar", lambda e, pb=pb, kc=kc, KET=KET: e.copy(out=KET.t[0:C, kc, :], in_=self.pbf(pb)[0:C, 0:128]), reads=[pb.r], writes=[KET.r])
                for vc in range(vcn):
                    ob = obs[vc]
                    S.op("tensor", lambda e, ob=ob, vc=vc, ch=ch, cs=cs, att=att: e.matmul(ob.t[:, cs], VT.t[0:C, ch, vc * 128:(vc + 1) * 128], att.t[0:C, 0:C], start=True, stop=False),
                         reads=[VT.r, att.r], writes=[ob.r])
                    for kc in range(kcn):
                        S.op("tensor", lambda e, ob=ob, vc=vc, kc=kc, cs=cs, Scb=Scb, so=so: e.matmul(ob.t[:, cs], Scb.t[:, so + kc * dv + vc * 128: so + kc * dv + (vc + 1) * 128], qe.t[:, kc, cs],
                                                                                                  start=False, stop=(kc == kcn - 1)),
                             reads=[Scb.r, qe.r], writes=[ob.r])
                for kc in range(kcn):
                    pb = self.psum()
                    S.op("tensor", lambda e, pb=pb, kc=kc, ch=ch, KET=KET: e.matmul(pb.t[:, 0:dv], KET.t[0:C, kc, :], VT.t[0:C, ch, 0:dv], start=True, stop=True),
                         reads=[KET.r, VT.r], writes=[pb.r])
                    sl = slice(so + kc * dv, so + (kc + 1) * dv)
                    el = eg.t[:, kc, (ch + 1) * C - 1:(ch + 1) * C]
                    S.op("vector", lambda e, sl=sl, el=el, Sc=Sc: e.tensor_scalar(out=Sc.t[:, sl], in0=Sc.t[:, sl], scalar1=el, scalar2=None, op0=ALU.mult), reads=[eg.r, Scb.r], writes=[Sc.r])
                    S.op("vector", lambda e, sl=sl, el=el, Sc=Sc, pb=pb: e.scalar_tensor_tensor(out=Sc.t[:, sl], in0=pb.t[:, 0:dv], scalar=el, in1=Sc.t[:, sl], op0=ALU.mult, op1=ALU.add),
                         reads=[pb.r, eg.r], writes=[Sc.r])
                    S.op("scalar", lambda e, sl=sl, Sc=Sc, Scb=Scb: e.copy(out=Scb.t[:, sl], in_=Sc.t[:, sl]), reads=[Sc.r], writes=[Scb.r])
                if tl.kind == "s":
                    S.dma("sync", st_s[ch, h], Sc.t[:, 0:kcn * dv], reads=[Sc.r])
            if tl.kind == "p" and tl.idx == self.NPT - 1:
                S.dma("sync", st_p[h], Sst.t[:, h * kcn * dv:(h + 1) * kcn * dv], reads=[Sst.r])
            pn = self.psum()
            for vc in range(vcn):
                q = self.sq[vc % 2]
                S.op("scalar", lambda e, q=q, vc=vc: e.activation(out=q.t[:, 0:N], in_=obs[vc].t[:, 0:N], func=AF.Square), reads=[obs[vc].r], writes=[q.r])
                S.op("tensor", lambda e, q=q, vc=vc, pn=pn: e.matmul(pn.t[:, 0:N], self.ones.t[:], q.t[:, 0:N], start=(vc == 0), stop=(vc == vcn - 1)),
                     reads=[q.r, self.ones.r], writes=[pn.r])
            S.op("scalar", lambda e, pn=pn: e.activation(out=t2.t[:, 0:N], in_=pn.t[:, 0:N], func=AF.Sqrt, bias=self.eps_ap(), scale=1.0 / dv), reads=[pn.r, self.pv.r], writes=[t2.r])
            S.op("vector", lambda e: e.reciprocal(out=t2.t[:, 0:N], in_=t2.t[:, 0:N]), writes=[t2.r])

            def cons_g(mc, msz, pb):
                S.op("scalar", lambda e: e.activation(out=t0.t[:, 0:N], in_=pb.t[:, 0:N], func=AF.Silu), reads=[pb.r], writes=[t0.r])
                S.op("vector", lambda e: e.scalar_tensor_tensor(out=t1b.t[:, 0:N], in0=obs[mc].t[:, 0:N], scalar=self.pcol(nname, mc), in1=t2.t[:, 0:N], op0=ALU.mult, op1=ALU.mult),
                     reads=[obs[mc].r, t2.r, self.pv.r], writes=[t1b.r])
                S.op("vector", lambda e: e.tensor_tensor(out=og.t[:, h * vcn + mc, 0:N], in0=t1b.t[:, 0:N], in1=t0.t[:, 0:N], op=ALU.mult), reads=[t0.r, t1b.r], writes=[og.r])
            self.proj(wg[:, h * dv:(h + 1) * dv], D, dv, xin, N, cons_g, mw=min(256, dv), wname="%s_g%d" % (kind, h))
        for h in range(H):
            head_body(h)
        oin = lambda k: (og.t[:, k, 0:N], [og.r])

        def cons_o(mc, msz, pb):
            S.op("vector", lambda e: e.tensor_tensor(out=self.hs.t[:, mc, 0:N], in0=self.hs.t[:, mc, 0:N], in1=pb.t[:, 0:N], op=ALU.add), reads=[pb.r], writes=[self.hs.r])
        self.proj(wo, D, D, oin, N, cons_o, wname="%s_o" % kind)
        self.store_h(tl)
    for tl in self.tiles:
        tile_body(tl)
    self.ring = self.psr


Net.la_setup = la_setup
Net.la_layer = la_layer


NCORES = 8
TPF, NSF = 2048, 16


def build_full(cols, pvn):
    nc = bass.Bass("TRN2", target_bir_lowering=False)
    es = ExitStack()
    cx = Ctx(nc, es)
    cfg = dict(TP=TPF, NS=NSF, DEPTH=4, pv_cols=cols, pv_n=pvn)
    net = Net(cx, cfg)
    net.la_setup()
    net.ffn_setup()
    net.rw_setup()
    net.s5_setup()
    S = cx.S
    net.s5_generate()
    net.s5_layer()
    barrier(S)
    net.ffn(0)
    barrier(S)
    net.rw_layer()
    barrier(S)
    net.ffn(1)
    barrier(S)
    net.la_layer("gla")
    barrier(S)
    net.ffn(2)
    barrier(S)
    net.la_layer("hg")
    barrier(S)
    net.ffn(3)
    barrier(S)
    net.final()
    cx.S.finish()
    cx.S.emit()
    es.close()
    return nc


def kernel(**inp):
    inp = {k: np.asarray(v) for k, v in inp.items()}
    cols, pv = pack_pvec(inp)
    nc = build_full(cols, pv.shape[1])
    B, T = inp["x_prompt"].shape[0], inp["x_prompt"].shape[1]
    rm = np.ones((128, 512), np.float32)
    rm[:, ::64] = 0
    shared = {"pvec": pv, "ident": np.eye(128, dtype=np.float32), "maskT": np.triu(np.ones((64, 64), np.float32)), "rmask": rm, "rwc": rw_consts()}
    shared.update(s5_inputs(inp))
    for k in ["rw_w_r", "rw_w_k", "rw_w_v", "rw_w_o", "rw_w1", "rw_w2", "rw_a1", "rw_a2", "rw_g1", "rw_g2", "ffn_w_up", "ffn_w_gate", "ffn_w_down", "gla_w_q", "gla_w_k", "gla_w_v", "gla_w_gk1", "gla_w_gk2", "gla_w_g", "gla_w_o",
              "hg_w_q", "hg_w_f", "hg_w_i", "hg_w_g", "hg_w_o"]:
        shared[k] = np.ascontiguousarray(inp[k], dtype=np.float32)
    in_maps = []
    for c in range(NCORES):
        b = c % B
        sl = slice(c * NSF, (c + 1) * NSF)
        m = dict(shared)
        m["xT"] = np.ascontiguousarray(np.concatenate([inp["x_prompt"][b], inp["x_sample"][sl, 0]], 0).T)
        cs = inp["state_ffn_conv"][:, sl]
        m["conv_s"] = np.ascontiguousarray(cs.reshape(4, NSF, 2, FC, 128).transpose(0, 4, 3, 1, 2).reshape(4, 128, -1))
        m["gla_s"] = np.ascontiguousarray(inp["state_gla"][sl].reshape(NSF, 4, 2, 128, 512).transpose(0, 1, 3, 2, 4).reshape(NSF, 4, 128, 1024))
        m["hg_s"] = np.ascontiguousarray(inp["state_hgrn"][sl])
        m["s5_h_s"] = s5_state_in(inp["state_s5_re"][sl], inp["state_s5_im"][sl], NSF)
        m["rw_shift_s"] = np.ascontiguousarray(inp["state_rwkv_shift"][sl].reshape(NSF, DC, 128).transpose(2, 1, 0).reshape(128, -1))
        m["rw_wkv_s"] = np.ascontiguousarray(inp["state_rwkv_wkv"][sl].transpose(3, 0, 1, 2).reshape(64, NSF, D))
        in_maps.append(m)
    res = run_bass_kernel_spmd(nc, in_maps, core_ids=list(range(NCORES)))
    R_ = res.results
    f32 = np.float32
    NSA = NCORES * NSF
    y_p = np.stack([R_[b]["yT"][:, :T].T for b in range(B)]).astype(f32)
    y_s = np.concatenate([R_[c]["yT"][:, T:].T for c in range(NCORES)])[:, None, :].astype(f32)
    conv_p = np.stack([R_[b]["conv_p_o"].reshape(4, 128, FC, 2).transpose(0, 3, 2, 1).reshape(4, 2, DFF) for b in range(B)], axis=1).astype(f32)
    conv_s = np.concatenate([R_[c]["conv_s_o"].reshape(4, 128, FC, NSF, 2).transpose(0, 3, 4, 2, 1).reshape(4, NSF, 2, DFF) for c in range(NCORES)], axis=1).astype(f32)
    gla_p = np.stack([R_[b]["gla_p_o"].reshape(4, 128, 2, 512).transpose(0, 2, 1, 3).reshape(4, 256, 512) for b in range(B)]).astype(f32)
    gla_s = np.concatenate([R_[c]["gla_s_o"].reshape(NSF, 4, 128, 2, 512).transpose(0, 1, 3, 2, 4).reshape(NSF, 4, 256, 512) for c in range(NCORES)]).astype(f32)
    hg_p = np.stack([R_[b]["hg_p_o"] for b in range(B)]).astype(f32)
    hg_s = np.concatenate([R_[c]["hg_s_o"] for c in range(NCORES)]).astype(f32)
    sh_p = np.stack([R_[b]["rw_shift_p_o"].T.reshape(-1) for b in range(B)]).astype(f32)
    sh_s = np.concatenate([R_[c]["rw_shift_s_o"].reshape(128, DC, NSF).transpose(2, 1, 0).reshape(NSF, D) for c in range(NCORES)]).astype(f32)
    wkv_p = np.stack([R_[b]["rw_wkv_p_o"].reshape(64, 32, 64).transpose(1, 2, 0) for b in range(B)]).astype(f32)
    wkv_s = np.concatenate([R_[c]["rw_wkv_s_o"].reshape(64, NSF, 32, 64).transpose(1, 2, 3, 0) for c in range(NCORES)]).astype(f32)
    s5p = [s5_state_out_p(R_[b]["s5_hp_o"]) for b in range(B)]
    s5s = [s5_state_out_s(R_[c]["s5_hs_o"], NSF) for c in range(NCORES)]
    s5_re_p = np.stack([a[0] for a in s5p]).astype(f32)
    s5_im_p = np.stack([a[1] for a in s5p]).astype(f32)
    s5_re_s = np.concatenate([a[0] for a in s5s]).astype(f32)
    s5_im_s = np.concatenate([a[1] for a in s5s]).astype(f32)
    return (y_p, y_s, s5_re_p, s5_im_p, sh_p, wkv_p, gla_p, hg_p, conv_p,
            s5_re_s, s5_im_s, sh_s, wkv_s, gla_s, hg_s, conv_s)


def barrier(S):
    evs = [(k, v) for k, v in S.cnt.items() if v > 0]
    for eng in S.ops:
        waits = []
        for ev in evs:
            if ev[0] == eng and eng == "tensor":
                continue
            S._need(eng, ev, waits)
        if waits:
            S.ops[eng].append((None, waits, None))


def rw_setup(self):
    c, S = self.cx, self.S
    NS = self.NS
    self.rwc = c.sb([128, 128 + 64 + 512 + 512])
    S.dma("sync", self.rwc.t[:], c.inp("rwc", [128, 1216]), writes=[self.rwc.r])
    self.rw_halo = c.sb([128, DC])
    self.rw_shs = c.sb([128, DC, NS])
    self.rw_tw = c.sb([128, 2, 512], BF16)
    self.rw_der = c.sb([128, 7 * DC])
    S.op("vector", lambda e: e.tensor_scalar(out=self.rw_der.t[:, 0:6 * DC], in0=self.pcol("rw_mix", 0, 6 * DC), scalar1=-1.0, scalar2=1.0, op0=ALU.mult, op1=ALU.add),
         reads=[self.pv.r], writes=[self.rw_der.r])
    S.op("vector", lambda e: e.tensor_scalar(out=self.rw_der.t[:, 6 * DC:7 * DC], in0=self.pcol("rw_w0", 0, DC), scalar1=-1.0, scalar2=None, op0=ALU.mult),
         reads=[self.pv.r], writes=[self.rw_der.r])
    self.rw_scr = c.scratch("rw_scr", [6, D, 512])
    self.rw_spR = [[R() for _ in range(DC)] for _ in range(6)]
    self.sti = 0


def rw_layer(self):
    S, c = self.S, self.cx
    NS = self.NS
    li = 1
    w_r, w_k, w_v, w_o = c.inp("rw_w_r", [D, D]), c.inp("rw_w_k", [D, D]), c.inp("rw_w_v", [D, D]), c.inp("rw_w_o", [D, D])
    w1, w2 = c.inp("rw_w1", [D, 96]), c.inp("rw_w2", [96, D])
    a1, a2 = c.inp("rw_a1", [D, 96]), c.inp("rw_a2", [96, D])
    g1, g2 = c.inp("rw_g1", [D, 256]), c.inp("rw_g2", [256, D])
    shift_in = c.inp("rw_shift_s", [128, DC * NS])
    wkv_in = c.inp("rw_wkv_s", [64, NS, D])
    shift_p_o = c.outp("rw_shift_p_o", [128, DC])
    shift_s_o = c.outp("rw_shift_s_o", [128, DC * NS])
    wkv_p_o = c.outp("rw_wkv_p_o", [64, D])
    wkv_s_o = c.outp("rw_wkv_s_o", [64, NS, D])
    hs, xn, og = self.hs, self.xn, self.la_og
    rwc = self.rwc
    bones = rwc.t[:, 0:128]
    ident2 = rwc.t[:, 128:192]
    mG12 = rwc.t[0:64, 192:704].rearrange("p (u w) -> p u w", u=4)
    mG3 = rwc.t[0:64, 704:1216].rearrange("p (u w) -> p u w", u=4)
    identF = self.ident
    E = []
    for b in self.la_f:
        E.append(View(b.t[:, 0, :]))
        E.append(View(b.t[:, 1, :]))
    E += [View(b.t[:]) for b in self.la_t] + [View(self.gbuf[1].t[:])] + [View(b.t[:]) for b in self.sq]
    stage = [E[10], E[11]]
    st_p = View(self.la_S.t[0:64, 0:2048].rearrange("p (h i) -> p h i", h=32))
    st_s = View(self.la_S.t[0:64, 2048:4096].rearrange("p (s i) -> p s i", s=NS))
    st_heads = [View(st_p.t[:, h, :]) for h in range(32)]
    S.op("vector", lambda e: e.memset(self.la_S.t[:], 0.0), writes=[self.la_S.r, st_p.r] + [v_.r for v_ in st_heads])
    S.op("vector", lambda e: e.memset(self.rw_halo.t[:], 0.0), writes=[self.rw_halo.r])
    S.dma("sync", self.rw_shs.t[:].rearrange("p c s -> p (c s)"), shift_in, writes=[self.rw_shs.r])
    hsF = hs.t[:].rearrange("p c n -> p (c n)")
    xnF = xn.t[:].rearrange("p c n -> p (c n)").bitcast(F32)
    actF = self.act.t[:, DC:FC, :].rearrange("p f n -> p (f n)").bitcast(F32)
    self.ring = self.psr[0:4]
    ob = self.obank[0]

    def tile_body(tl):
        N = tl.n
        C = 64 if tl.kind == "p" else 1
        nch = N // C
        U = nch
        W2 = 2 * C
        barrier(S)
        self.load_h(tl, False)
        self.rms(hs, N, "norm_mix", li, out_f32=hs)
        if tl.kind == "s":
            S.dma("sync", shift_s_o.rearrange("p (c s) -> p c s", c=DC), hs.t[:, :, 0:N], reads=[hs.r])

        def variant(mi):
            tmp = E[12]
            for cc in range(DC):
                m = self.pcol("rw_mix", mi * DC + cc)
                om = self.rw_der.t[:, mi * DC + cc: mi * DC + cc + 1]
                if tl.kind == "p":
                    S.op("vector", lambda e, cc=cc, m=m: e.tensor_scalar(out=tmp.t[:, 1:N], in0=hs.t[:, cc, 0:N - 1], scalar1=m, scalar2=None, op0=ALU.mult),
                         reads=[hs.r, self.pv.r], writes=[tmp.r])
                    S.op("vector", lambda e, cc=cc, m=m: e.tensor_scalar(out=tmp.t[:, 0:1], in0=self.rw_halo.t[:, cc:cc + 1], scalar1=m, scalar2=None, op0=ALU.mult),
                         reads=[self.rw_halo.r, self.pv.r], writes=[tmp.r])
                else:
                    S.op("vector", lambda e, cc=cc, m=m: e.tensor_scalar(out=tmp.t[:, 0:N], in0=self.rw_shs.t[:, cc, :], scalar1=m, scalar2=None, op0=ALU.mult),
                         reads=[self.rw_shs.r, self.pv.r], writes=[tmp.r])
                S.op("vector", lambda e, cc=cc, om=om: e.scalar_tensor_tensor(out=xn.t[:, cc, 0:N], in0=hs.t[:, cc, 0:N], scalar=om, in1=tmp.t[:, 0:N], op0=ALU.mult, op1=ALU.add),
                     reads=[hs.r, tmp.r, self.rw_der.r], writes=[xn.r])
        xin = lambda k: (xn.t[:, k, 0:N], [xn.r])

        def spill(ti):
            def cons(mc, msz, pb):
                st = stage[self.sti % 2]
                self.sti += 1
                S.op("scalar", lambda e: e.copy(out=st.t[:, 0:N], in_=pb.t[:, 0:N]), reads=[pb.r], writes=[st.r])
                S.dma("sync", self.rw_scr[ti][mc * 128:(mc + 1) * 128, 0:N], st.t[:, 0:N], reads=[st.r], writes=[self.rw_spR[ti][mc]])
            return cons
        tw = self.rw_tw
        variant(0)
        self.proj(w_r, D, D, xin, N, spill(0), wname="rw_r")
        variant(2)
        self.proj(w_k, D, D, xin, N, spill(1), wname="rw_k")
        variant(3)
        self.proj(w_v, D, D, xin, N, spill(2), wname="rw_v")
        variant(1)

        def cons_tw(mc, msz, pb):
            S.op("scalar", lambda e: e.activation(out=tw.t[0:96, 0, 0:N], in_=pb.t[0:96, 0:N], func=AF.Tanh), reads=[pb.r], writes=[tw.r])
        self.proj(w1, D, 96, xin, N, cons_tw)
        self.proj(w2, 96, D, lambda k: (tw.t[0:96, 0, 0:N], [tw.r]), N, spill(3))
        variant(4)

        def cons_ta(mc, msz, pb):
            S.op("scalar", lambda e: e.copy(out=tw.t[0:96, 0, 0:N], in_=pb.t[0:96, 0:N]), reads=[pb.r], writes=[tw.r])
        self.proj(a1, D, 96, xin, N, cons_ta)
        self.proj(a2, 96, D, lambda k: (tw.t[0:96, 0, 0:N], [tw.r]), N, spill(4))
        variant(5)

        def cons_tg(mc, msz, pb):
            S.op("scalar", lambda e: e.activation(out=tw.t[:, mc, 0:N], in_=pb.t[:, 0:N], func=AF.Sigmoid), reads=[pb.r], writes=[tw.r])
        self.proj(g1, D, 256, xin, N, cons_tg)
        self.proj(g2, 256, D, lambda k: (tw.t[:, k, 0:N], [tw.r]), N, spill(5))
        if tl.kind == "p":
            S.op("vector", lambda e: e.tensor_copy(out=self.rw_halo.t[:], in_=hs.t[:, :, N - 1]), reads=[hs.r], writes=[self.rw_halo.r])
            if tl.idx == self.NPT - 1:
                S.dma("sync", shift_p_o, self.rw_halo.t[:], reads=[self.rw_halo.r])
        barrier(S)
        o = [0]

        def carve(arena, shape):
            n = shape[1] * shape[2]
            v = View(arena[0:shape[0], o[0]:o[0] + n].rearrange("p (a b) -> p a b", a=shape[1]))
            o[0] += n
            return v
        G1m, G2m, G3m = [carve(hsF, [64, U, W2]) for _ in range(3)]
        Z1a, Qh, Eh, Fh = [carve(hsF, [64, U, 64]) for _ in range(4)]
        Z1b, Dh = [carve(hsF, [64, U, C]) for _ in range(2)]
        Xs = [carve(hsF, [64, U, C]) for _ in range(2)]
        Ys = [carve(hsF, [64, U, C]) for _ in range(2)]
        assert o[0] <= 8192
        o[0] = 0
        TMs = [carve(actF, [64, 8, 512])]
        Ts = [carve(actF, [64, U, C]) for _ in range(2)]
        Dg = carve(actF, [128, nch, 64])
        assert o[0] <= 7168
        QEDF2 = None
        if nch > 8:
            o[0] = 0
            TMs.append(carve(xnF, [64, 8, 512]))
        else:
            o[0] = 0
            QEDF2 = [carve(xnF, [64, U, 64]), carve(xnF, [64, U, 64]), carve(xnF, [64, U, C]), carve(xnF, [64, U, 64])]
        TM = TMs[0]

        def tmr(u, c0, c1):
            return TMs[u // 8].t[0:C, u % 8, c0:c1]

        def pair_body(hp):
            bufs = {}
            for ti, nm in enumerate(["r", "k", "v", "wl", "a", "g"]):
                b = E[ti]
                S.dma("sync", b.t[:, 0:N], self.rw_scr[ti][hp * 128:(hp + 1) * 128, 0:N], reads=[self.rw_spR[ti][hp]], writes=[b.r])
                bufs[nm] = b
            r, k, v, wl, a, g = [bufs[n] for n in ["r", "k", "v", "wl", "a", "g"]]
            kk, t1, t2, cum, eg, en, bonus, btp, ktp = E[6], E[7], E[8], E[9], E[10], E[11], E[12], E[13], E[14]
            Xb_, Yb_ = self.la_Ss, None
            X = View(self.la_Ss.t[:, 0:1024].rearrange("p (a n) -> p a n", a=2))
            Y = View(self.la_Sbf.t[:, 0:2048].bitcast(F32).rearrange("p (a n) -> p a n", a=2))
            sl = slice(0, N)

            def act_(out, in_, func, reads, writes, **kw):
                S.op("scalar", lambda e: e.activation(out=out, in_=in_, func=func, **kw), reads=reads, writes=writes)

            def tt(out, in0, in1, op, reads, writes):
                S.op("vector", lambda e: e.tensor_tensor(out=out, in0=in0, in1=in1, op=op), reads=reads, writes=writes)

            def ts(out, in0, s1, s2, op0, op1, reads, writes):
                if s2 is None:
                    S.op("vector", lambda e: e.tensor_scalar(out=out, in0=in0, scalar1=s1, scalar2=None, op0=op0), reads=reads, writes=writes)
                else:
                    S.op("vector", lambda e: e.tensor_scalar(out=out, in0=in0, scalar1=s1, scalar2=s2, op0=op0, op1=op1), reads=reads, writes=writes)
            pvr = self.pv.r
            act_(wl.t[:, sl], wl.t[:, sl], AF.Exp, [self.rw_der.r], [wl.r], scale=-1.0, bias=self.rw_der.t[:, 6 * DC + hp:6 * DC + hp + 1])
            act_(wl.t[:, sl], wl.t[:, sl], AF.Ln, [pvr], [wl.r], bias=self.pcol("one"))
            act_(wl.t[:, sl], wl.t[:, sl], AF.Exp, [pvr], [wl.r], scale=-1.0, bias=self.pcol("neghalf"))
            ts(wl.t[:, sl], wl.t[:, sl], -1.0, None, ALU.mult, None, [], [wl.r])
            act_(a.t[:, sl], a.t[:, sl], AF.Sigmoid, [pvr], [a.r], bias=self.pcol("rw_a0", hp))
            ts(kk.t[:, sl], k.t[:, sl], self.pcol("rw_k_k", hp), None, ALU.mult, None, [k.r, pvr], [kk.r])
            tt(t1.t[:, sl], kk.t[:, sl], kk.t[:, sl], ALU.mult, [kk.r], [t1.r])
            pb = self.psum()
            S.op("tensor", lambda e, pb=pb: e.matmul(pb.t[:, sl], bones, t1.t[:, sl], start=True, stop=True), reads=[t1.r, rwc.r], writes=[pb.r])
            act_(t1.t[:, sl], pb.t[:, sl], AF.Sqrt, [pb.r], [t1.r])
            ts(t1.t[:, sl], t1.t[:, sl], 1e-12, None, ALU.max, None, [], [t1.r])
            S.op("vector", lambda e: e.reciprocal(out=t1.t[:, sl], in_=t1.t[:, sl]), writes=[t1.r])
            tt(kk.t[:, sl], kk.t[:, sl], t1.t[:, sl], ALU.mult, [t1.r], [kk.r])
            ts(t2.t[:, sl], a.t[:, sl], -1.0, self.pcol("rw_k_a", hp), ALU.add, ALU.mult, [a.r, pvr], [t2.r])
            S.op("vector", lambda e: e.scalar_tensor_tensor(out=k.t[:, sl], in0=t2.t[:, sl], scalar=1.0, in1=k.t[:, sl], op0=ALU.add, op1=ALU.mult), reads=[t2.r], writes=[k.r])
            tt(t1.t[:, sl], r.t[:, sl], k.t[:, sl], ALU.mult, [r.r, k.r], [t1.r])
            ts(t1.t[:, sl], t1.t[:, sl], self.pcol("rw_r_k", hp), None, ALU.mult, None, [pvr], [t1.r])
            pb2 = self.psum()
            S.op("tensor", lambda e, pb2=pb2: e.matmul(pb2.t[:, sl], bones, t1.t[:, sl], start=True, stop=True), reads=[t1.r, rwc.r], writes=[pb2.r])
            tt(bonus.t[:, sl], pb2.t[:, sl], v.t[:, sl], ALU.mult, [pb2.r, v.r], [bonus.r])
            S.op("vector", lambda e: e.tensor_tensor_scan(out=cum.t[:, sl], data0=self.rmask.t[:, sl] if C > 1 else self.zero_ap(N), data1=wl.t[:, sl], initial=0.0, op0=ALU.mult, op1=ALU.add),
                 reads=[wl.r, self.rmask.r, self.zeros.r], writes=[cum.r])
            act_(eg.t[:, sl], cum.t[:, sl], AF.Exp, [cum.r], [eg.r])
            act_(en.t[:, sl], cum.t[:, sl], AF.Exp, [cum.r], [en.r], scale=-1.0)
            tt(t2.t[:, sl], cum.t[:, sl], wl.t[:, sl], ALU.subtract, [cum.r, wl.r], [t2.r])
            act_(t2.t[:, sl], t2.t[:, sl], AF.Exp, [], [t2.r])
            S.op("vector", lambda e: e.scalar_tensor_tensor(out=X.t[:, 0, sl], in0=kk.t[:, sl], scalar=-1.0, in1=t2.t[:, sl], op0=ALU.mult, op1=ALU.mult), reads=[kk.r, t2.r], writes=[X.r])
            tt(X.t[:, 1, sl], r.t[:, sl], eg.t[:, sl], ALU.mult, [r.r, eg.r], [X.r])
            tt(t1.t[:, sl], kk.t[:, sl], a.t[:, sl], ALU.mult, [kk.r, a.r], [t1.r])
            tt(Y.t[:, 0, sl], t1.t[:, sl], en.t[:, sl], ALU.mult, [t1.r, en.r], [Y.r])
            tt(Y.t[:, 1, sl], k.t[:, sl], en.t[:, sl], ALU.mult, [k.r, en.r], [Y.r])
            for ch in range(nch):
                cs = slice(ch * C, (ch + 1) * C)
                el = eg.t[:, (ch + 1) * C - 1:(ch + 1) * C]
                ts(btp.t[:, cs], Y.t[:, 0, cs], el, None, ALU.mult, None, [Y.r, eg.r], [btp.r])
                ts(ktp.t[:, cs], Y.t[:, 1, cs], el, None, ALU.mult, None, [Y.r, eg.r], [ktp.r])
                ts(Dg.t[:, ch, :], ident2, el, None, ALU.mult, None, [rwc.r, eg.r], [Dg.r])
            for ch in range(nch):
                cs = slice(ch * C, (ch + 1) * C)
                pbt = self.psum()
                for wi_, src in enumerate([X.t[:, 0, cs], btp.t[:, cs], ktp.t[:, cs], v.t[:, cs]]):
                    S.op("tensor", lambda e, pbt=pbt, wi_=wi_, src=src: e.transpose(out=pbt.t[0:C, wi_ * 128:(wi_ + 1) * 128], in_=src, identity=identF.t[:]),
                         reads=[X.r, btp.r, ktp.r, v.r, identF.r], writes=[pbt.r])
                S.op("scalar", lambda e, pbt=pbt, ch=ch: e.copy(out=tmr(ch, 0, 512), in_=pbt.t[0:C, 0:512]), reads=[pbt.r], writes=[TM.r])

            def head_body(hd, Qh, Eh, Dh, Fh):
                P = slice(hd * 64, hd * 64 + 64)
                hcol = slice(hd * 64, hd * 64 + 64)
                UB = 512 // W2
                for (Gm, lh, rh, mk) in (((G1m, Y.t[P, 0, :], X, mG12), (G2m, Y.t[P, 1, :], X, mG12), (G3m, X.t[P, 0, :], Y, mG3)) if C > 1 else ((G1m, Y.t[P, 0, :], X, mG12), (G2m, Y.t[P, 1, :], X, mG12))):
                    for u0 in range(0, U, 4):
                        pbg = self.psum()
                        for u in range(u0, min(U, u0 + 4)):
                            cs = slice(u * C, (u + 1) * C)
                            S.op("tensor", lambda e, pbg=pbg, u=u, u0=u0, cs=cs, lh=lh, rh=rh: e.matmul(pbg.t[0:C, (u - u0) * W2:(u - u0 + 1) * W2], lh[:, cs], rh.t[P, :, cs], start=True, stop=True),
                                 reads=[X.r, Y.r], writes=[pbg.r])
                        nu = min(U, u0 + 4) - u0
                        mview = mk[0:C, 0:nu, :] if C == 64 else mk[0:1, 0:nu, 0:128:64]
                        S.op("vector", lambda e, pbg=pbg, u0=u0, nu=nu, Gm=Gm, mview=mview: e.tensor_tensor(out=Gm.t[0:C, u0:u0 + nu, :], in0=pbg.t[0:C, 0:nu * W2].rearrange("p (u w) -> p u w", u=nu), in1=mview, op=ALU.mult),
                             reads=[pbg.r, rwc.r], writes=[Gm.r])
                Tc, Tn = Ts[0], Ts[1]
                for u in (range(U) if C > 1 else []):
                    S.op("vector", lambda e, u=u, Tc=Tc: e.tensor_tensor(out=Tc.t[0:C, u, :], in0=G1m.t[0:C, u, 0:C], in1=identF.t[0:C, 0:C], op=ALU.add), reads=[G1m.r, identF.r], writes=[Tc.r])
                Xc, Yc = View(G1m.t[:, :, 0:C]), View(G3m.t[:, :, 0:C])
                Xc.r, Yc.r = G1m.r, G3m.r
                nlev = 5 if C == 64 else 0
                for lv in range(1, nlev + 1):
                    UBc = max(1, 512 // C)

                    def mm_level(lhb, rhb, dst, addto=None):
                        for u0 in range(0, U, UBc):
                            pbm = self.psum()
                            nu = min(U, u0 + UBc) - u0
                            for u in range(u0, u0 + nu):
                                S.op("tensor", lambda e, pbm=pbm, u=u, u0=u0: e.matmul(pbm.t[0:C, (u - u0) * C:(u - u0 + 1) * C], lhb.t[0:C, u, :], rhb.t[0:C, u, :], start=True, stop=True),
                                     reads=[lhb.r, rhb.r], writes=[pbm.r])
                            pv_ = pbm.t[0:C, 0:nu * C].rearrange("p (u w) -> p u w", u=nu)
                            if addto is None:
                                S.op("scalar", lambda e, pv_=pv_, u0=u0, nu=nu: e.copy(out=dst.t[0:C, u0:u0 + nu, :], in_=pv_), reads=[pbm.r], writes=[dst.r])
                            else:
                                S.op("vector", lambda e, pv_=pv_, u0=u0, nu=nu: e.tensor_tensor(out=dst.t[0:C, u0:u0 + nu, :], in0=pv_, in1=addto.t[0:C, u0:u0 + nu, :], op=ALU.add),
                                     reads=[pbm.r, addto.r], writes=[dst.r])
                    Xn, Yn = Xs[lv % 2], Ys[lv % 2]
                    if lv < nlev:
                        mm_level(Yc, Xc, Xn)
                    mm_level(Xc, Yc, Yn)
                    mm_level(Yn, Tc, Tn, addto=Tc)
                    Xc, Yc = Xn, Yn
                    Tc, Tn = Tn, Tc
                T = Tc
                for u0 in (range(0, U, 8) if C == 1 else []):
                    S.op("scalar", lambda e, u0=u0: e.copy(out=Z1a.t[0:1, u0:u0 + 8, :], in_=TMs[u0 // 8].t[0:1, 0:8, hd * 64:hd * 64 + 64]), reads=[TM.r], writes=[Z1a.r])
                for u0 in (range(0, U, 8) if C > 1 else []):
                    nu = min(U, u0 + 8) - u0
                    pa_, pb_ = self.psum(), self.psum()
                    for u in range(u0, u0 + nu):
                        S.op("tensor", lambda e, pa_=pa_, u=u, u0=u0: e.matmul(pa_.t[0:C, (u - u0) * 64:(u - u0 + 1) * 64], T.t[0:C, u, :], tmr(u, hd * 64, hd * 64 + 64), start=True, stop=True),
                             reads=[T.r, TM.r], writes=[pa_.r])
                        S.op("tensor", lambda e, pb_=pb_, u=u, u0=u0: e.matmul(pb_.t[0:C, (u - u0) * C:(u - u0 + 1) * C], T.t[0:C, u, :], G3m.t[0:C, u, C:W2], start=True, stop=True),
                             reads=[T.r, G3m.r], writes=[pb_.r])
                    S.op("scalar", lambda e, pa_=pa_, u0=u0, nu=nu: e.copy(out=Z1a.t[0:C, u0:u0 + nu, :], in_=pa_.t[0:C, 0:nu * 64].rearrange("p (u w) -> p u w", u=nu)), reads=[pa_.r], writes=[Z1a.r])
                    S.op("vector", lambda e, pb_=pb_, u0=u0, nu=nu: e.tensor_copy(out=Z1b.t[0:C, u0:u0 + nu, :], in_=pb_.t[0:C, 0:nu * C].rearrange("p (u w) -> p u w", u=nu)), reads=[pb_.r], writes=[Z1b.r])
                for u0 in range(0, U, 8):
                    nu = min(U, u0 + 8) - u0
                    pq, pe, pd, pf = self.psum(), self.psum(), self.psum(), self.psum()
                    for u in range(u0, u0 + nu):
                        j = u - u0
                        cs = slice(u * C, (u + 1) * C)
                        S.op("tensor", lambda e, pq=pq, j=j, cs=cs: e.matmul(pq.t[0:64, j * C:(j + 1) * C], identF.t[:, hcol], X.t[:, 1, cs], start=True, stop=False), reads=[identF.r, X.r], writes=[pq.r])
                        S.op("tensor", lambda e, pq=pq, j=j, u=u: e.matmul(pq.t[0:64, j * C:(j + 1) * C], Z1a.t[0:C, u, :], G1m.t[0:C, u, C:W2], start=False, stop=True), reads=[Z1a.r, G1m.r], writes=[pq.r])
                        S.op("tensor", lambda e, pe=pe, j=j, u=u: e.matmul(pe.t[0:64, j * 64:(j + 1) * 64], identF.t[:, hcol], Dg.t[:, u, :], start=True, stop=False), reads=[identF.r, Dg.r], writes=[pe.r])
                        S.op("tensor", lambda e, pe=pe, j=j, u=u: e.matmul(pe.t[0:64, j * 64:(j + 1) * 64], Z1a.t[0:C, u, :], tmr(u, 128 + hd * 64, 128 + hd * 64 + 64), start=False, stop=True), reads=[Z1a.r, TM.r], writes=[pe.r])
                        if C > 1:
                            S.op("tensor", lambda e, pd=pd, j=j, u=u: e.matmul(pd.t[0:C, j * C:(j + 1) * C], Z1b.t[0:C, u, :], G1m.t[0:C, u, C:W2], start=True, stop=True), reads=[Z1b.r, G1m.r], writes=[pd.r])
                            S.op("tensor", lambda e, pf=pf, j=j, u=u: e.matmul(pf.t[0:C, j * 64:(j + 1) * 64], Z1b.t[0:C, u, :], tmr(u, 128 + hd * 64, 128 + hd * 64 + 64), start=True, stop=True), reads=[Z1b.r, TM.r], writes=[pf.r])
                    S.op("scalar", lambda e, pq=pq, u0=u0, nu=nu: e.copy(out=Qh.t[:, u0:u0 + nu, 0:C], in_=pq.t[0:64, 0:nu * C].rearrange("p (u w) -> p u w", u=nu)), reads=[pq.r], writes=[Qh.r])
                    S.op("scalar", lambda e, pe=pe, u0=u0, nu=nu: e.copy(out=Eh.t[:, u0:u0 + nu, :], in_=pe.t[0:64, 0:nu * 64].rearrange("p (u w) -> p u w", u=nu)), reads=[pe.r], writes=[Eh.r])
                    if C > 1:
                        S.op("vector", lambda e, pd=pd, u0=u0, nu=nu: e.tensor_tensor(out=Dh.t[0:C, u0:u0 + nu, :], in0=pd.t[0:C, 0:nu * C].rearrange("p (u w) -> p u w", u=nu), in1=G2m.t[0:C, u0:u0 + nu, C:W2], op=ALU.add),
                             reads=[pd.r, G2m.r], writes=[Dh.r])
                        S.op("vector", lambda e, pf=pf, u0=u0, nu=nu: e.tensor_tensor(out=Fh.t[0:C, u0:u0 + nu, :], in0=pf.t[0:C, 0:nu * 64].rearrange("p (u w) -> p u w", u=nu), in1=TMs[u0 // 8].t[0:C, 0:nu, 256 + hd * 64:256 + hd * 64 + 64], op=ALU.add),
                             reads=[pf.r, TM.r], writes=[Fh.r])
                    else:
                        S.op("vector", lambda e, u0=u0, nu=nu: e.tensor_copy(out=Dh.t[0:1, u0:u0 + nu, :], in_=G2m.t[0:1, u0:u0 + nu, C:W2]), reads=[G2m.r], writes=[Dh.r])
                        S.op("vector", lambda e, u0=u0, nu=nu: e.tensor_copy(out=Fh.t[0:1, u0:u0 + nu, :], in_=TMs[u0 // 8].t[0:1, 0:nu, 256 + hd * 64:256 + hd * 64 + 64]), reads=[TM.r], writes=[Fh.r])
            def seq_step(hd, u, Qh, Eh, Dh, Fh):
                P = slice(hd * 64, hd * 64 + 64)
                if True:
                    cs = slice(u * C, (u + 1) * C)
                    if tl.kind == "p":
                        stb = st_heads[2 * hp + hd]
                        stv = stb.t
                    else:
                        stb, stv = st_s, st_s.t[:, u, hd * 64:hd * 64 + 64]
                    vt = tmr(u, 384 + hd * 64, 384 + hd * 64 + 64)
                    S.op("tensor", lambda e, u=u, cs=cs, stv=stv: e.matmul(ob.t[P, cs], stv, Qh.t[:, u, 0:C], start=True, stop=False), reads=[stb.r, Qh.r], writes=[ob.r])
                    S.op("tensor", lambda e, u=u, cs=cs, vt=vt: e.matmul(ob.t[P, cs], vt, Dh.t[0:C, u, :], start=False, stop=True), reads=[TM.r, Dh.r], writes=[ob.r])
                    pst = self.psum()
                    S.op("tensor", lambda e, u=u, pst=pst, stv=stv: e.matmul(pst.t[0:64, 0:64], Eh.t[:, u, :], stv, start=True, stop=False), reads=[Eh.r, stb.r], writes=[pst.r])
                    S.op("tensor", lambda e, u=u, pst=pst, vt=vt: e.matmul(pst.t[0:64, 0:64], Fh.t[0:C, u, :], vt, start=False, stop=True), reads=[Fh.r, TM.r], writes=[pst.r])
                    S.op("scalar", lambda e, pst=pst, stv=stv: e.copy(out=stv, in_=pst.t[0:64, 0:64]), reads=[pst.r], writes=[stb.r])
            if tl.kind == "s":
                S.dma("sync", st_s.t[:], wkv_in[:, :, hp * 128:(hp + 1) * 128], writes=[st_s.r])
            set0 = (Qh, Eh, Dh, Fh)
            if tl.kind == "p":
                set1 = tuple(QEDF2)
                head_body(0, *set0)
                head_body(1, *set1)
                for u in range(U):
                    seq_step(0, u, *set0)
                    seq_step(1, u, *set1)
            else:
                for hd in range(2):
                    head_body(hd, *set0)
                    for u in range(U):
                        seq_step(hd, u, *set0)
            if tl.kind == "s":
                S.dma("sync", wkv_s_o[:, :, hp * 128:(hp + 1) * 128], st_s.t[:], reads=[st_s.r])
            osb, d = t1, t2
            S.op("scalar", lambda e: e.copy(out=osb.t[:, sl], in_=ob.t[:, sl]), reads=[ob.r], writes=[osb.r])
            pm = self.psum()
            S.op("tensor", lambda e, pm=pm: e.matmul(pm.t[:, sl], bones, osb.t[:, sl], start=True, stop=True), reads=[osb.r, rwc.r], writes=[pm.r])
            S.op("vector", lambda e, pm=pm: e.scalar_tensor_tensor(out=d.t[:, sl], in0=pm.t[:, sl], scalar=-1.0 / 64.0, in1=osb.t[:, sl], op0=ALU.mult, op1=ALU.add), reads=[pm.r, osb.r], writes=[d.r])
            tt(osb.t[:, sl], d.t[:, sl], d.t[:, sl], ALU.mult, [d.r], [osb.r])
            pv2 = self.psum()
            S.op("tensor", lambda e, pv2=pv2: e.matmul(pv2.t[:, sl], bones, osb.t[:, sl], start=True, stop=True), reads=[osb.r, rwc.r], writes=[pv2.r])
            act_(osb.t[:, sl], pv2.t[:, sl], AF.Sqrt, [pv2.r, pvr], [osb.r], scale=1.0 / 64.0, bias=self.pcol("lneps"))
            S.op("vector", lambda e: e.reciprocal(out=osb.t[:, sl], in_=osb.t[:, sl]), writes=[osb.r])
            tt(d.t[:, sl], d.t[:, sl], osb.t[:, sl], ALU.mult, [osb.r], [d.r])
            ts(d.t[:, sl], d.t[:, sl], self.pcol("rw_ln_w", hp), self.pcol("rw_ln_b", hp), ALU.mult, ALU.add, [pvr], [d.r])
            tt(d.t[:, sl], d.t[:, sl], bonus.t[:, sl], ALU.add, [bonus.r], [d.r])
            tt(og.t[:, hp, sl], d.t[:, sl], g.t[:, sl], ALU.mult, [d.r, g.r], [og.r])
        for hp in range(DC):
            pair_body(hp)
        if tl.kind == "p" and tl.idx == self.NPT - 1:
            S.dma("sync", wkv_p_o, st_p.t[:].rearrange("p h i -> p (h i)"), reads=[st_p.r] + [v_.r for v_ in st_heads])
        barrier(S)
        self.load_h(tl, False)
        oin = lambda k: (og.t[:, k, 0:N], [og.r])

        def cons_o(mc, msz, pb):
            S.op("vector", lambda e: e.tensor_tensor(out=hs.t[:, mc, 0:N], in0=hs.t[:, mc, 0:N], in1=pb.t[:, 0:N], op=ALU.add), reads=[pb.r], writes=[hs.r])
        self.proj(w_o, D, D, oin, N, cons_o, wname="rw_o")
        self.store_h(tl)
    for tl in self.tiles:
        tile_body(tl)
    barrier(S)
    self.ring = self.psr


Net.rw_setup = rw_setup
Net.rw_layer = rw_layer


def s5_setup(self):
    c, S = self.cx, self.S
    NS = self.NS
    self.s5_par = c.inp("s5_par", [128, 5 * 64])
    self.s5_B = c.inp("s5_B", [2, 128, 1024])
    self.s5_C = c.inp("s5_C", [2, 128, 1024])
    self.s5_msk = c.inp("s5_msk", [128, 128 + 8])
    self.s5_wglu = c.inp("s5_w_glu", [D, D])
    self.s5_hin = c.inp("s5_h_s", [2, 128, 64 * NS])
    self.s5_hp_o = c.outp("s5_hp_o", [2, 128, 64])
    self.s5_hs_o = c.outp("s5_hs_o", [2, 128, 64 * NS])
    self.KBDd = c.scratch("s5_kbd", [16, 128, 8 * 128], BF16)
    self.WBDd = c.scratch("s5_wbd", [16, 128, 8 * 8 * 2 * 64], BF16)
    self.s5_small = c.sb([128, 6, 64])
    self.s5_mk = c.sb([128, 136])
    S.dma("sync", self.s5_mk.t[:], self.s5_msk, writes=[self.s5_mk.r])


def s5_generate(self):
    S, c = self.S, self.cx
    sm = self.s5_small
    A1r, A1i, A8r, A8i, Hcr, Hci = [sm.t[:, j, :] for j in range(6)]
    tsm = [self.la_t[1].t[:, j * 64:(j + 1) * 64] for j in range(8)] + [self.la_t[2].t[:, j * 64:(j + 1) * 64] for j in range(2)]
    par = self.la_t[0]
    S.dma("sync", par.t[:, 0:320], self.s5_par, writes=[par.r])
    lam_re, lam_im, logdt, halfpi = [par.t[:, j * 64:(j + 1) * 64] for j in range(4)]
    smr = sm.r

    def tt(out, a, b, op, extra=()):
        S.op("vector", lambda e: e.tensor_tensor(out=out, in0=a, in1=b, op=op), reads=[par.r] + list(extra), writes=[smr])

    def tsc(out, a, s1, s2, op0, op1=None):
        if s2 is None:
            S.op("vector", lambda e: e.tensor_scalar(out=out, in0=a, scalar1=s1, scalar2=None, op0=op0), reads=[par.r], writes=[smr])
        else:
            S.op("vector", lambda e: e.tensor_scalar(out=out, in0=a, scalar1=s1, scalar2=s2, op0=op0, op1=op1), reads=[par.r], writes=[smr])

    def act_(out, a, func, **kw):
        S.op("scalar", lambda e: e.activation(out=out, in_=a, func=func, **kw), reads=[par.r, self.pv.r], writes=[smr])
    lr, dt, xr, th, cc, ss, t0, t1, t2, t3 = tsm
    tsc(lr, lam_re, -1e-4, None, ALU.min)
    act_(dt, logdt, AF.Exp)
    tt(xr, lr, dt, ALU.mult)
    tt(th, lam_im, dt, ALU.mult)
    act_(ss, th, AF.Sin, scale=1.0 / 16.0)
    act_(cc, th, AF.Sin, scale=1.0 / 16.0, bias=self.pcol("halfpi"))
    for _ in range(4):
        tt(t0, cc, cc, ALU.mult)
        tt(t1, ss, ss, ALU.mult)
        tt(t2, cc, ss, ALU.mult)
        tt(cc, t0, t1, ALU.subtract)
        tsc(ss, t2, 2.0, None, ALU.mult)
    act_(t3, xr, AF.Exp)
    tt(A1r, t3, cc, ALU.mult)
    tt(A1i, t3, ss, ALU.mult)
    tt(t0, lr, lr, ALU.mult)
    tt(t1, lam_im, lam_im, ALU.mult)
    tt(t0, t0, t1, ALU.add)
    S.op("vector", lambda e: e.reciprocal(out=t0, in_=t0), writes=[smr])
    tsc(t1, A1r, -1.0, None, ALU.add)
    tt(t2, t1, lr, ALU.mult)
    tt(t3, A1i, lam_im, ALU.mult)
    tt(t2, t2, t3, ALU.add)
    tt(cc, t2, t0, ALU.mult)
    tt(t2, A1i, lr, ALU.mult)
    tt(t3, t1, lam_im, ALU.mult)
    tt(t2, t2, t3, ALU.subtract)
    tt(ss, t2, t0, ALU.mult)
    S.op("vector", lambda e: e.tensor_copy(out=A8r, in_=A1r), writes=[smr])
    S.op("vector", lambda e: e.tensor_copy(out=A8i, in_=A1i), writes=[smr])
    for _ in range(3):
        tt(t0, A8r, A8r, ALU.mult)
        tt(t1, A8i, A8i, ALU.mult)
        tt(t2, A8r, A8i, ALU.mult)
        tt(A8r, t0, t1, ALU.subtract)
        tsc(A8i, t2, 2.0, None, ALU.mult)
    S.op("vector", lambda e: e.memset(sm.t[:, 4:6, :], 0.0), writes=[smr])
    big = [View(b.t[:].rearrange("p k n -> p (k n)").rearrange("p (g q) -> p g q", q=16)) for b in self.la_f]
    Wr, Wi, T1, T2, Bb = big
    Cre = View(self.la_Ss.t[:, 0:1024].rearrange("p (g q) -> p g q", q=16))
    Cni = View(self.la_Sbf.t[:, 0:2048].bitcast(F32).rearrange("p (g q) -> p g q", q=16))
    self.s5_Cre, self.s5_Cni = Cre, Cni
    S.dma("sync", Cre.t[:].rearrange("p g q -> p (g q)"), self.s5_C[0], writes=[Cre.r])
    S.dma("sync", Cni.t[:].rearrange("p g q -> p (g q)"), self.s5_C[1], writes=[Cni.r])
    S.op("vector", lambda e: e.tensor_scalar(out=Cni.t[:], in0=Cni.t[:], scalar1=-1.0, scalar2=None, op0=ALU.mult), writes=[Cni.r])
    S.dma("sync", T1.t[:].rearrange("p g q -> p (g q)"), self.s5_B[0], writes=[T1.r])
    S.dma("sync", T2.t[:].rearrange("p g q -> p (g q)"), self.s5_B[1], writes=[T2.r])
    bc = lambda a: a.rearrange("p (g o) -> p g o", o=1).broadcast_to([128, 64, 16])

    def btt(out, a, b, op, rd):
        S.op("vector", lambda e: e.tensor_tensor(out=out.t[:], in0=a, in1=b, op=op), reads=rd + [smr], writes=[out.r])
    btt(Wr, T1.t[:], bc(cc), ALU.mult, [T1.r])
    btt(Bb, T2.t[:], bc(ss), ALU.mult, [T2.r])
    btt(Wr, Wr.t[:], Bb.t[:], ALU.subtract, [Bb.r])
    btt(Wi, T2.t[:], bc(cc), ALU.mult, [T2.r])
    btt(Bb, T1.t[:], bc(ss), ALU.mult, [T1.r])
    btt(Wi, Wi.t[:], Bb.t[:], ALU.add, [Bb.r])
    self.ring = self.psr[0:4] + self.obank
    identF = self.ident
    bdm = self.s5_mk.t[:, 0:128]
    stgK = [View(self.act.t[:, j, 0:128]) for j in range(2)]
    stgW = [View(self.act.t[:, 2 + 2 * j:4 + 2 * j, :].rearrange("p f n -> p (f n)")) for j in range(2)]
    ki = [0]
    for tau in range(8):
        sp = 7 - tau
        for gb in range(16):
            gh, g0 = gb // 8, (gb % 8) * 8
            P = slice(gh * 64, gh * 64 + 64)
            wre = Wr.t[P, g0:g0 + 8, :].rearrange("p g q -> p (g q)")
            wim = Wi.t[P, g0:g0 + 8, :].rearrange("p g q -> p (g q)")
            cre = Cre.t[P, g0:g0 + 8, :].rearrange("p g q -> p (g q)")
            cni = Cni.t[P, g0:g0 + 8, :].rearrange("p g q -> p (g q)")
            pk = self.psum()
            S.op("tensor", lambda e, pk=pk, wre=wre, cre=cre: e.matmul(pk.t[:, 0:128], wre, cre, start=True, stop=False), reads=[Wr.r, Cre.r], writes=[pk.r])
            S.op("tensor", lambda e, pk=pk, wim=wim, cni=cni: e.matmul(pk.t[:, 0:128], wim, cni, start=False, stop=True), reads=[Wi.r, Cni.r], writes=[pk.r])
            sk = stgK[ki[0] % 2]
            S.op("vector", lambda e, pk=pk, sk=sk: e.tensor_tensor(out=sk.t, in0=pk.t[:, 0:128], in1=bdm, op=ALU.mult), reads=[pk.r, self.s5_mk.r], writes=[sk.r])
            S.dma("sync", self.KBDd[gb][:, tau * 128:(tau + 1) * 128], sk.t, reads=[sk.r])
            sw = stgW[ki[0] % 2]
            ki[0] += 1
            for ri, wsrc in enumerate((wre, wim)):
                pt = self.psum()
                S.op("tensor", lambda e, pt=pt, wsrc=wsrc, P=P: e.transpose(out=pt.t[:, 0:64], in_=wsrc, identity=identF.t[P, P]), reads=[Wr.r, Wi.r, identF.r], writes=[pt.r])
                for g8 in range(8):
                    S.op("vector", lambda e, pt=pt, g8=g8, ri=ri, sw=sw: e.tensor_scalar(out=self.s5_swslice(sw, g8, ri), in0=pt.t[:, 0:64], scalar1=self.s5_mk.t[:, 128 + g8:129 + g8], scalar2=None, op0=ALU.mult),
                         reads=[pt.r, self.s5_mk.r], writes=[sw.r])
            dview = self.WBDd[gb].rearrange("p (g s r n) -> p g s r n", g=8, s=8, r=2)[:, :, sp, :, :]
            S.dma("sync", dview, sw.t[:, 0:1024].rearrange("p (g r n) -> p g r n", g=8, r=2), reads=[sw.r])
        if tau < 7:
            btt(T1, Wr.t[:], bc(A1r), ALU.mult, [Wr.r])
            btt(T2, Wi.t[:], bc(A1i), ALU.mult, [Wi.r])
            btt(T1, T1.t[:], T2.t[:], ALU.subtract, [T2.r])
            btt(T2, Wr.t[:], bc(A1i), ALU.mult, [Wr.r])
            btt(Bb, Wi.t[:], bc(A1r), ALU.mult, [Wi.r])
            btt(Wi, T2.t[:], Bb.t[:], ALU.add, [T2.r, Bb.r])
            S.op("vector", lambda e: e.tensor_copy(out=Wr.t[:], in_=T1.t[:]), reads=[T1.r], writes=[Wr.r])
    barrier(S)
    self.ring = self.psr


def s5_swslice(self, sw, g8, ri):
    o = (g8 * 2 + ri) * 64
    return sw.t[:, o:o + 64]


Net.s5_setup = s5_setup
Net.s5_generate = s5_generate
Net.s5_swslice = s5_swslice


def s5_layer(self):
    S, c = self.S, self.cx
    NS = self.NS
    hs, xn = self.hs, self.xn
    sm = self.s5_small
    A1r, A1i, A8r, A8i, Hcr, Hci = [sm.t[:, j, :] for j in range(6)]
    smr = sm.r
    Cre, Cni = self.s5_Cre, self.s5_Cni
    HBr = View(self.la_S.t[:, 0:4096].rearrange("p (g c) -> p g c", c=64))
    actF = self.act.t[:].rearrange("p f n -> p (f n)")
    HBi = View(actF[:, 0:8192].bitcast(F32).rearrange("p (g c) -> p g c", c=64))
    YH = View(actF[0:64, 8192:16384].rearrange("p (t g q) -> p t g q", t=4, q=16))
    Zl = View(actF[:, 16384:16640].bitcast(F32).rearrange("p (r g) -> p r g", r=2))
    RT = [View(actF[:, 16640 + j * 2048:16640 + (j + 1) * 2048].bitcast(F32).rearrange("p (g c) -> p g c", c=64)) for j in range(2)]
    tq = [View(b.t[:, 0:64]) for b in self.la_t] + [View(b.t[:, 64:128]) for b in self.la_t]
    ybuf = hs
    identb = self.identb
    bcs = lambda a, n: a.rearrange("p (g o) -> p g o", o=1).broadcast_to([128, a.shape[1], n])
    self.ring = self.psr[0:4] + self.obank

    def tile_body(tl):
        N = tl.n
        prompt = tl.kind == "p"
        NCH = 64 if prompt else NS
        self.load_h(tl, True)
        self.rms(hs, N, "norm_mix", 0, out_bf=xn)
        barrier(S)
        if not prompt:
            for ri, HB in enumerate((HBr, HBi)):
                S.dma("sync", HB.t[:, :, 0:NS], self.s5_hin[ri].rearrange("p (g s) -> p g s", s=NS), writes=[HB.r])
        for gb in range(16):
            gh, g0 = gb // 8, (gb % 8) * 8
            P = slice(gh * 64, gh * 64 + 64)
            pz = [self.psum(), self.psum()]
            for half in range(2):
                slot = self.wslot()
                wv = slot.t[:, 0:4096].rearrange("p (g s r n) -> p g s r n", g=4, s=8, r=2)
                S.dma("sync", slot.t[:, 0:4096], self.WBDd[gb][:, half * 4096:(half + 1) * 4096], writes=[slot.r])
                for gl in range(4):
                    j = half * 4 + gl
                    for ri in range(2):
                        sps = range(8) if prompt else [7]
                        for si, sp in enumerate(sps):
                            rhs = xn.t[:, gb, sp:N:8] if prompt else xn.t[:, gb, 0:N]
                            S.op("tensor", lambda e, ri=ri, j=j, gl=gl, sp=sp, si=si, rhs=rhs, wv=wv, P=P, pz=pz, nl=len(sps): e.matmul(pz[ri].t[P, j * NCH:(j + 1) * NCH], wv[:, gl, sp, ri, :], rhs, start=(si == 0), stop=(si == nl - 1)),
                                 reads=[slot.r, xn.r], writes=[pz[ri].r])
            for ri, HB in enumerate((HBr, HBi)):
                src = pz[ri].t[P, 0:8 * NCH].rearrange("p (g c) -> p g c", c=NCH)
                if prompt:
                    S.op("scalar", lambda e, HB=HB, src=src, P=P, g0=g0: e.copy(out=HB.t[P, g0:g0 + 8, 1:64], in_=src[:, :, 0:63]), reads=[pz[ri].r], writes=[HB.r])
                    S.op("scalar", lambda e, src=src, P=P, g0=g0, ri=ri: e.copy(out=Zl.t[P, ri, g0:g0 + 8], in_=src[:, :, 63]), reads=[pz[ri].r], writes=[Zl.r])
                else:
                    S.op("scalar", lambda e, HB=HB, src=src, P=P, g0=g0: e.copy(out=HB.t[P, g0:g0 + 8, NS:2 * NS], in_=src), reads=[pz[ri].r], writes=[HB.r])

        def vtt(out, a, b, op, rd, wr):
            S.op("vector", lambda e: e.tensor_tensor(out=out, in0=a, in1=b, op=op), reads=rd + [smr], writes=wr)
        t1, t2, t3, t4 = tq[0], tq[1], tq[2], tq[3]
        if prompt:
            S.op("vector", lambda e: e.tensor_copy(out=HBr.t[:, :, 0], in_=Hcr), reads=[smr], writes=[HBr.r])
            S.op("vector", lambda e: e.tensor_copy(out=HBi.t[:, :, 0], in_=Hci), reads=[smr], writes=[HBi.r])
            for cch in range(64):
                hr, hi = HBr.t[:, :, cch], HBi.t[:, :, cch]
                if cch < 63:
                    nr, ni, wr_, wi_ = HBr.t[:, :, cch + 1], HBi.t[:, :, cch + 1], [HBr.r], [HBi.r]
                else:
                    nr, ni, wr_, wi_ = Zl.t[:, 0, :], Zl.t[:, 1, :], [Zl.r], [Zl.r]
                vtt(t1.t, A8r, hr, ALU.mult, [HBr.r], [t1.r])
                vtt(t2.t, A8i, hi, ALU.mult, [HBi.r], [t2.r])
                vtt(t1.t, t1.t, t2.t, ALU.subtract, [t2.r], [t1.r])
                vtt(nr, nr, t1.t, ALU.add, [t1.r], wr_)
                vtt(t3.t, A8r, hi, ALU.mult, [HBi.r], [t3.r])
                vtt(t4.t, A8i, hr, ALU.mult, [HBr.r], [t4.r])
                vtt(t3.t, t3.t, t4.t, ALU.add, [t4.r], [t3.r])
                vtt(ni, ni, t3.t, ALU.add, [t3.r], wi_)
            S.op("vector", lambda e: e.tensor_copy(out=sm.t[:, 4:6, :], in_=Zl.t[:]), reads=[Zl.r], writes=[smr])
            if tl.idx == self.NPT - 1:
                S.dma("sync", self.s5_hp_o.rearrange("r p g -> p r g"), sm.t[:, 4:6, :], reads=[smr])
            hsl = slice(0, 64)
            nq, tcount = 8, 8
        else:
            a, b, o = slice(0, NS), slice(NS, 2 * NS), slice(2 * NS, 3 * NS)
            R0, R1 = RT[0], RT[1]
            r0 = View(self.la_f[0].t[:].rearrange("p k n -> p (k n)")[:, 0:64 * NS].rearrange("p (g s) -> p g s", s=NS))
            r1 = View(self.la_f[1].t[:].rearrange("p k n -> p (k n)")[:, 0:64 * NS].rearrange("p (g s) -> p g s", s=NS))
            vtt(r0.t, HBr.t[:, :, a], bcs(A1r, NS), ALU.mult, [HBr.r], [r0.r])
            vtt(r1.t, HBi.t[:, :, a], bcs(A1i, NS), ALU.mult, [HBi.r], [r1.r])
            vtt(r0.t, r0.t, r1.t, ALU.subtract, [r1.r], [r0.r])
            vtt(HBr.t[:, :, o], r0.t, HBr.t[:, :, b], ALU.add, [r0.r], [HBr.r])
            vtt(r0.t, HBi.t[:, :, a], bcs(A1r, NS), ALU.mult, [HBi.r], [r0.r])
            vtt(r1.t, HBr.t[:, :, a], bcs(A1i, NS), ALU.mult, [HBr.r], [r1.r])
            vtt(r0.t, r0.t, r1.t, ALU.add, [r1.r], [r0.r])
            vtt(HBi.t[:, :, o], r0.t, HBi.t[:, :, b], ALU.add, [r0.r], [HBi.r])
            for ri, HB in enumerate((HBr, HBi)):
                S.dma("sync", self.s5_hs_o[ri].rearrange("p (g s) -> p g s", s=NS), HB.t[:, :, o], reads=[HB.r])
            hsl = o
            nq, tcount = 0, 1
        for th in range((tcount + 3) // 4):
            tps = list(range(th * 4, min(tcount, th * 4 + 4)))
            for tp in tps:
                if prompt:
                    for gq in range(4):
                        gs = slice(gq * 16, gq * 16 + 16)
                        R0, R1 = RT[0], RT[1]
                        xr_, xi_ = HBr.t[:, gs, :], HBi.t[:, gs, :]
                        ar, ai = bcs(A1r[:, gs], 64), bcs(A1i[:, gs], 64)
                        vtt(R0.t, xr_, ar, ALU.mult, [HBr.r], [R0.r])
                        vtt(R1.t, xi_, ai, ALU.mult, [HBi.r], [R1.r])
                        vtt(R0.t, R0.t, R1.t, ALU.subtract, [R1.r], [R0.r])
                        vtt(R1.t, xr_, ai, ALU.mult, [HBr.r], [R1.r])
                        vtt(xi_, xi_, ar, ALU.mult, [], [HBi.r])
                        vtt(xi_, xi_, R1.t, ALU.add, [R1.r], [HBi.r])
                        S.op("vector", lambda e, xr_=xr_, R0=R0: e.tensor_copy(out=xr_, in_=R0.t), reads=[R0.r], writes=[HBr.r])
                for gh in range(2):
                    P = slice(gh * 64, gh * 64 + 64)
                    for blk in range(2):
                        ph = self.psum()
                        for gi in range(32):
                            gl = blk * 32 + gi
                            S.op("tensor", lambda e, ph=ph, gi=gi, gl=gl, P=P: e.matmul(ph.t[0:NCH, gi * 16:(gi + 1) * 16], HBr.t[P, gl, hsl], Cre.t[P, gl, :], start=True, stop=False), reads=[HBr.r, Cre.r], writes=[ph.r])
                            S.op("tensor", lambda e, ph=ph, gi=gi, gl=gl, P=P: e.matmul(ph.t[0:NCH, gi * 16:(gi + 1) * 16], HBi.t[P, gl, hsl], Cni.t[P, gl, :], start=False, stop=True), reads=[HBi.r, Cni.r], writes=[ph.r])
                        gbase = gh * 64 + blk * 32
                        S.op("scalar", lambda e, ph=ph, gbase=gbase, tp=tp: e.copy(out=YH.t[0:NCH, tp % 4, gbase:gbase + 32, :], in_=ph.t[0:NCH, 0:512].rearrange("p (g q) -> p g q", q=16)), reads=[ph.r], writes=[YH.r])
            for gb in range(16):
                if prompt:
                    kslot = self.wslot()
                    kv = kslot.t[:, 0:1024].rearrange("p (t m) -> p t m", t=8)
                    S.dma("sync", kslot.t[:, 0:1024], self.KBDd[gb], writes=[kslot.r])
                for tp in tps:
                    py = self.psum()
                    if prompt:
                        for sp in range(tp + 1):
                            S.op("tensor", lambda e, py=py, kv=kv, tp=tp, sp=sp, gb=gb: e.matmul(py.t[:, 0:64], kv[:, tp - sp, :], xn.t[:, gb, sp:N:8], start=(sp == 0), stop=False), reads=[kslot.r, xn.r], writes=[py.r])
                    S.op("tensor", lambda e, py=py, tp=tp, gb=gb: e.matmul(py.t[:, 0:NCH], YH.t[0:NCH, tp % 4, gb * 8:(gb + 1) * 8, :].rearrange("p g q -> p (g q)"), identb.t[0:NCH, 0:NCH], start=(not prompt), stop=True),
                         reads=[YH.r, identb.r], writes=[py.r])
                    ysl = ybuf.t[:, gb, tp:N:8] if prompt else ybuf.t[:, gb, 0:N]
                    xsl = xn.t[:, gb, tp:N:8] if prompt else xn.t[:, gb, 0:N]
                    S.op("vector", lambda e, py=py, ysl=ysl, xsl=xsl, gb=gb: e.scalar_tensor_tensor(out=ysl, in0=xsl, scalar=self.pcol("s5_d", gb), in1=py.t[:, 0:NCH], op0=ALU.mult, op1=ALU.add),
                         reads=[py.r, xn.r, self.pv.r], writes=[ybuf.r])
        if self.cfg.get("dbg") and not prompt:
            dbg = self.cx.outp("dbg_y", [128, DC * NS])
            S.dma("sync", dbg.rearrange("p (c s) -> p c s", c=DC), ybuf.t[:, :, 0:N], reads=[ybuf.r])
        for cc_ in range(DC):
            S.op("scalar", lambda e, cc_=cc_: e.activation(out=xn.t[:, cc_, 0:N], in_=ybuf.t[:, cc_, 0:N], func=AF.Gelu_apprx_tanh), reads=[ybuf.r], writes=[xn.r])
        barrier(S)
        self.load_h(tl, True)
        zin = lambda k: (xn.t[:, k, 0:N], [xn.r])
        g_t = tq[4]
        gt = View(self.la_f[2].t[:, 0, :])

        def cons_glu(mc, msz, pb):
            S.op("scalar", lambda e: e.activation(out=gt.t[:, 0:N], in_=pb.t[:, 0:N], func=AF.Sigmoid), reads=[pb.r], writes=[gt.r])
            S.op("vector", lambda e: e.tensor_tensor(out=gt.t[:, 0:N], in0=gt.t[:, 0:N], in1=xn.t[:, mc, 0:N], op=ALU.mult), reads=[xn.r], writes=[gt.r])
            S.op("vector", lambda e: e.tensor_tensor(out=hs.t[:, mc, 0:N], in0=hs.t[:, mc, 0:N], in1=gt.t[:, 0:N], op=ALU.add), reads=[gt.r], writes=[hs.r])
        self.proj(self.s5_wglu, D, D, zin, N, cons_glu, wname="s5_glu")
        self.store_h(tl)
        barrier(S)
    for tl in self.tiles:
        tile_body(tl)
    self.ring = self.psr


Net.s5_layer = s5_layer
```
